# Optimizing a Trainium2 kernel written in Bass

```python
import math
import jax, jax.numpy as jnp
from jax import lax
import numpy as np

D_MODEL = 1024
BATCH = 4
SEQ = 8192
DEPTH = 4

GRID_W = 64
CTX_LEN = 256
EPS = 1e-6
N_HEADS = 8
HEAD_DIM = 64
V_DIM = 2 * HEAD_DIM
QK_W = N_HEADS * 2 * HEAD_DIM
ATTN_W = N_HEADS * V_DIM
ROPE_HALF = HEAD_DIM // 2
ROPE_BASE = 10000.0
Q_BLOCK = 128
POOL_SIZES = (2, 4, 8, 16)
N_POOL_GROUPS = 4
POOL_W = D_MODEL
POOL_G = POOL_W // N_POOL_GROUPS
CONV_W = D_MODEL
CONV_K = 3
N_BRANCH = 3
N_MOD = 6
N_KEYS = 128
N_EXPERTS = N_KEYS * N_KEYS
PEER_HEADS = 8
PEER_TOPK = 16
PEER_QDIM = 256
PEER_HALF = PEER_QDIM // 2
PEER_CHUNK = 128
Q_OFF = 0
K_OFF = Q_OFF + QK_W
V_OFF = K_OFF + QK_W
POOL_OFF = V_OFF + ATTN_W
CIN_OFF = POOL_OFF + POOL_W
CB_OFF = CIN_OFF + CONV_W
CC_OFF = CB_OFF + CONV_W
GATE_OFF = CC_OFF + CONV_W
IN_W = GATE_OFF + N_BRANCH * D_MODEL

kernel_name = "hybrid_diffattn_pool_shortconv_peer_dit"


def rms_norm(x, g):
    xf = x.astype(jnp.float32)
    y = xf * lax.rsqrt(jnp.mean(xf * xf, axis=-1, keepdims=True) + EPS)
    return (y * g.astype(jnp.float32)).astype(x.dtype)


def modulate(xn, shift, scale):
    return xn * (1 + scale) + shift


def axial_rope(n):
    n_rows = n // GRID_W
    rows = jnp.repeat(jnp.arange(n_rows, dtype=jnp.float32), GRID_W)
    cols = jnp.tile(jnp.arange(GRID_W, dtype=jnp.float32), n_rows)
    n_freq = ROPE_HALF // 2
    inv = ROPE_BASE ** (-jnp.arange(n_freq, dtype=jnp.float32) / n_freq)
    ang = jnp.concatenate([rows[:, None] * inv, cols[:, None] * inv], axis=-1)
    return jnp.cos(ang), jnp.sin(ang)


def apply_rope(x, cos, sin):
    cos = cos[None, :, None, None, :]
    sin = sin[None, :, None, None, :]
    x1, x2 = x[..., :ROPE_HALF], x[..., ROPE_HALF:]
    return jnp.concatenate([x1 * cos - x2 * sin, x2 * cos + x1 * sin], axis=-1).astype(x.dtype)


def qk_heads(p, g):
    b, l = p.shape[:2]
    return rms_norm(p.reshape(b, l, N_HEADS, 2, HEAD_DIM), g)


def diff_lambda(lv, lam_init):
    lvf = lv.astype(jnp.float32)
    return jnp.exp(jnp.sum(lvf[0] * lvf[1])) - jnp.exp(jnp.sum(lvf[2] * lvf[3])) + lam_init


def diff_attn_core(q, k, v, lam):
    s = jnp.einsum('bqhid,bkhid->bhiqk', q, k).astype(jnp.float32) * (HEAD_DIM ** -0.5)
    pr = jax.nn.softmax(s, axis=-1)
    a = pr[:, :, 0] - lam * pr[:, :, 1]
    return jnp.einsum('bhqk,bkhe->bqhe', a.astype(v.dtype), v)


def blocked_diff_attn(q, k, v, lam):
    b, l = q.shape[:2]
    nb = l // Q_BLOCK
    qb = q.reshape(b, nb, Q_BLOCK, N_HEADS, 2, HEAD_DIM).swapaxes(0, 1)
    out = lax.map(lambda qq: diff_attn_core(qq, k, v, lam), qb)
    return out.swapaxes(0, 1).reshape(b, l, N_HEADS, V_DIM)


def attn_out_norm(o, g, lam_init):
    b, l = o.shape[:2]
    return (rms_norm(o, g) * (1.0 - lam_init)).reshape(b, l, ATTN_W)


def centred_pool_minus_self(u, w):
    l = u.shape[1]
    lo = w // 2
    hi = w - 1 - lo
    uf = u.astype(jnp.float32)
    cs = jnp.pad(jnp.cumsum(uf, axis=1), ((0, 0), (1, 0), (0, 0)))
    t = jnp.arange(l)
    start = jnp.clip(t - lo, 0, l)
    end = jnp.clip(t + hi + 1, 0, l)
    cnt = (end - start).astype(jnp.float32)
    mean = (cs[:, end] - cs[:, start]) / cnt[None, :, None]
    return (mean - uf).astype(u.dtype)


def pool_branch(u, pool_w, pool_scale):
    b, l, _ = u.shape
    ug = u.reshape(b, l, N_POOL_GROUPS, POOL_G)
    pooled = jnp.stack([centred_pool_minus_self(ug[:, :, i], w) for i, w in enumerate(POOL_SIZES)], axis=2)
    y = jnp.einsum('blgc,gcd->blgd', pooled, pool_w).reshape(b, l, POOL_W)
    return y * pool_scale


def depthwise_conv(u, w):
    pad = CONV_K // 2
    return lax.conv_general_dilated(u, w[:, None, :], window_strides=(1,), padding=((pad, pad),),
                                    dimension_numbers=('NWC', 'WIO', 'NWC'),
                                    feature_group_count=u.shape[-1])


def conv_branch(h, b_gate, c_gate, conv_w, conv_out_w):
    return (b_gate * depthwise_conv(c_gate * h, conv_w)) @ conv_out_w


def mixer_output(o, p, attn_g, lam_init, pool_w, pool_scale, conv_w, conv_out_w, w_out):
    b, l = o.shape[:2]
    attn = attn_out_norm(o, attn_g, lam_init)
    pool = pool_branch(p[..., POOL_OFF:CIN_OFF], pool_w, pool_scale)
    conv = conv_branch(p[..., CIN_OFF:CB_OFF], p[..., CB_OFF:CC_OFF], p[..., CC_OFF:GATE_OFF], conv_w, conv_out_w)
    g = jax.nn.sigmoid(p[..., GATE_OFF:]).reshape(b, l, N_BRANCH, D_MODEL)
    merged = g[:, :, 0] * attn + g[:, :, 1] * pool + g[:, :, 2] * conv
    return merged @ w_out


def peer_ffn(x, w_q, sub_keys, u_emb, v_emb):
    b, l, d = x.shape
    t_all = b * l
    xb = x.reshape(t_all // PEER_CHUNK, PEER_CHUNK, d)

    def chunk(xc):
        q = (xc @ w_q).reshape(PEER_CHUNK, PEER_HEADS, 2, PEER_HALF)
        s = jnp.einsum('thid,hikd->thik', q, sub_keys).astype(jnp.float32)
        s1, i1 = lax.top_k(s[:, :, 0], PEER_TOPK)
        s2, i2 = lax.top_k(s[:, :, 1], PEER_TOPK)
        cand = (s1[..., :, None] + s2[..., None, :]).reshape(PEER_CHUNK, PEER_HEADS, PEER_TOPK * PEER_TOPK)
        cidx = (i1[..., :, None] * N_KEYS + i2[..., None, :]).reshape(PEER_CHUNK, PEER_HEADS, PEER_TOPK * PEER_TOPK)
        top_s, pos = lax.top_k(cand, PEER_TOPK)
        eidx = jnp.take_along_axis(cidx, pos, axis=-1)
        gate = jax.nn.softmax(top_s, axis=-1)
        u = u_emb[eidx]
        hid = jax.nn.gelu(jnp.einsum('td,thkd->thk', xc, u))
        wgt = (gate * hid.astype(jnp.float32)).astype(xc.dtype)
        return jnp.einsum('thk,thkd->td', wgt, v_emb[eidx])

    return lax.map(chunk, xb).reshape(b, l, d)


def setup_inputs(seed: int = 0) -> dict:
    key = jax.random.key(seed)
    ks = jax.random.split(key, 22)
    f32 = jnp.float32
    L = DEPTH
    D = D_MODEL

    def nrm(k, shape, scale):
        return jax.random.normal(k, shape, f32) * scale

    def gain(k, shape):
        return 1.0 + 0.05 * jax.random.normal(k, shape, f32)

    return {
        "x": nrm(ks[0], (BATCH, SEQ, D), 1.0),
        "c": nrm(ks[1], (BATCH, D), 1.0),
        "ctx": nrm(ks[2], (BATCH, CTX_LEN, D), 1.0),
        "c_ctx": nrm(ks[3], (D,), 1.0),
        "norm1_g": gain(ks[4], (L, D)),
        "norm2_g": gain(ks[5], (L, D)),
        "w_mod": nrm(ks[6], (L, D, N_MOD * D), 0.5 * D ** -0.5),
        "b_mod": nrm(ks[7], (L, N_MOD * D), 0.02),
        "w_in": nrm(ks[8], (L, D, IN_W), D ** -0.5),
        "q_norm_g": gain(ks[9], (L, HEAD_DIM)),
        "k_norm_g": gain(ks[10], (L, HEAD_DIM)),
        "lam_vecs": nrm(ks[11], (L, 4, HEAD_DIM), 0.1),
        "attn_norm_g": gain(ks[12], (L, V_DIM)),
        "pool_w": nrm(ks[13], (L, N_POOL_GROUPS, POOL_G, POOL_G), POOL_G ** -0.5),
        "pool_scale": gain(ks[14], (L, POOL_W)),
        "conv_w": nrm(ks[15], (L, CONV_K, CONV_W), CONV_K ** -0.5),
        "conv_out_w": nrm(ks[16], (L, CONV_W, D), CONV_W ** -0.5),
        "w_out": nrm(ks[17], (L, D, D), D ** -0.5),
        "peer_wq": nrm(ks[18], (L, D, PEER_HEADS * PEER_QDIM), D ** -0.5),
        "peer_keys": nrm(ks[19], (L, PEER_HEADS, 2, N_KEYS, PEER_HALF), PEER_HALF ** -0.5),
        "peer_u": nrm(ks[20], (L, N_EXPERTS, D), D ** -0.5),
        "peer_v": nrm(ks[21], (L, N_EXPERTS, D), PEER_HEADS ** -0.5),
    }


def reference(x, c, ctx, c_ctx, norm1_g, norm2_g, w_mod, b_mod, w_in, q_norm_g, k_norm_g,
              lam_vecs, attn_norm_g, pool_w, pool_scale, conv_w, conv_out_w, w_out,
              peer_wq, peer_keys, peer_u, peer_v):
    b, s, d = x.shape
    n_ctx = ctx.shape[1]
    cos, sin = axial_rope(s)
    h, hc = x, ctx
    for l in range(DEPTH):
        last = l == DEPTH - 1
        lam_init = 0.8 - 0.6 * math.exp(-0.3 * l)
        mod = (jax.nn.silu(c) @ w_mod[l] + b_mod[l]).reshape(b, N_MOD, 1, d)
        mod_c = (jax.nn.silu(c_ctx) @ w_mod[l] + b_mod[l]).reshape(N_MOD, 1, 1, d)
        lam = diff_lambda(lam_vecs[l], lam_init)
        wl = w_in[l]

        xn = modulate(rms_norm(h, norm1_g[l]), mod[:, 0], mod[:, 1])
        cn = modulate(rms_norm(hc, norm1_g[l]), mod_c[0], mod_c[1])
        if last:
            pkv = cn @ wl[:, K_OFF:POOL_OFF]
            kc_raw, vc_raw = pkv[..., :QK_W], pkv[..., QK_W:]
        else:
            pc = cn @ wl
            kc_raw, vc_raw = pc[..., K_OFF:V_OFF], pc[..., V_OFF:POOL_OFF]
        kc = qk_heads(kc_raw, k_norm_g[l])
        vc = vc_raw.reshape(b, n_ctx, N_HEADS, V_DIM)

        p = xn @ wl
        q = apply_rope(qk_heads(p[..., Q_OFF:K_OFF], q_norm_g[l]), cos, sin)
        k = apply_rope(qk_heads(p[..., K_OFF:V_OFF], k_norm_g[l]), cos, sin)
        v = p[..., V_OFF:POOL_OFF].reshape(b, s, N_HEADS, V_DIM)
        o = blocked_diff_attn(q, jnp.concatenate([k, kc], axis=1), jnp.concatenate([v, vc], axis=1), lam)
        mix = mixer_output(o, p, attn_norm_g[l], lam_init, pool_w[l], pool_scale[l],
                           conv_w[l], conv_out_w[l], w_out[l])

        if not last:
            qc = qk_heads(pc[..., Q_OFF:K_OFF], q_norm_g[l])
            oc = diff_attn_core(qc, kc, vc, lam)
            mix_c = mixer_output(oc, pc, attn_norm_g[l], lam_init, pool_w[l], pool_scale[l],
                                 conv_w[l], conv_out_w[l], w_out[l])
            hc = hc + mod_c[2] * mix_c
        h = h + mod[:, 2] * mix

        xn2 = modulate(rms_norm(h, norm2_g[l]), mod[:, 3], mod[:, 4])
        h = h + mod[:, 5] * peer_ffn(xn2, peer_wq[l], peer_keys[l], peer_u[l], peer_v[l])
        if not last:
            cn2 = modulate(rms_norm(hc, norm2_g[l]), mod_c[3], mod_c[4])
            hc = hc + mod_c[5] * peer_ffn(cn2, peer_wq[l], peer_keys[l], peer_u[l], peer_v[l])
    return h
```

```python
import math
from contextlib import ExitStack
import numpy as np
import ml_dtypes
import concourse.bass as bass
import concourse.mybir as mybir
from concourse.bass_utils import run_bass_kernel_spmd

F32 = mybir.dt.float32
BF16 = mybir.dt.bfloat16
I32 = mybir.dt.int32
U32 = mybir.dt.uint32
ALU = mybir.AluOpType
AF = mybir.ActivationFunctionType
AX = mybir.AxisListType

D = 1024
KD = 8
EPS = 1e-6
Q_OFF, K_OFF, V_OFF, POOL_OFF, CIN_OFF, CB_OFF, CC_OFF, GATE_OFF = 0, 1024, 2048, 3072, 4096, 5120, 6144, 7168
IN_W = 10240
POOL_SIZES = (2, 4, 8, 16)
N_EXP = 16384


class Buf:
    _n = 0

    def __init__(self, t, name):
        self.t = t
        self.name = name
        self.last_w = None
        self.readers = []
        self.dsem = None
        Buf._n += 1
        self.id = Buf._n

    def __getitem__(self, idx):
        return self.t[idx]


class KB:
    ENG = ('pe', 'act', 'dve', 'pool', 'sp')

    def __init__(self, n_dsem=96):
        self.nc = bass.Bass("TRN2", target_bir_lowering=False)
        nc = self.nc
        self.es = ExitStack()
        self.E = {'pe': nc.tensor, 'act': nc.scalar, 'dve': nc.vector, 'pool': nc.gpsimd, 'sp': nc.sync}
        self.sems = {}
        self.cnt = {}
        for e in self.ENG:
            self.sems[('e', e)] = self.es.enter_context(nc.semaphore("p_" + e))
            self.cnt[('e', e)] = 0
        self.free_dsems = []
        for i in range(n_dsem):
            k = ('d', i)
            self.sems[k] = self.es.enter_context(nc.semaphore("d_%d" % i))
            self.cnt[k] = 0
            self.free_dsems.append(k)
        self.seen = {}
        self.n_ins = 0
        self.stack = [(self.es, [])]

    def push(self):
        self.stack.append((ExitStack(), []))

    def pop(self):
        self.barrier()
        es, bufs = self.stack.pop()
        for b in bufs:
            if b.dsem is not None:
                self.free_dsems.append(b.dsem)
        es.close()

    def sbuf(self, name, shape, dt):
        es, bufs = self.stack[-1]
        t = es.enter_context(self.nc.sbuf_tensor(name + "_%d" % Buf._n, list(shape), dt))
        b = Buf(t, name)
        bufs.append(b)
        return b

    def psum(self, name, shape, dt=F32):
        es, bufs = self.stack[-1]
        t = es.enter_context(self.nc.psum_tensor(name + "_%d" % Buf._n, list(shape), dt))
        b = Buf(t, name)
        bufs.append(b)
        return b

    def _wait(self, eng, key, val):
        k = (eng, key)
        if self.seen.get(k, 0) >= val:
            return
        self.E[eng].wait_ge(self.sems[key], val)
        self.seen[k] = val

    def _deps(self, eng, reads, writes, is_dma=False):
        deps = {}
        me = ('e', eng)

        def add(st):
            key, val = st
            if deps.get(key, 0) < val:
                deps[key] = val
        for b in reads:
            if b.last_w is not None:
                if b.last_w[0] == me and eng == 'pe' and not is_dma:
                    continue
                add(b.last_w)
        for b in writes:
            if b.last_w is not None and (is_dma or b.last_w[0] != me):
                add(b.last_w)
            for r in b.readers:
                if r[0] == me and not is_dma:
                    continue
                add(r)
        for key, val in deps.items():
            self._wait(eng, key, val)

    def _stamp(self, st, reads, writes):
        for b in reads:
            b.readers.append(st)
            if len(b.readers) > 16:
                m = {}
                for k, v in b.readers:
                    if m.get(k, 0) < v:
                        m[k] = v
                b.readers = list(m.items())
        for b in writes:
            b.last_w = st
            b.readers = []

    def op(self, eng, fn, reads=(), writes=()):
        self._deps(eng, reads, writes)
        ins = fn(self.E[eng])
        key = ('e', eng)
        self.cnt[key] += 1
        ins.then_inc(self.sems[key], 1)
        self._stamp((key, self.cnt[key]), reads, writes)
        self.n_ins += 1
        return ins

    def dma(self, q, out, in_, reads=(), writes=(), indirect=None):
        self._deps(q, reads, writes, is_dma=True)
        b = (list(writes) + list(reads))[0]
        if b.dsem is None:
            b.dsem = self.free_dsems.pop(0)
        if indirect is None:
            ins = self.E[q].dma_start(out=out, in_=in_)
        else:
            ins = self.E[q].indirect_dma_start(out=out, out_offset=None, in_=in_, in_offset=indirect)
        self.cnt[b.dsem] += 16
        ins.then_inc(self.sems[b.dsem], 16)
        self._stamp((b.dsem, self.cnt[b.dsem]), reads, writes)
        self.n_ins += 1
        return ins

    def barrier(self):
        for f in self.ENG:
            for key, val in self.cnt.items():
                if val > 0:
                    self._wait(f, key, val)

    def finish(self):
        self.barrier()
        while len(self.stack) > 1:
            self.pop()
        return self.nc


class Ring:
    def __init__(self, bufs):
        self.bufs = bufs
        self.i = 0

    def next(self):
        b = self.bufs[self.i % len(self.bufs)]
        self.i += 1
        return b


def band_mats():
    out = np.zeros((128, 20, 128), np.float32)
    for wi, w in enumerate(POOL_SIZES):
        lo = w // 2
        hi = w - 1 - lo
        for t in range(128):
            for tp in range(-128, 256):
                if t - lo <= tp <= t + hi:
                    if tp < 0:
                        out[tp + 128, wi * 5 + 0, t] = 1.0 / w
                    elif tp < 128:
                        out[tp, wi * 5 + 1, t] = 1.0 / w
                    else:
                        out[tp - 128, wi * 5 + 2, t] = 1.0 / w
            s0, e0 = max(t - lo, 0), t + hi
            cnt = e0 - s0 + 1
            for tp in range(s0, min(e0, 127) + 1):
                out[tp, wi * 5 + 3, t] = 1.0 / cnt
            s1, e1 = t - lo, min(t + hi, 127)
            cnt = e1 - s1 + 1
            for tp in range(max(s1, 0), e1 + 1):
                out[tp, wi * 5 + 4, t] = 1.0 / cnt
        for kind in (1, 3, 4):
            out[:, wi * 5 + kind, :] -= np.eye(128, dtype=np.float32)
    return out


def scat_mats():
    out = np.zeros((128, 3, 384), np.float32)
    for t in range(128):
        if t - 1 >= 0:
            out[t - 1, 1, 0 * 128 + t] = 1.0
        else:
            out[127, 0, 0 * 128 + t] = 1.0
        out[t, 1, 1 * 128 + t] = 1.0
        if t + 1 < 128:
            out[t + 1, 1, 2 * 128 + t] = 1.0
        else:
            out[0, 2, 2 * 128 + t] = 1.0
    return out


def rope_tabs(S, CT):
    n_rows = S // 64
    rows = np.repeat(np.arange(n_rows, dtype=np.float32), 64)
    cols = np.tile(np.arange(64, dtype=np.float32), n_rows)
    inv = (10000.0 ** (-np.arange(16, dtype=np.float32) / 16)).astype(np.float32)
    ang = np.concatenate([rows[:, None] * inv, cols[:, None] * inv], axis=-1)
    cos = np.concatenate([np.cos(ang), np.ones((CT, 32), np.float32)], 0).astype(np.float32)
    sin = np.concatenate([np.sin(ang), np.zeros((CT, 32), np.float32)], 0).astype(np.float32)
    idx = np.arange(128) % 32
    return np.ascontiguousarray(cos[:, idx].T), np.ascontiguousarray(sin[:, idx].T)


def rot_mat():
    R = np.zeros((128, 128), np.float32)
    for p in range(128):
        if (p % 64) < 32:
            R[p + 32, p] = -1.0
        else:
            R[p - 32, p] = 1.0
    return R


def bd_mat():
    M = np.zeros((128, 128), np.float32)
    M[:64, :64] = 1.0 / 64
    M[64:, 64:] = 1.0 / 64
    return M


def build(S, CT, NL, dbg=False):
    kb = KB()
    nc = kb.nc
    T = S + CT
    NT = T // 128
    blocks = [(i * 512, 512) for i in range(S // 512)] + [(S, CT)]
    n_lat_blocks = S // 512
    SB = 256
    sblocks = [(i * SB, SB) for i in range(S // SB)] + [(S, CT)]
    n_lat_sblocks = S // SB

    def din(name, shape, dt=F32):
        return nc.dram_tensor(name, list(shape), dt, kind="ExternalInput").ap()

    def dscr(name, shape, dt):
        return nc.dram_tensor(name, list(shape), dt, kind="ExternalOutput" if dbg else "Internal").ap()

    hT0 = din("hT0", [D, T])
    cvec = din("cvec", [128, KD, 2])
    w_mod = din("w_mod", [NL, D, 6 * D])
    b_modT = din("b_modT", [128, NL, 48])
    g1T = din("g1T", [128, NL, KD])
    g2T = din("g2T", [128, NL, KD])
    w_in = din("w_in", [NL, D, IN_W])
    qk_g = din("qk_g", [128, NL, 2])
    lam_v = din("lam_v", [NL, 256])
    attn_g = din("attn_g", [NL, 128])
    pool_w = din("pool_w", [NL, D, 256])
    pscT = din("pscT", [128, NL, KD])
    cwT = din("cwT", [128, NL, KD, 3])
    conv_out_w = din("conv_out_w", [NL, D, D])
    w_out = din("w_out", [NL, D, D])
    peer_wq = din("peer_wq", [NL, D, 2048])
    keysT = din("keysT", [NL, 128, 2048])
    peer_u = din("peer_u", [NL * N_EXP, D])
    peer_v = din("peer_v", [NL * N_EXP, D])
    cosT = din("cosT", [128, T])
    sinT = din("sinT", [128, T])
    c_rot = din("c_rot", [128, 128])
    c_bd = din("c_bd", [128, 128])
    c_idf = din("c_idf", [128, 128])
    c_band = din("c_band", [128, 20, 128])
    c_scat = din("c_scat", [128, 3, 384])
    c_iota = din("c_iota", [128, 16])
    out_hT = nc.dram_tensor("out_hT", [D, S], F32, kind="ExternalOutput").ap()

    hT = dscr("s_hT", [D, T], F32)
    xnT = dscr("s_xnT", [D, T], BF16)
    qT = dscr("s_qT", [D, T], BF16)
    kT = dscr("s_kT", [D, T], BF16)
    Vt = dscr("s_Vt", [T, D], BF16)
    uh = dscr("s_uh", [T, 2 * D], BF16)
    attn = dscr("s_attn", [T, D], BF16)
    w_in_b = dscr("s_w_in_b", [D, IN_W], BF16)
    co_b = dscr("s_co_b", [D, D], BF16)
    wo_b = dscr("s_wo_b", [D, D], BF16)
    pw_b = dscr("s_pw_b", [D, 256], BF16)
    wq_b = dscr("s_wq_b", [D, 2048], BF16)
    keys_b = dscr("s_keys_b", [128, 2048], BF16)

    def fm(ap2d, c0, n):
        return ap2d[:, c0:c0 + n].rearrange("(k p) t -> p k t", p=128)

    ones_m = kb.sbuf("ones_m", [128, 128], F32)
    rot_s = kb.sbuf("rot_s", [128, 128], F32)
    bd_s = kb.sbuf("bd_s", [128, 128], F32)
    idf = kb.sbuf("idf", [128, 128], F32)
    idb = kb.sbuf("idb", [128, 128], BF16)
    modT = kb.sbuf("modT", [128, NL, 48, 2], F32)
    A1 = kb.sbuf("A1", [128, NL, KD, 2], F32)
    A2 = kb.sbuf("A2", [128, NL, KD, 2], F32)
    g1s = kb.sbuf("g1s", [128, NL, KD], F32)
    g2s = kb.sbuf("g2s", [128, NL, KD], F32)
    qkg = kb.sbuf("qkg", [128, NL, 2], F32)
    psc = kb.sbuf("psc", [128, NL, KD], F32)
    cws = kb.sbuf("cws", [128, NL, KD, 3], F32)
    iota16 = kb.sbuf("iota16", [128, 16], F32)

    kb.op('dve', lambda e: e.memset(ones_m[:], 1.0 / D), writes=[ones_m])
    for sb, src in ((rot_s, c_rot), (bd_s, c_bd), (idf, c_idf), (g1s, g1T), (g2s, g2T), (qkg, qk_g),
                    (psc, pscT), (cws, cwT), (iota16, c_iota)):
        kb.dma('sp', sb[:], src, writes=[sb])
    kb.op('dve', lambda e: e.tensor_copy(out=idb[:], in_=idf[:]), reads=[idf], writes=[idb])
    band = kb.sbuf("band", [128, 20, 128], BF16)
    scat = kb.sbuf("scat", [128, 3, 384], BF16)
    kb.push()
    bandf = kb.sbuf("bandf", [128, 20, 128], F32)
    scatf = kb.sbuf("scatf", [128, 3, 384], F32)
    kb.dma('sp', bandf[:], c_band, writes=[bandf])
    kb.dma('sp', scatf[:], c_scat, writes=[scatf])
    kb.op('dve', lambda e: e.tensor_copy(out=band[:], in_=bandf[:]), reads=[bandf], writes=[band])
    kb.op('dve', lambda e: e.tensor_copy(out=scat[:], in_=scatf[:]), reads=[scatf], writes=[scat])
    kb.pop()

    kb.push()
    stg = Ring([kb.sbuf("cp%d" % i, [128, KD, 512], F32) for i in range(2)])
    for (t0, n) in blocks:
        b = stg.next()
        kb.dma('sp', b[:, :, :n], fm(hT0, t0, n), writes=[b])
        kb.dma('sp', fm(hT, t0, n), b[:, :, :n], reads=[b])
    sc = kb.sbuf("sc", [128, KD, 2], F32)
    scs = kb.sbuf("scs", [128, KD, 2], F32)
    bm = kb.sbuf("bm", [128, NL, 48], F32)
    kb.dma('sp', sc[:], cvec, writes=[sc])
    kb.dma('sp', bm[:], b_modT, writes=[bm])
    kb.op('act', lambda e: e.activation(out=scs[:], in_=sc[:], func=AF.Silu), reads=[sc], writes=[scs])
    wmr = Ring([kb.sbuf("wm%d" % i, [128, KD, 512], F32) for i in range(2)])
    mps = kb.psum("mps", [128, 48, 2])
    for l in range(NL):
        for jt in range(12):
            wm = wmr.next()
            kb.dma('sp', wm[:], fm(w_mod[l], jt * 512, 512), writes=[wm])
            for jj in range(4):
                j = jt * 4 + jj
                for k in range(KD):
                    kb.op('pe', lambda e: e.matmul(mps[:, j, :], lhsT=wm[:, k, jj * 128:(jj + 1) * 128],
                                                   rhs=scs[:, k, :], start=(k == 0), stop=(k == KD - 1)),
                          reads=[wm, scs], writes=[mps])
        kb.op('dve', lambda e: e.tensor_tensor(out=modT[:, l], in0=mps[:],
                                               in1=bm[:, l, :].unsqueeze(2).to_broadcast([128, 48, 2]), op=ALU.add),
              reads=[mps, bm], writes=[modT])
        kb.op('dve', lambda e: e.scalar_tensor_tensor(
            out=A1[:, l], in0=modT[:, l, 8:16, :], scalar=1.0,
            in1=g1s[:, l, :].unsqueeze(2).to_broadcast([128, KD, 2]), op0=ALU.add, op1=ALU.mult),
            reads=[modT, g1s], writes=[A1])
        kb.op('dve', lambda e: e.scalar_tensor_tensor(
            out=A2[:, l], in0=modT[:, l, 32:40, :], scalar=1.0,
            in1=g2s[:, l, :].unsqueeze(2).to_broadcast([128, KD, 2]), op0=ALU.add, op1=ALU.mult),
            reads=[modT, g2s], writes=[A2])
    kb.pop()

    def convert(src2d, dst2d, R, C):
        kb.push()
        sf = Ring([kb.sbuf("cvf%d" % i, [128, 2048], F32) for i in range(3)])
        sb_ = Ring([kb.sbuf("cvb%d" % i, [128, 2048], BF16) for i in range(3)])
        engs = ['pool', 'act', 'dve']
        i = 0
        for r0 in range(0, R, 128):
            for c0 in range(0, C, 2048):
                cw = min(2048, C - c0)
                a = sf.next()
                b = sb_.next()
                kb.dma('sp', a[:, :cw], src2d[r0:r0 + 128, c0:c0 + cw], writes=[a])
                eng = engs[i % 3]
                i += 1
                if eng == 'act':
                    kb.op('act', lambda e: e.copy(out=b[:, :cw], in_=a[:, :cw]), reads=[a], writes=[b])
                else:
                    kb.op(eng, lambda e: e.tensor_copy(out=b[:, :cw], in_=a[:, :cw]), reads=[a], writes=[b])
                kb.dma('sp', dst2d[r0:r0 + 128, c0:c0 + cw], b[:, :cw], reads=[b])
        kb.pop()

    def rsqrt_eps(ob, oap, ib, iap, eps):
        kb.op('dve', lambda e: e.tensor_scalar(out=oap, in0=iap, scalar1=eps, scalar2=None, op0=ALU.add), reads=[ib], writes=[ob])
        kb.op('act', lambda e: e.activation(out=oap, in_=oap, func=AF.Sqrt), reads=[ob], writes=[ob])
        kb.op('dve', lambda e: e.reciprocal(out=oap, in_=oap), reads=[ob], writes=[ob])

    def norm_block(hb, n, Acol, Bcol, outs, sqr, psn, rs, tmpr):
        for k in range(KD):
            sq = sqr.next()
            kb.op('act', lambda e: e.activation(out=sq[:, :n], in_=hb[:, k, :n], func=AF.Square), reads=[hb], writes=[sq])
            kb.op('pe', lambda e: e.matmul(psn[:, :n], lhsT=ones_m[:], rhs=sq[:, :n], start=(k == 0), stop=(k == KD - 1)),
                  reads=[ones_m, sq], writes=[psn])
        rsqrt_eps(rs, rs[:, :n], psn, psn[:, :n], EPS)
        for k in range(KD):
            tmp = tmpr.next()
            kb.op('dve', lambda e: e.scalar_tensor_tensor(out=tmp[:, :n], in0=hb[:, k, :n], scalar=Acol(k),
                                                          in1=rs[:, :n], op0=ALU.mult, op1=ALU.mult),
                  reads=[hb, rs, A1, A2], writes=[tmp])
            for ob in outs:
                kb.op('pool', lambda e: e.tensor_scalar(out=ob[:, k, :n], in0=tmp[:, :n], scalar1=Bcol(k), scalar2=None,
                                                        op0=ALU.add), reads=[tmp, modT], writes=[ob])

    for l in range(NL):
        last = (l == NL - 1)
        lam_init = 0.8 - 0.6 * math.exp(-0.3 * l)
        act_blocks = blocks[:n_lat_blocks] if last else blocks
        act_sblocks = sblocks[:n_lat_sblocks] if last else sblocks

        convert(w_in[l], w_in_b, D, IN_W)
        convert(conv_out_w[l], co_b, D, D)
        convert(w_out[l], wo_b, D, D)
        convert(pool_w[l], pw_b, D, 256)
        convert(peer_wq[l], wq_b, D, 2048)
        convert(keysT[l], keys_b, 128, 2048)

        kb.push()
        hb = kb.sbuf("hb", [128, KD, 512], F32)
        xn = kb.sbuf("xn", [128, KD, 512], BF16)
        sqr = Ring([kb.sbuf("sq%d" % i, [128, 512], F32) for i in range(2)])
        tmpr = Ring([kb.sbuf("tmp%d" % i, [128, 512], F32) for i in range(2)])
        rs = kb.sbuf("rs", [128, 512], F32)
        rs2 = kb.sbuf("rs2", [128, 512], F32)
        qn = kb.sbuf("qn", [128, 512], F32)
        t1 = kb.sbuf("t1", [128, 512], F32)
        t2 = kb.sbuf("t2", [128, 512], F32)
        cosb = kb.sbuf("cosb", [128, 512], F32)
        sinb = kb.sbuf("sinb", [128, 512], F32)
        qor = Ring([kb.sbuf("qo%d" % i, [128, 512], BF16) for i in range(2)])
        wr = Ring([kb.sbuf("wA%d" % i, [128, KD, 512], BF16) for i in range(3)])
        vbr = Ring([kb.sbuf("vb%d" % i, [128, 512], BF16) for i in range(2)])
        c1r = Ring([kb.sbuf("c1%d" % i, [128, 512], F32) for i in range(2)])
        psn = kb.psum("psn", [128, 512])
        psr = Ring([kb.psum("psA%d" % i, [128, 512]) for i in range(3)])
        ps2 = kb.psum("ps2", [128, 512])
        ps3 = kb.psum("ps3", [128, 512])
        psc2 = kb.psum("psc2", [128, 512])

        def loadw(col0):
            w = wr.next()
            kb.dma('sp', w[:], fm(w_in_b, col0, 512), writes=[w])
            return w

        for bi, (t0, n) in enumerate(blocks):
            nt = n // 128
            mc = 0 if bi < n_lat_blocks else 1
            kb.dma('sp', hb[:, :, :n], fm(hT, t0, n), writes=[hb])
            kb.dma('sp', cosb[:, :n], cosT[:, t0:t0 + n], writes=[cosb])
            kb.dma('sp', sinb[:, :n], sinT[:, t0:t0 + n], writes=[sinb])
            norm_block(hb, n, lambda k: A1[:, l, k, mc:mc + 1], lambda k: modT[:, l, k, mc:mc + 1], [xn],
                       sqr, psn, rs, tmpr)
            kb.dma('sp', fm(xnT, t0, n), xn[:, :, :n], reads=[xn])
            for (coloff, dst, gcol) in ((Q_OFF, qT, 0), (K_OFF, kT, 1)):
                for wt in range(2):
                    w = loadw(coloff + wt * 512)
                    for hh in range(4):
                        h = wt * 4 + hh
                        ps = psr.next()
                        for k in range(KD):
                            kb.op('pe', lambda e: e.matmul(ps[:, :n], lhsT=w[:, k, hh * 128:(hh + 1) * 128], rhs=xn[:, k, :n],
                                                           start=(k == 0), stop=(k == KD - 1)), reads=[w, xn], writes=[ps])
                        sq = sqr.next()
                        kb.op('act', lambda e: e.activation(out=sq[:, :n], in_=ps[:, :n], func=AF.Square), reads=[ps], writes=[sq])
                        kb.op('pe', lambda e: e.matmul(ps2[:, :n], lhsT=bd_s[:], rhs=sq[:, :n], start=True, stop=True),
                              reads=[bd_s, sq], writes=[ps2])
                        rsqrt_eps(rs2, rs2[:, :n], ps2, ps2[:, :n], EPS)
                        kb.op('dve', lambda e: e.scalar_tensor_tensor(out=qn[:, :n], in0=ps[:, :n], scalar=qkg[:, l, gcol:gcol + 1],
                                                                      in1=rs2[:, :n], op0=ALU.mult, op1=ALU.mult),
                              reads=[ps, rs2, qkg], writes=[qn])
                        kb.op('pe', lambda e: e.matmul(ps3[:, :n], lhsT=rot_s[:], rhs=qn[:, :n], start=True, stop=True),
                              reads=[rot_s, qn], writes=[ps3])
                        kb.op('pool', lambda e: e.tensor_tensor(out=t1[:, :n], in0=qn[:, :n], in1=cosb[:, :n], op=ALU.mult),
                              reads=[qn, cosb], writes=[t1])
                        kb.op('dve', lambda e: e.tensor_tensor(out=t2[:, :n], in0=ps3[:, :n], in1=sinb[:, :n], op=ALU.mult),
                              reads=[ps3, sinb], writes=[t2])
                        qo = qor.next()
                        kb.op('pool', lambda e: e.tensor_tensor(out=qo[:, :n], in0=t1[:, :n], in1=t2[:, :n], op=ALU.add),
                              reads=[t1, t2], writes=[qo])
                        kb.dma('sp', dst[h * 128:(h + 1) * 128, t0:t0 + n], qo[:, :n], reads=[qo])
            for (coloff, dstap, dcol) in ((V_OFF, Vt, 0), (POOL_OFF, uh, 0)):
                for wt in range(2):
                    w = loadw(coloff + wt * 512)
                    for ti in range(nt):
                        ps = psr.next()
                        for k in range(KD):
                            kb.op('pe', lambda e: e.matmul(ps[:], lhsT=xn[:, k, ti * 128:(ti + 1) * 128], rhs=w[:, k, :],
                                                           start=(k == 0), stop=(k == KD - 1)), reads=[w, xn], writes=[ps])
                        vb = vbr.next()
                        kb.op('act', lambda e: e.copy(out=vb[:], in_=ps[:]), reads=[ps], writes=[vb])
                        r0 = t0 + ti * 128
                        kb.dma('sp', dstap[r0:r0 + 128, dcol + wt * 512: dcol + (wt + 1) * 512], vb[:], reads=[vb])
            for wt in range(2):
                w1 = loadw(CIN_OFF + wt * 512)
                w2 = loadw(CC_OFF + wt * 512)
                for ti in range(nt):
                    ps = psr.next()
                    for k in range(KD):
                        kb.op('pe', lambda e: e.matmul(ps[:], lhsT=xn[:, k, ti * 128:(ti + 1) * 128], rhs=w1[:, k, :],
                                                       start=(k == 0), stop=(k == KD - 1)), reads=[w1, xn], writes=[ps])
                    for k in range(KD):
                        kb.op('pe', lambda e: e.matmul(psc2[:], lhsT=xn[:, k, ti * 128:(ti + 1) * 128], rhs=w2[:, k, :],
                                                       start=(k == 0), stop=(k == KD - 1)), reads=[w2, xn], writes=[psc2])
                    c1 = c1r.next()
                    kb.op('act', lambda e: e.copy(out=c1[:], in_=ps[:]), reads=[ps], writes=[c1])
                    vb = vbr.next()
                    kb.op('dve', lambda e: e.tensor_tensor(out=vb[:], in0=psc2[:], in1=c1[:], op=ALU.mult),
                          reads=[psc2, c1], writes=[vb])
                    r0 = t0 + ti * 128
                    kb.dma('sp', uh[r0:r0 + 128, D + wt * 512: D + (wt + 1) * 512], vb[:], reads=[vb])
        kb.pop()

        kb.push()
        lvb = kb.sbuf("lvb", [128, 256], F32)
        lprod = kb.sbuf("lprod", [128, 128], F32)
        lsum = kb.sbuf("lsum", [128, 2], F32)
        lexp = kb.sbuf("lexp", [128, 2], F32)
        nlam = kb.sbuf("nlam", [128, 1], F32)
        gvec = kb.sbuf("gvec", [128, 128], F32)
        kb.dma('sp', lvb[:], lam_v[l:l + 1, :].partition_broadcast(128), writes=[lvb])
        kb.dma('sp', gvec[:], attn_g[l:l + 1, :].partition_broadcast(128), writes=[gvec])
        lv4 = lvb[:].rearrange("p (a d) -> p a d", a=4)
        kb.op('dve', lambda e: e.tensor_tensor(out=lprod[:, 0:64], in0=lv4[:, 0, :], in1=lv4[:, 1, :], op=ALU.mult),
              reads=[lvb], writes=[lprod])
        kb.op('dve', lambda e: e.tensor_tensor(out=lprod[:, 64:128], in0=lv4[:, 2, :], in1=lv4[:, 3, :], op=ALU.mult),
              reads=[lvb, lprod], writes=[lprod])
        kb.op('dve', lambda e: e.tensor_reduce(out=lsum[:], in_=lprod[:].rearrange("p (a d) -> p a d", a=2), axis=AX.X, op=ALU.add),
              reads=[lprod], writes=[lsum])
        kb.op('act', lambda e: e.activation(out=lexp[:], in_=lsum[:], func=AF.Exp), reads=[lsum], writes=[lexp])
        kb.op('dve', lambda e: e.tensor_tensor(out=nlam[:], in0=lexp[:, 1:2], in1=lexp[:, 0:1], op=ALU.subtract),
              reads=[lexp], writes=[nlam])
        kb.op('dve', lambda e: e.tensor_scalar(out=nlam[:], in0=nlam[:], scalar1=-lam_init, scalar2=None, op0=ALU.add),
              reads=[nlam], writes=[nlam])
        kb.op('dve', lambda e: e.tensor_scalar(out=gvec[:], in0=gvec[:], scalar1=math.sqrt(128.0) * (1.0 - lam_init),
                                               scalar2=None, op0=ALU.mult), reads=[gvec], writes=[gvec])

        kTr = Ring([kb.sbuf("kTh%d" % i, [128, T], BF16) for i in range(2)])
        qTr = Ring([kb.sbuf("qTh%d" % i, [128, T], BF16) for i in range(2)])
        var = Ring([kb.sbuf("vaug%d" % i, [128, NT, 129], BF16) for i in range(2)])
        for vb_ in var.bufs:
            kb.op('pool', lambda e: e.memset(vb_[:], 1.0), writes=[vb_])
        ptr = Ring([kb.sbuf("pt%d" % i, [128, 512], BF16) for i in range(4)])
        osb = [kb.sbuf("osb%d" % i, [128, 4, 129], F32) for i in range(2)]
        rz = kb.sbuf("rz", [128, 2, 4], F32)
        ocb = kb.sbuf("ocb", [128, 128], F32)
        osq = kb.sbuf("osq", [128, 128], F32)
        oss = kb.sbuf("oss", [128, 1], F32)
        ors = kb.sbuf("ors", [128, 1], F32)
        aor = Ring([kb.sbuf("ao%d" % i, [128, 4, 128], BF16) for i in range(2)])
        pss = Ring([kb.psum("pS%d" % i, [128, 512]) for i in range(3)])
        pso = kb.psum("pO", [128, 4, 512])
        for h in range(8):
            kh = kTr.next()
            qh = qTr.next()
            va = var.next()
            kb.dma('sp', kh[:], kT[h * 128:(h + 1) * 128, :], writes=[kh])
            kb.dma('sp', qh[:], qT[h * 128:(h + 1) * 128, :], writes=[qh])
            for c0 in range(0, NT, 16):
                cn = min(16, NT - c0)
                kb.dma('sp', va[:, c0:c0 + cn, 0:128],
                       Vt[c0 * 128:(c0 + cn) * 128, h * 128:(h + 1) * 128].rearrange("(c p) e -> p c e", p=128), writes=[va])
            for bi, (t0, n) in enumerate(act_blocks):
                nq = n // 128
                is_ctx = bi >= n_lat_blocks
                kcs = list(range(S // 128, NT)) if is_ctx else list(range(NT))
                for s in range(2):
                    for ci, kc in enumerate(kcs):
                        ps = pss.next()
                        kb.op('pe', lambda e: e.matmul(ps[:, :n], lhsT=kh[s * 64:(s + 1) * 64, kc * 128:(kc + 1) * 128],
                                                       rhs=qh[s * 64:(s + 1) * 64, t0:t0 + n], start=True, stop=True),
                              reads=[kh, qh], writes=[ps])
                        pt = ptr.next()
                        kb.op('act', lambda e: e.activation(out=pt[:, :n], in_=ps[:, :n], func=AF.Exp, scale=0.125),
                              reads=[ps], writes=[pt])
                        for qs in range(nq):
                            kb.op('pe', lambda e: e.matmul(pso[:, qs, 0:129], lhsT=pt[:, qs * 128:(qs + 1) * 128], rhs=va[:, kc, :],
                                                           start=(ci == 0), stop=(ci == len(kcs) - 1)),
                                  reads=[pt, va], writes=[pso])
                    kb.op('dve', lambda e: e.tensor_copy(out=osb[s][:, :nq, :], in_=pso[:, :nq, 0:129]), reads=[pso], writes=[osb[s]])
                kb.op('dve', lambda e: e.reciprocal(out=rz[:, 0, :nq], in_=osb[0][:, :nq, 128]), reads=[osb[0]], writes=[rz])
                kb.op('dve', lambda e: e.reciprocal(out=rz[:, 1, :nq], in_=osb[1][:, :nq, 128]), reads=[osb[1]], writes=[rz])
                kb.op('dve', lambda e: e.tensor_scalar(out=rz[:, 1, :nq], in0=rz[:, 1, :nq], scalar1=nlam[:, 0:1], scalar2=None,
                                                       op0=ALU.mult), reads=[rz, nlam], writes=[rz])
                ao = aor.next()
                for qs in range(nq):
                    kb.op('dve', lambda e: e.tensor_scalar(out=ocb[:], in0=osb[0][:, qs, 0:128], scalar1=rz[:, 0, qs:qs + 1],
                                                           scalar2=None, op0=ALU.mult), reads=[osb[0], rz], writes=[ocb])
                    kb.op('dve', lambda e: e.scalar_tensor_tensor(out=ocb[:], in0=osb[1][:, qs, 0:128], scalar=rz[:, 1, qs:qs + 1],
                                                                  in1=ocb[:], op0=ALU.mult, op1=ALU.add),
                          reads=[osb[1], rz, ocb], writes=[ocb])
                    kb.op('dve', lambda e: e.tensor_tensor(out=osq[:], in0=ocb[:], in1=ocb[:], op=ALU.mult), reads=[ocb], writes=[osq])
                    kb.op('dve', lambda e: e.tensor_reduce(out=oss[:], in_=osq[:], axis=AX.X, op=ALU.add), reads=[osq], writes=[oss])
                    rsqrt_eps(ors, ors[:], oss, oss[:], 128.0 * EPS)
                    kb.op('dve', lambda e: e.scalar_tensor_tensor(out=ao[:, qs, :], in0=ocb[:], scalar=ors[:, 0:1], in1=gvec[:],
                                                                  op0=ALU.mult, op1=ALU.mult), reads=[ocb, ors, gvec], writes=[ao])
                kb.dma('sp', attn[t0:t0 + n, h * 128:(h + 1) * 128].rearrange("(q p) e -> p q e", p=128), ao[:, :nq, :], reads=[ao])
        kb.pop()

        kb.push()
        cow = kb.sbuf("cow", [128, KD, D], BF16)
        wow = kb.sbuf("wow", [128, KD, D], BF16)
        pww = kb.sbuf("pww", [128, KD, 256], BF16)
        kb.dma('sp', cow[:], fm(co_b, 0, D), writes=[cow])
        kb.dma('sp', wow[:], fm(wo_b, 0, D), writes=[wow])
        kb.dma('sp', pww[:], fm(pw_b, 0, 256), writes=[pww])
        xn = kb.sbuf("xnB", [128, KD, SB], BF16)
        uht = kb.sbuf("uht", [128, 4, 2 * D], BF16)
        att = kb.sbuf("att", [128, 2, D], BF16)
        cbT = kb.sbuf("cbT", [128, KD, SB], BF16)
        plT = kb.sbuf("plT", [128, KD, SB], BF16)
        bcT = kb.sbuf("bcT", [128, KD, SB], BF16)
        mgT = kb.sbuf("mgT", [128, KD, SB], BF16)
        wcr = Ring([kb.sbuf("wcb%d" % i, [128, KD, 512], BF16) for i in range(2)])
        gwr = Ring([kb.sbuf("gw%d" % i, [128, KD, 3, 128], BF16) for i in range(2)])
        gTr = Ring([kb.sbuf("gT%d" % i, [128, 3, SB], F32) for i in range(2)])
        hcr = Ring([kb.sbuf("hcb%d" % i, [128, SB], F32) for i in range(2)])
        hnr = Ring([kb.sbuf("hnb%d" % i, [128, SB], F32) for i in range(2)])
        m0 = kb.sbuf("m0", [128, SB], F32)
        m1 = kb.sbuf("m1", [128, SB], F32)
        m2 = kb.sbuf("m2", [128, SB], F32)
        cacc = Ring([kb.sbuf("cacc%d" % i, [128, 128], F32) for i in range(2)])
        psr = Ring([kb.psum("psB%d" % i, [128, 512]) for i in range(4)])
        pcv = Ring([kb.psum("pcv%d" % i, [128, 512]) for i in range(2)])
        ptp = Ring([kb.psum("ptp%d" % i, [128, 512], BF16) for i in range(2)])
        for bi, (t0, n) in enumerate(act_sblocks):
            nt = n // 128
            tile0 = t0 // 128
            is_ctx = bi >= n_lat_sblocks
            mc = 1 if is_ctx else 0
            seq_first = (tile0 == 0) or (tile0 == S // 128)
            seq_last_tile = (S // 128 - 1) if not is_ctx else (NT - 1)
            kb.dma('sp', xn[:, :, :n], fm(xnT, t0, n), writes=[xn])
            kb.dma('sp', att[:, :nt, :], attn[t0:t0 + n, :].rearrange("(q p) e -> p q e", p=128), writes=[att])
            lo_t = tile0 if seq_first else tile0 - 1
            hi_t = min(tile0 + nt, seq_last_tile)
            kb.dma('sp', uht[:, lo_t - (tile0 - 1): hi_t - (tile0 - 1) + 1, :],
                   uh[lo_t * 128:(hi_t + 1) * 128, :].rearrange("(q p) e -> p q e", p=128), writes=[uht])
            for wt in range(2):
                w = wcr.next()
                kb.dma('sp', w[:], fm(w_in_b, CB_OFF + wt * 512, 512), writes=[w])
                for cc in range(4):
                    ps = psr.next()
                    for k in range(KD):
                        kb.op('pe', lambda e: e.matmul(ps[:, :n], lhsT=w[:, k, cc * 128:(cc + 1) * 128], rhs=xn[:, k, :n],
                                                       start=(k == 0), stop=(k == KD - 1)), reads=[w, xn], writes=[ps])
                    kb.op('act', lambda e: e.copy(out=cbT[:, wt * 4 + cc, :n], in_=ps[:, :n]), reads=[ps], writes=[cbT])
            for c in range(KD):
                wi = c // 2
                ps = psr.next()
                for ti in range(nt):
                    tg = tile0 + ti
                    first = (tg == 0) or (tg == S // 128)
                    lastt = (tg == seq_last_tile)
                    terms = []
                    if not first:
                        terms.append((ti, wi * 5 + 0))
                    terms.append((ti + 1, wi * 5 + (3 if first else (4 if lastt else 1))))
                    if not lastt:
                        terms.append((ti + 2, wi * 5 + 2))
                    for j, (slot, kind) in enumerate(terms):
                        kb.op('pe', lambda e: e.matmul(ps[:, ti * 128:(ti + 1) * 128], lhsT=uht[:, slot, c * 128:(c + 1) * 128],
                                                       rhs=band[:, kind, :], start=(j == 0), stop=(j == len(terms) - 1)),
                              reads=[uht, band], writes=[ps])
                kb.op('act', lambda e: e.copy(out=plT[:, c, :n], in_=ps[:, :n]), reads=[ps], writes=[plT])
            for c in range(KD):
                for ti in range(nt):
                    tg = tile0 + ti
                    first = (tg == 0) or (tg == S // 128)
                    lastt = (tg == seq_last_tile)
                    terms = [(ti + 1, 1)]
                    if not first:
                        terms.append((ti, 0))
                    if not lastt:
                        terms.append((ti + 2, 2))
                    pc = pcv.next()
                    for j, (slot, kind) in enumerate(terms):
                        kb.op('pe', lambda e: e.matmul(pc[:, 0:384], lhsT=uht[:, slot, D + c * 128: D + (c + 1) * 128],
                                                       rhs=scat[:, kind, :], start=(j == 0), stop=(j == len(terms) - 1)),
                              reads=[uht, scat], writes=[pc])
                    ca = cacc.next()
                    kb.op('dve', lambda e: e.tensor_scalar(out=ca[:], in0=pc[:, 0:128], scalar1=cws[:, l, c, 0:1], scalar2=None,
                                                           op0=ALU.mult), reads=[pc, cws], writes=[ca])
                    kb.op('dve', lambda e: e.scalar_tensor_tensor(out=ca[:], in0=pc[:, 128:256], scalar=cws[:, l, c, 1:2], in1=ca[:],
                                                                  op0=ALU.mult, op1=ALU.add), reads=[pc, cws, ca], writes=[ca])
                    kb.op('dve', lambda e: e.scalar_tensor_tensor(out=ca[:], in0=pc[:, 256:384], scalar=cws[:, l, c, 2:3], in1=ca[:],
                                                                  op0=ALU.mult, op1=ALU.add), reads=[pc, cws, ca], writes=[ca])
                    kb.op('pool', lambda e: e.tensor_tensor(out=bcT[:, c, ti * 128:(ti + 1) * 128], in0=ca[:],
                                                            in1=cbT[:, c, ti * 128:(ti + 1) * 128], op=ALU.mult),
                          reads=[ca, cbT], writes=[bcT])
            for dc in range(KD):
                gw = gwr.next()
                for j in range(3):
                    kb.dma('sp', gw[:, :, j, :], fm(w_in_b, GATE_OFF + j * D + dc * 128, 128), writes=[gw])
                gT = gTr.next()
                for j in range(3):
                    ps = psr.next()
                    for k in range(KD):
                        kb.op('pe', lambda e: e.matmul(ps[:, :n], lhsT=gw[:, k, j, :], rhs=xn[:, k, :n],
                                                       start=(k == 0), stop=(k == KD - 1)), reads=[gw, xn], writes=[ps])
                    kb.op('act', lambda e: e.activation(out=gT[:, j, :n], in_=ps[:, :n], func=AF.Sigmoid), reads=[ps], writes=[gT])
                pt_ = ptp.next()
                for ti in range(nt):
                    kb.op('pe', lambda e: e.transpose(pt_[:, ti * 128:(ti + 1) * 128], att[:, ti, dc * 128:(dc + 1) * 128], idb[:]),
                          reads=[att, idb], writes=[pt_])
                kb.op('dve', lambda e: e.tensor_tensor(out=m0[:, :n], in0=pt_[:, :n], in1=gT[:, 0, :n], op=ALU.mult),
                      reads=[pt_, gT], writes=[m0])
                ps = psr.next()
                g = dc // 2
                dh = dc % 2
                for kk in range(2):
                    kb.op('pe', lambda e: e.matmul(ps[:, :n], lhsT=pww[:, g * 2 + kk, dh * 128:(dh + 1) * 128], rhs=plT[:, g * 2 + kk, :n],
                                                   start=(kk == 0), stop=(kk == 1)), reads=[pww, plT], writes=[ps])
                kb.op('dve', lambda e: e.scalar_tensor_tensor(out=m1[:, :n], in0=ps[:, :n], scalar=psc[:, l, dc:dc + 1], in1=gT[:, 1, :n],
                                                              op0=ALU.mult, op1=ALU.mult), reads=[ps, psc, gT], writes=[m1])
                ps = psr.next()
                for k in range(KD):
                    kb.op('pe', lambda e: e.matmul(ps[:, :n], lhsT=cow[:, k, dc * 128:(dc + 1) * 128], rhs=bcT[:, k, :n],
                                                   start=(k == 0), stop=(k == KD - 1)), reads=[cow, bcT], writes=[ps])
                kb.op('dve', lambda e: e.tensor_tensor(out=m2[:, :n], in0=ps[:, :n], in1=gT[:, 2, :n], op=ALU.mult),
                      reads=[ps, gT], writes=[m2])
                kb.op('pool', lambda e: e.tensor_tensor(out=m0[:, :n], in0=m0[:, :n], in1=m1[:, :n], op=ALU.add),
                      reads=[m0, m1], writes=[m0])
                kb.op('pool', lambda e: e.tensor_tensor(out=mgT[:, dc, :n], in0=m0[:, :n], in1=m2[:, :n], op=ALU.add),
                      reads=[m0, m2], writes=[mgT])
            for dc in range(KD):
                ps = psr.next()
                for k in range(KD):
                    kb.op('pe', lambda e: e.matmul(ps[:, :n], lhsT=wow[:, k, dc * 128:(dc + 1) * 128], rhs=mgT[:, k, :n],
                                                   start=(k == 0), stop=(k == KD - 1)), reads=[wow, mgT], writes=[ps])
                hc_ = hcr.next()
                kb.dma('sp', hc_[:, :n], hT[dc * 128:(dc + 1) * 128, t0:t0 + n], writes=[hc_])
                hn = hnr.next()
                kb.op('dve', lambda e: e.scalar_tensor_tensor(out=hn[:, :n], in0=ps[:, :n], scalar=modT[:, l, 16 + dc, mc:mc + 1],
                                                              in1=hc_[:, :n], op0=ALU.mult, op1=ALU.add),
                      reads=[ps, modT, hc_], writes=[hn])
                kb.dma('sp', hT[dc * 128:(dc + 1) * 128, t0:t0 + n], hn[:, :n], reads=[hn])
        kb.pop()

        kb.push()
        wqs = kb.sbuf("wqs", [128, KD, 2048], BF16)
        kys = kb.sbuf("kys", [128, 16, 128], BF16)
        kb.dma('sp', wqs[:], fm(wq_b, 0, 2048), writes=[wqs])
        kb.dma('sp', kys[:], keys_b.rearrange("p (g k) -> p g k", g=16), writes=[kys])
        hb = kb.sbuf("hbC", [128, KD, SB], F32)
        xf = kb.sbuf("xfC", [128, KD, SB], F32)
        xb = kb.sbuf("xbC", [128, KD, SB], BF16)
        sqr = Ring([kb.sbuf("sqC%d" % i, [128, SB], F32) for i in range(2)])
        tmpr = Ring([kb.sbuf("tmpC%d" % i, [128, SB], F32) for i in range(2)])
        rs = kb.sbuf("rsC", [128, SB], F32)
        qp = kb.sbuf("qp", [128, 16, SB], BF16)
        sS = kb.sbuf("sS", [128, 16, 128], F32)
        sS2 = kb.sbuf("sS2", [128, 16, 128], F32)
        stop_ = kb.sbuf("stop", [128, 16, 16], F32)
        itop = kb.sbuf("itop", [128, 16, 16], U32)
        itf = kb.sbuf("itf", [128, 16, 16], F32)
        cand = kb.sbuf("cand", [128, 8, 256], F32)
        cand2 = kb.sbuf("cand2", [128, 8, 256], F32)
        tops = kb.sbuf("tops", [128, 8, 16], F32)
        pos = kb.sbuf("pos", [128, 8, 16], U32)
        pa = kb.sbuf("pa", [128, 8, 16], U32)
        pbb = kb.sbuf("pbb", [128, 8, 16], U32)
        paf = kb.sbuf("paf", [128, 8, 16], F32)
        pbf = kb.sbuf("pbf", [128, 8, 16], F32)
        oh = kb.sbuf("oh", [128, 8, 16, 16], F32)
        i1s = kb.sbuf("i1s", [128, 8, 16], F32)
        i2s = kb.sbuf("i2s", [128, 8, 16], F32)
        eif = kb.sbuf("eif", [128, 128], F32)
        eii = kb.sbuf("eii", [128, 128], I32)
        mx = kb.sbuf("mx", [128, 8], F32)
        ex = kb.sbuf("ex", [128, 8, 16], F32)
        esum = kb.sbuf("esum", [128, 8], F32)
        gate = kb.sbuf("gate", [128, 8, 16], F32)
        xtok = kb.sbuf("xtok", [128, D], F32)
        hid = kb.sbuf("hid", [128, 128], F32)
        gl1 = kb.sbuf("gl1", [128, 128], F32)
        gl2 = kb.sbuf("gl2", [128, 128], F32)
        wgt = kb.sbuf("wgt", [128, 128], F32)
        acc = kb.sbuf("accC", [128, D], F32)
        NG = 4
        ubr = Ring([kb.sbuf("ub%d" % i, [128, D], F32) for i in range(NG)])
        vbr2 = Ring([kb.sbuf("vbC%d" % i, [128, D], F32) for i in range(NG)])
        prod = kb.sbuf("prodC", [128, D], F32)
        hnr = Ring([kb.sbuf("hnC%d" % i, [128, 128], F32) for i in range(2)])
        psn = kb.psum("psnC", [128, 512])
        psq = Ring([kb.psum("psq%d" % i, [128, 512]) for i in range(2)])
        pssc = kb.psum("pssc", [128, 4, 512])
        pst = kb.psum("pstC", [128, 512])
        for bi, (t0, n) in enumerate(act_sblocks):
            nt = n // 128
            mc = 1 if bi >= n_lat_sblocks else 0
            kb.dma('sp', hb[:, :, :n], fm(hT, t0, n), writes=[hb])
            norm_block(hb, n, lambda k: A2[:, l, k, mc:mc + 1], lambda k: modT[:, l, 24 + k, mc:mc + 1], [xf, xb],
                       sqr, psn, rs, tmpr)
            for g in range(16):
                ps = psq.next()
                for k in range(KD):
                    kb.op('pe', lambda e: e.matmul(ps[:, :n], lhsT=wqs[:, k, g * 128:(g + 1) * 128], rhs=xb[:, k, :n],
                                                   start=(k == 0), stop=(k == KD - 1)), reads=[wqs, xb], writes=[ps])
                kb.op('act', lambda e: e.copy(out=qp[:, g, :n], in_=ps[:, :n]), reads=[ps], writes=[qp])
            for ti in range(nt):
                tsl = slice(ti * 128, (ti + 1) * 128)
                for g in range(16):
                    kb.op('pe', lambda e: e.matmul(pssc[:, g // 4, (g % 4) * 128:(g % 4 + 1) * 128], lhsT=qp[:, g, tsl], rhs=kys[:, g, :],
                                                   start=True, stop=True), reads=[qp, kys], writes=[pssc])
                kb.op('act', lambda e: e.copy(out=sS[:].rearrange("p (a b) k -> p a (b k)", a=4), in_=pssc[:]), reads=[pssc], writes=[sS])
                for g in range(16):
                    kb.op('dve', lambda e: e.max(out=stop_[:, g, 0:8], in_=sS[:, g, :]), reads=[sS], writes=[stop_])
                    kb.op('dve', lambda e: e.max_index(out=itop[:, g, 0:8], in_max=stop_[:, g, 0:8], in_values=sS[:, g, :]),
                          reads=[sS, stop_], writes=[itop])
                    kb.op('dve', lambda e: e.match_replace(out=sS2[:, g, :], in_to_replace=stop_[:, g, 0:8], in_values=sS[:, g, :],
                                                           imm_value=-1e30), reads=[sS, stop_], writes=[sS2])
                    kb.op('dve', lambda e: e.max(out=stop_[:, g, 8:16], in_=sS2[:, g, :]), reads=[sS2], writes=[stop_])
                    kb.op('dve', lambda e: e.max_index(out=itop[:, g, 8:16], in_max=stop_[:, g, 8:16], in_values=sS2[:, g, :]),
                          reads=[sS2, stop_], writes=[itop])
                kb.op('dve', lambda e: e.tensor_copy(out=itf[:], in_=itop[:]), reads=[itop], writes=[itf])
                s4 = stop_[:].rearrange("p (h i) a -> p h i a", i=2)
                kb.op('dve', lambda e: e.tensor_tensor(
                    out=cand[:].rearrange("p h (a b) -> p h a b", a=16),
                    in0=s4[:, :, 0, :].unsqueeze(3).to_broadcast([128, 8, 16, 16]),
                    in1=s4[:, :, 1, :].unsqueeze(2).to_broadcast([128, 8, 16, 16]), op=ALU.add), reads=[stop_], writes=[cand])
                for h in range(8):
                    kb.op('dve', lambda e: e.max(out=tops[:, h, 0:8], in_=cand[:, h, :]), reads=[cand], writes=[tops])
                    kb.op('dve', lambda e: e.max_index(out=pos[:, h, 0:8], in_max=tops[:, h, 0:8], in_values=cand[:, h, :]),
                          reads=[cand, tops], writes=[pos])
                    kb.op('dve', lambda e: e.match_replace(out=cand2[:, h, :], in_to_replace=tops[:, h, 0:8], in_values=cand[:, h, :],
                                                           imm_value=-1e30), reads=[cand, tops], writes=[cand2])
                    kb.op('dve', lambda e: e.max(out=tops[:, h, 8:16], in_=cand2[:, h, :]), reads=[cand2], writes=[tops])
                    kb.op('dve', lambda e: e.max_index(out=pos[:, h, 8:16], in_max=tops[:, h, 8:16], in_values=cand2[:, h, :]),
                          reads=[cand2, tops], writes=[pos])
                kb.op('dve', lambda e: e.tensor_single_scalar(out=pa[:], in_=pos[:], scalar=4, op=ALU.logical_shift_right),
                      reads=[pos], writes=[pa])
                kb.op('dve', lambda e: e.tensor_single_scalar(out=pbb[:], in_=pos[:], scalar=15, op=ALU.bitwise_and),
                      reads=[pos], writes=[pbb])
                kb.op('dve', lambda e: e.tensor_copy(out=paf[:], in_=pa[:]), reads=[pa], writes=[paf])
                kb.op('dve', lambda e: e.tensor_copy(out=pbf[:], in_=pbb[:]), reads=[pbb], writes=[pbf])
                i4 = itf[:].rearrange("p (h i) a -> p h i a", i=2)
                for (pf, isel, ii) in ((paf, i1s, 0), (pbf, i2s, 1)):
                    kb.op('dve', lambda e: e.tensor_tensor(
                        out=oh[:], in0=pf[:].unsqueeze(3).to_broadcast([128, 8, 16, 16]),
                        in1=iota16[:].unsqueeze(1).unsqueeze(1).to_broadcast([128, 8, 16, 16]), op=ALU.is_equal),
                        reads=[pf, iota16], writes=[oh])
                    kb.op('dve', lambda e: e.tensor_tensor(
                        out=oh[:], in0=oh[:], in1=i4[:, :, ii, :].unsqueeze(2).to_broadcast([128, 8, 16, 16]), op=ALU.mult),
                        reads=[oh, itf], writes=[oh])
                    kb.op('dve', lambda e: e.tensor_reduce(out=isel[:], in_=oh[:], axis=AX.X, op=ALU.add), reads=[oh], writes=[isel])
                kb.op('dve', lambda e: e.scalar_tensor_tensor(out=eif[:].rearrange("p (h k) -> p h k", h=8), in0=i1s[:], scalar=128.0,
                                                              in1=i2s[:], op0=ALU.mult, op1=ALU.add), reads=[i1s, i2s], writes=[eif])
                kb.op('dve', lambda e: e.tensor_scalar(out=eii[:], in0=eif[:], scalar1=float(l * N_EXP), scalar2=None, op0=ALU.add),
                      reads=[eif], writes=[eii])
                kb.op('dve', lambda e: e.tensor_tensor(out=ex[:], in0=tops[:], in1=tops[:, :, 0:1].to_broadcast([128, 8, 16]),
                                                       op=ALU.subtract), reads=[tops], writes=[ex])
                kb.op('act', lambda e: e.activation(out=ex[:], in_=ex[:], func=AF.Exp), reads=[ex], writes=[ex])
                kb.op('dve', lambda e: e.tensor_reduce(out=esum[:], in_=ex[:], axis=AX.X, op=ALU.add), reads=[ex], writes=[esum])
                kb.op('dve', lambda e: e.reciprocal(out=esum[:], in_=esum[:]), reads=[esum], writes=[esum])
                kb.op('dve', lambda e: e.tensor_tensor(out=gate[:], in0=ex[:], in1=esum[:].unsqueeze(2).to_broadcast([128, 8, 16]),
                                                       op=ALU.mult), reads=[ex, esum], writes=[gate])
                for k in range(KD):
                    if k % 4 == 0:
                        pass
                    kb.op('pe', lambda e: e.transpose(pst[:, (k % 4) * 128:(k % 4 + 1) * 128], xf[:, k, tsl], idf[:]),
                          reads=[xf, idf], writes=[pst])
                    if k % 4 == 3:
                        kb.op('act', lambda e: e.copy(out=xtok[:, (k - 3) * 128:(k + 1) * 128], in_=pst[:]), reads=[pst], writes=[xtok])
                for s_ in range(128):
                    ub = ubr.next()
                    kb.dma('pool', ub[:], peer_u, writes=[ub], reads=[eii],
                           indirect=bass.IndirectOffsetOnAxis(ap=eii[:, s_:s_ + 1], axis=0))
                    kb.op('dve', lambda e: e.tensor_tensor(out=prod[:], in0=ub[:], in1=xtok[:], op=ALU.mult),
                          reads=[ub, xtok], writes=[prod])
                    kb.op('dve', lambda e: e.tensor_reduce(out=hid[:, s_:s_ + 1], in_=prod[:], axis=AX.X, op=ALU.add),
                          reads=[prod], writes=[hid])
                kb.op('dve', lambda e: e.tensor_tensor(out=gl1[:], in0=hid[:], in1=hid[:], op=ALU.mult), reads=[hid], writes=[gl1])
                kb.op('dve', lambda e: e.tensor_scalar(out=gl1[:], in0=gl1[:], scalar1=0.044715, scalar2=1.0, op0=ALU.mult, op1=ALU.add),
                      reads=[gl1], writes=[gl1])
                kb.op('dve', lambda e: e.tensor_tensor(out=gl1[:], in0=gl1[:], in1=hid[:], op=ALU.mult), reads=[gl1, hid], writes=[gl1])
                kb.op('act', lambda e: e.activation(out=gl2[:], in_=gl1[:], func=AF.Tanh, scale=math.sqrt(2.0 / math.pi)),
                      reads=[gl1], writes=[gl2])
                kb.op('dve', lambda e: e.tensor_scalar(out=gl2[:], in0=gl2[:], scalar1=1.0, scalar2=0.5, op0=ALU.add, op1=ALU.mult),
                      reads=[gl2], writes=[gl2])
                kb.op('dve', lambda e: e.tensor_tensor(out=gl2[:], in0=gl2[:], in1=hid[:], op=ALU.mult), reads=[gl2, hid], writes=[gl2])
                kb.op('dve', lambda e: e.tensor_tensor(out=wgt[:], in0=gl2[:], in1=gate[:].rearrange("p h k -> p (h k)"), op=ALU.mult),
                      reads=[gl2, gate], writes=[wgt])
                for s_ in range(128):
                    vb = vbr2.next()
                    kb.dma('pool', vb[:], peer_v, writes=[vb], reads=[eii],
                           indirect=bass.IndirectOffsetOnAxis(ap=eii[:, s_:s_ + 1], axis=0))
                    if s_ == 0:
                        kb.op('dve', lambda e: e.tensor_scalar(out=acc[:], in0=vb[:], scalar1=wgt[:, 0:1], scalar2=None, op0=ALU.mult),
                              reads=[vb, wgt], writes=[acc])
                    else:
                        kb.op('dve', lambda e: e.scalar_tensor_tensor(out=acc[:], in0=vb[:], scalar=wgt[:, s_:s_ + 1], in1=acc[:],
                                                                      op0=ALU.mult, op1=ALU.add), reads=[vb, wgt, acc], writes=[acc])
                for k in range(KD):
                    pq = psq.next()
                    kb.op('pe', lambda e: e.transpose(pq[:, 0:128], acc[:, k * 128:(k + 1) * 128], idf[:]), reads=[acc, idf], writes=[pq])
                    hn = hnr.next()
                    kb.op('dve', lambda e: e.scalar_tensor_tensor(out=hn[:], in0=pq[:, 0:128], scalar=modT[:, l, 40 + k, mc:mc + 1],
                                                                  in1=hb[:, k, tsl], op0=ALU.mult, op1=ALU.add),
                          reads=[pq, modT, hb], writes=[hn])
                    tt = t0 + ti * 128
                    if last:
                        kb.dma('sp', out_hT[k * 128:(k + 1) * 128, tt:tt + 128], hn[:], reads=[hn])
                    else:
                        kb.dma('sp', hT[k * 128:(k + 1) * 128, tt:tt + 128], hn[:], reads=[hn])
        kb.pop()

    return kb.finish(), kb


def _fmaj(v):
    v = np.asarray(v, np.float32)
    lead = v.shape[:-1]
    r = v.reshape(lead + (KD, 128))
    r = np.moveaxis(r, -1, 0)
    return np.ascontiguousarray(r)


def prep_inputs(S, CT, NL, b, x, c, ctx, c_ctx, norm1_g, norm2_g, w_mod, b_mod, w_in, q_norm_g, k_norm_g,
                lam_vecs, attn_norm_g, pool_w, pool_scale, conv_w, conv_out_w, w_out, peer_wq, peer_keys, peer_u, peer_v,
                shared=None):
    f = np.float32
    if shared is None:
        cos, sin = rope_tabs(S, CT)
        shared = {
            "w_mod": np.ascontiguousarray(w_mod, f),
            "b_modT": np.ascontiguousarray(np.asarray(b_mod, f).reshape(NL, 48, 128).transpose(2, 0, 1)),
            "g1T": _fmaj(norm1_g), "g2T": _fmaj(norm2_g),
            "w_in": np.ascontiguousarray(w_in, f),
            "qk_g": np.ascontiguousarray(np.stack([np.tile(np.asarray(q_norm_g, f), (1, 2)), np.tile(np.asarray(k_norm_g, f), (1, 2))], -1).transpose(1, 0, 2)),
            "lam_v": np.ascontiguousarray(np.asarray(lam_vecs, f).reshape(NL, 256)),
            "attn_g": np.ascontiguousarray(attn_norm_g, f),
            "pool_w": np.ascontiguousarray(np.asarray(pool_w, f).reshape(NL, D, 256)),
            "pscT": _fmaj(pool_scale),
            "cwT": np.ascontiguousarray(np.moveaxis(_fmaj(conv_w), 2, 3)),
            "conv_out_w": np.ascontiguousarray(conv_out_w, f),
            "w_out": np.ascontiguousarray(w_out, f),
            "peer_wq": np.ascontiguousarray(peer_wq, f),
            "keysT": np.ascontiguousarray(np.asarray(peer_keys, f).reshape(NL, 16, 128, 128).transpose(0, 3, 1, 2).reshape(NL, 128, 2048)),
            "peer_u": np.ascontiguousarray(np.asarray(peer_u, f).reshape(NL * N_EXP, D)),
            "peer_v": np.ascontiguousarray(np.asarray(peer_v, f).reshape(NL * N_EXP, D)),
            "cosT": cos, "sinT": sin,
            "c_rot": rot_mat(), "c_bd": bd_mat(), "c_idf": np.eye(128, dtype=f),
            "c_band": band_mats(), "c_scat": scat_mats(),
            "c_iota": np.ascontiguousarray(np.tile(np.arange(16, dtype=f), (128, 1))),
        }
    m = dict(shared)
    m["hT0"] = np.ascontiguousarray(np.concatenate([np.asarray(x[b], f).T, np.asarray(ctx[b], f).T], axis=1))
    m["cvec"] = np.ascontiguousarray(np.stack([_fmaj(c[b]), _fmaj(c_ctx)], -1))
    return m, shared


_CACHE = {}


def kernel(**inputs):
    x = np.asarray(inputs["x"])
    B, S, _ = x.shape
    CT = inputs["ctx"].shape[1]
    NL = inputs["w_mod"].shape[0]
    key = (S, CT, NL)
    if key not in _CACHE:
        _CACHE[key] = build(S, CT, NL)[0]
    nc = _CACHE[key]
    in_maps = []
    shared = None
    for core in range(8):
        m, shared = prep_inputs(S, CT, NL, core % B, shared=shared, **inputs)
        in_maps.append(m)
    res = run_bass_kernel_spmd(nc, in_maps, core_ids=list(range(8)))
    out = np.stack([np.ascontiguousarray(np.asarray(res.results[b]["out_hT"]).T) for b in range(B)], 0)
    return out.astype(np.float32)
```

```python
import math
from contextlib import ExitStack
import numpy as np
import ml_dtypes
import concourse.bass as bass
import concourse.mybir as mybir
from concourse.bass_utils import run_bass_kernel_spmd

F32 = mybir.dt.float32
BF16 = mybir.dt.bfloat16
I32 = mybir.dt.int32
U32 = mybir.dt.uint32
ALU = mybir.AluOpType
AF = mybir.ActivationFunctionType
AX = mybir.AxisListType

D = 1024
KD = 8
EPS = 1e-6
Q_OFF, K_OFF, V_OFF, POOL_OFF, CIN_OFF, CB_OFF, CC_OFF, GATE_OFF = 0, 1024, 2048, 3072, 4096, 5120, 6144, 7168
IN_W = 10240
POOL_SIZES = (2, 4, 8, 16)
N_EXP = 16384


class Buf:
    _n = 0

    def __init__(self, t, name):
        self.t = t
        self.name = name
        self.last_w = None
        self.readers = []
        self.dsem = None
        Buf._n += 1
        self.id = Buf._n

    def __getitem__(self, idx):
        return self.t[idx]


class KB:
    ENG = ('pe', 'act', 'dve', 'pool', 'sp')

    def __init__(self, n_dsem=96):
        self.nc = bass.Bass("TRN2", target_bir_lowering=False)
        nc = self.nc
        self.es = ExitStack()
        self.E = {'pe': nc.tensor, 'act': nc.scalar, 'dve': nc.vector, 'pool': nc.gpsimd, 'sp': nc.sync}
        self.sems = {}
        self.cnt = {}
        for e in self.ENG:
            self.sems[('e', e)] = self.es.enter_context(nc.semaphore("p_" + e))
            self.cnt[('e', e)] = 0
        self.free_dsems = []
        for i in range(n_dsem):
            k = ('d', i)
            self.sems[k] = self.es.enter_context(nc.semaphore("d_%d" % i))
            self.cnt[k] = 0
            self.free_dsems.append(k)
        self.seen = {}
        self.n_ins = 0
        self.stack = [(self.es, [])]

    def push(self):
        self.stack.append((ExitStack(), []))

    def pop(self, tag=None):
        if tag is not None:
            self.log = getattr(self, "log", [])
            self.log.append((tag, self.n_ins))
        self.barrier()
        es, bufs = self.stack.pop()
        for b in bufs:
            if b.dsem is not None:
                self.free_dsems.append(b.dsem)
        es.close()

    def sbuf(self, name, shape, dt):
        es, bufs = self.stack[-1]
        t = es.enter_context(self.nc.sbuf_tensor(name + "_%d" % Buf._n, list(shape), dt))
        b = Buf(t, name)
        bufs.append(b)
        return b

    def psum(self, name, shape, dt=F32):
        es, bufs = self.stack[-1]
        t = es.enter_context(self.nc.psum_tensor(name + "_%d" % Buf._n, list(shape), dt))
        b = Buf(t, name)
        bufs.append(b)
        return b

    def _wait(self, eng, key, val):
        k = (eng, key)
        if self.seen.get(k, 0) >= val:
            return
        self.E[eng].wait_ge(self.sems[key], val)
        self.seen[k] = val

    def _deps(self, eng, reads, writes, is_dma=False):
        deps = {}
        me = ('e', eng)

        def add(st):
            key, val = st
            if deps.get(key, 0) < val:
                deps[key] = val
        for b in reads:
            if b.last_w is not None:
                if b.last_w[0] == me and eng == 'pe' and not is_dma:
                    continue
                add(b.last_w)
        for b in writes:
            if b.last_w is not None and (is_dma or b.last_w[0] != me):
                add(b.last_w)
            for r in b.readers:
                if r[0] == me and not is_dma:
                    continue
                add(r)
        for key, val in deps.items():
            self._wait(eng, key, val)

    def _stamp(self, st, reads, writes):
        for b in reads:
            b.readers.append(st)
            if len(b.readers) > 16:
                m = {}
                for k, v in b.readers:
                    if m.get(k, 0) < v:
                        m[k] = v
                b.readers = list(m.items())
        for b in writes:
            b.last_w = st
            b.readers = []

    def op(self, eng, fn, reads=(), writes=()):
        self._deps(eng, reads, writes)
        ins = fn(self.E[eng])
        key = ('e', eng)
        self.cnt[key] += 1
        ins.then_inc(self.sems[key], 1)
        self._stamp((key, self.cnt[key]), reads, writes)
        self.n_ins += 1
        return ins

    def dma(self, q, out, in_, reads=(), writes=(), indirect=None):
        self._deps(q, reads, writes, is_dma=True)
        b = (list(writes) + list(reads))[0]
        if b.dsem is None:
            b.dsem = self.free_dsems.pop(0)
        if indirect is None:
            ins = self.E[q].dma_start(out=out, in_=in_)
        else:
            ins = self.E[q].indirect_dma_start(out=out, out_offset=None, in_=in_, in_offset=indirect)
        self.cnt[b.dsem] += 16
        ins.then_inc(self.sems[b.dsem], 16)
        self._stamp((b.dsem, self.cnt[b.dsem]), reads, writes)
        self.n_ins += 1
        return ins

    def barrier(self):
        for f in self.ENG:
            for key, val in self.cnt.items():
                if val > 0:
                    self._wait(f, key, val)

    def finish(self):
        self.barrier()
        while len(self.stack) > 1:
            self.pop()
        return self.nc


class Ring:
    def __init__(self, bufs):
        self.bufs = bufs
        self.i = 0

    def next(self):
        b = self.bufs[self.i % len(self.bufs)]
        self.i += 1
        return b


def band_mats():
    out = np.zeros((128, 20, 128), np.float32)
    for wi, w in enumerate(POOL_SIZES):
        lo = w // 2
        hi = w - 1 - lo
        for t in range(128):
            for tp in range(-128, 256):
                if t - lo <= tp <= t + hi:
                    if tp < 0:
                        out[tp + 128, wi * 5 + 0, t] = 1.0 / w
                    elif tp < 128:
                        out[tp, wi * 5 + 1, t] = 1.0 / w
                    else:
                        out[tp - 128, wi * 5 + 2, t] = 1.0 / w
            s0, e0 = max(t - lo, 0), t + hi
            cnt = e0 - s0 + 1
            for tp in range(s0, min(e0, 127) + 1):
                out[tp, wi * 5 + 3, t] = 1.0 / cnt
            s1, e1 = t - lo, min(t + hi, 127)
            cnt = e1 - s1 + 1
            for tp in range(max(s1, 0), e1 + 1):
                out[tp, wi * 5 + 4, t] = 1.0 / cnt
        for kind in (1, 3, 4):
            out[:, wi * 5 + kind, :] -= np.eye(128, dtype=np.float32)
    return out


def scat_mats():
    out = np.zeros((128, 3, 384), np.float32)
    for t in range(128):
        if t - 1 >= 0:
            out[t - 1, 1, 0 * 128 + t] = 1.0
        else:
            out[127, 0, 0 * 128 + t] = 1.0
        out[t, 1, 1 * 128 + t] = 1.0
        if t + 1 < 128:
            out[t + 1, 1, 2 * 128 + t] = 1.0
        else:
            out[0, 2, 2 * 128 + t] = 1.0
    return out


def rope_tabs(S, CT):
    n_rows = S // 64
    rows = np.repeat(np.arange(n_rows, dtype=np.float32), 64)
    cols = np.tile(np.arange(64, dtype=np.float32), n_rows)
    inv = (10000.0 ** (-np.arange(16, dtype=np.float32) / 16)).astype(np.float32)
    ang = np.concatenate([rows[:, None] * inv, cols[:, None] * inv], axis=-1)
    cos = np.concatenate([np.cos(ang), np.ones((CT, 32), np.float32)], 0).astype(np.float32)
    sin = np.concatenate([np.sin(ang), np.zeros((CT, 32), np.float32)], 0).astype(np.float32)
    idx = np.arange(128) % 32
    return np.ascontiguousarray(cos[:, idx].T), np.ascontiguousarray(sin[:, idx].T)


def rot_mat():
    R = np.zeros((128, 128), np.float32)
    for p in range(128):
        if (p % 64) < 32:
            R[p + 32, p] = -1.0
        else:
            R[p - 32, p] = 1.0
    return R


def bd_mat():
    M = np.zeros((128, 128), np.float32)
    M[:64, :64] = 1.0 / 64
    M[64:, 64:] = 1.0 / 64
    return M


def build(S, CT, NL, dbg=False):
    kb = KB()
    nc = kb.nc
    T = S + CT
    NT = T // 128
    blocks = [(i * 512, 512) for i in range(S // 512)] + [(S, CT)]
    n_lat_blocks = S // 512
    SB = 256
    sblocks = [(i * SB, SB) for i in range(S // SB)] + [(S, CT)]
    n_lat_sblocks = S // SB

    def din(name, shape, dt=F32):
        return nc.dram_tensor(name, list(shape), dt, kind="ExternalInput").ap()

    def dscr(name, shape, dt):
        return nc.dram_tensor(name, list(shape), dt, kind="ExternalOutput" if dbg else "Internal").ap()

    hT0 = din("hT0", [D, T])
    cvec = din("cvec", [128, KD, 2])
    w_mod = din("w_mod", [NL, D, 6 * D])
    b_modT = din("b_modT", [128, NL, 48])
    g1T = din("g1T", [128, NL, KD])
    g2T = din("g2T", [128, NL, KD])
    w_in = din("w_in", [NL, D, IN_W])
    qk_g = din("qk_g", [128, NL, 2])
    lam_v = din("lam_v", [NL, 256])
    attn_g = din("attn_g", [NL, 128])
    pool_w = din("pool_w", [NL, D, 256])
    pscT = din("pscT", [128, NL, KD])
    cwT = din("cwT", [128, NL, KD, 3])
    conv_out_w = din("conv_out_w", [NL, D, D])
    w_out = din("w_out", [NL, D, D])
    peer_wq = din("peer_wq", [NL, D, 2048])
    keysT = din("keysT", [NL, 128, 2048])
    peer_uL = din("peer_uL", [NL, N_EXP, D])
    peer_v = din("peer_v", [NL, N_EXP, D])
    c_iota128 = din("c_iota128", [128, 128])
    cosT = din("cosT", [128, T])
    sinT = din("sinT", [128, T])
    c_rot = din("c_rot", [128, 128])
    c_bd = din("c_bd", [128, 128])
    c_idf = din("c_idf", [128, 128])
    c_band = din("c_band", [128, 20, 128])
    c_scat = din("c_scat", [128, 3, 384])
    c_iota = din("c_iota", [128, 16])
    out_hT = nc.dram_tensor("out_hT", [D, S], F32, kind="ExternalOutput").ap()

    hT = dscr("s_hT", [D, T], F32)
    xnT = dscr("s_xnT", [D, T], BF16)
    qT = dscr("s_qT", [D, T], BF16)
    kT = dscr("s_kT", [D, T], BF16)
    Vt = dscr("s_Vt", [T, D], BF16)
    uh = dscr("s_uh", [T, 2 * D], BF16)
    attnT = dscr("s_attnT", [D, T], BF16)
    w_in_b = dscr("s_w_in_b", [D, IN_W], BF16)
    co_b = dscr("s_co_b", [D, D], BF16)
    wo_b = dscr("s_wo_b", [D, D], BF16)
    pw_b = dscr("s_pw_b", [D, 256], BF16)
    wq_b = dscr("s_wq_b", [D, 2048], BF16)
    keys_b = dscr("s_keys_b", [128, 2048], BF16)
    uT_b = dscr("s_uT_b", [N_EXP, D], BF16)
    v_b = dscr("s_v_b", [N_EXP, D], BF16)

    def fm(ap2d, c0, n):
        return ap2d[:, c0:c0 + n].rearrange("(k p) t -> p k t", p=128)

    ones_m = kb.sbuf("ones_m", [128, 128], F32)
    rot_s = kb.sbuf("rot_s", [128, 128], F32)
    bd_s = kb.sbuf("bd_s", [128, 128], F32)
    idf = kb.sbuf("idf", [128, 128], F32)
    idb = kb.sbuf("idb", [128, 128], BF16)
    modT = kb.sbuf("modT", [128, NL, 48, 2], F32)
    A1 = kb.sbuf("A1", [128, NL, KD, 2], F32)
    A2 = kb.sbuf("A2", [128, NL, KD, 2], F32)
    g1s = kb.sbuf("g1s", [128, NL, KD], F32)
    g2s = kb.sbuf("g2s", [128, NL, KD], F32)
    qkg = kb.sbuf("qkg", [128, NL, 2], F32)
    psc = kb.sbuf("psc", [128, NL, KD], F32)
    cws = kb.sbuf("cws", [128, NL, KD, 3], F32)
    iota16 = kb.sbuf("iota16", [128, 16], F32)
    iota128 = kb.sbuf("iota128", [128, 128], F32)

    kb.op('dve', lambda e: e.memset(ones_m[:], 1.0 / D), writes=[ones_m])
    for sb, src in ((rot_s, c_rot), (bd_s, c_bd), (idf, c_idf), (g1s, g1T), (g2s, g2T), (qkg, qk_g),
                    (psc, pscT), (cws, cwT), (iota16, c_iota), (iota128, c_iota128)):
        kb.dma('sp', sb[:], src, writes=[sb])
    kb.op('dve', lambda e: e.tensor_copy(out=idb[:], in_=idf[:]), reads=[idf], writes=[idb])
    band = kb.sbuf("band", [128, 20, 128], BF16)
    scat = kb.sbuf("scat", [128, 3, 384], BF16)
    kb.push()
    bandf = kb.sbuf("bandf", [128, 20, 128], F32)
    scatf = kb.sbuf("scatf", [128, 3, 384], F32)
    kb.dma('sp', bandf[:], c_band, writes=[bandf])
    kb.dma('sp', scatf[:], c_scat, writes=[scatf])
    kb.op('dve', lambda e: e.tensor_copy(out=band[:], in_=bandf[:]), reads=[bandf], writes=[band])
    kb.op('dve', lambda e: e.tensor_copy(out=scat[:], in_=scatf[:]), reads=[scatf], writes=[scat])
    kb.pop()

    kb.push()
    stg = Ring([kb.sbuf("cp%d" % i, [128, KD, 512], F32) for i in range(2)])
    for (t0, n) in blocks:
        b = stg.next()
        kb.dma('sp', b[:, :, :n], fm(hT0, t0, n), writes=[b])
        kb.dma('sp', fm(hT, t0, n), b[:, :, :n], reads=[b])
    sc = kb.sbuf("sc", [128, KD, 2], F32)
    scs = kb.sbuf("scs", [128, KD, 2], F32)
    bm = kb.sbuf("bm", [128, NL, 48], F32)
    kb.dma('sp', sc[:], cvec, writes=[sc])
    kb.dma('sp', bm[:], b_modT, writes=[bm])
    kb.op('act', lambda e: e.activation(out=scs[:], in_=sc[:], func=AF.Silu), reads=[sc], writes=[scs])
    wmr = Ring([kb.sbuf("wm%d" % i, [128, KD, 512], F32) for i in range(2)])
    mps = kb.psum("mps", [128, 48, 2])
    for l in range(NL):
        for jt in range(12):
            wm = wmr.next()
            kb.dma('sp', wm[:], fm(w_mod[l], jt * 512, 512), writes=[wm])
            for jj in range(4):
                j = jt * 4 + jj
                for k in range(KD):
                    kb.op('pe', lambda e: e.matmul(mps[:, j, :], lhsT=wm[:, k, jj * 128:(jj + 1) * 128],
                                                   rhs=scs[:, k, :], start=(k == 0), stop=(k == KD - 1)),
                          reads=[wm, scs], writes=[mps])
        kb.op('dve', lambda e: e.tensor_tensor(out=modT[:, l], in0=mps[:],
                                               in1=bm[:, l, :].unsqueeze(2).to_broadcast([128, 48, 2]), op=ALU.add),
              reads=[mps, bm], writes=[modT])
        kb.op('dve', lambda e: e.scalar_tensor_tensor(
            out=A1[:, l], in0=modT[:, l, 8:16, :], scalar=1.0,
            in1=g1s[:, l, :].unsqueeze(2).to_broadcast([128, KD, 2]), op0=ALU.add, op1=ALU.mult),
            reads=[modT, g1s], writes=[A1])
        kb.op('dve', lambda e: e.scalar_tensor_tensor(
            out=A2[:, l], in0=modT[:, l, 32:40, :], scalar=1.0,
            in1=g2s[:, l, :].unsqueeze(2).to_broadcast([128, KD, 2]), op0=ALU.add, op1=ALU.mult),
            reads=[modT, g2s], writes=[A2])
    kb.pop()

    def convert(src2d, dst2d, R, C):
        kb.push()
        sf = Ring([kb.sbuf("cvf%d" % i, [128, 2048], F32) for i in range(3)])
        sb_ = Ring([kb.sbuf("cvb%d" % i, [128, 2048], BF16) for i in range(3)])
        engs = ['pool', 'act', 'dve']
        i = 0
        for r0 in range(0, R, 128):
            for c0 in range(0, C, 2048):
                cw = min(2048, C - c0)
                a = sf.next()
                b = sb_.next()
                kb.dma('sp', a[:, :cw], src2d[r0:r0 + 128, c0:c0 + cw], writes=[a])
                eng = engs[i % 3]
                i += 1
                if eng == 'act':
                    kb.op('act', lambda e: e.copy(out=b[:, :cw], in_=a[:, :cw]), reads=[a], writes=[b])
                else:
                    kb.op(eng, lambda e: e.tensor_copy(out=b[:, :cw], in_=a[:, :cw]), reads=[a], writes=[b])
                kb.dma('sp', dst2d[r0:r0 + 128, c0:c0 + cw], b[:, :cw], reads=[b])
        kb.pop()

    def rsqrt_eps(ob, oap, ib, iap, eps):
        kb.op('dve', lambda e: e.tensor_scalar(out=oap, in0=iap, scalar1=eps, scalar2=None, op0=ALU.add), reads=[ib], writes=[ob])
        kb.op('act', lambda e: e.activation(out=oap, in_=oap, func=AF.Sqrt), reads=[ob], writes=[ob])
        kb.op('dve', lambda e: e.reciprocal(out=oap, in_=oap), reads=[ob], writes=[ob])

    def norm_block(hb, n, Acol, Bcol, outs, sqr, psn, rs, tmpr):
        for k in range(KD):
            sq = sqr.next()
            kb.op('act', lambda e: e.activation(out=sq[:, :n], in_=hb[:, k, :n], func=AF.Square), reads=[hb], writes=[sq])
            kb.op('pe', lambda e: e.matmul(psn[:, :n], lhsT=ones_m[:], rhs=sq[:, :n], start=(k == 0), stop=(k == KD - 1)),
                  reads=[ones_m, sq], writes=[psn])
        rsqrt_eps(rs, rs[:, :n], psn, psn[:, :n], EPS)
        for k in range(KD):
            tmp = tmpr.next()
            kb.op('dve', lambda e: e.scalar_tensor_tensor(out=tmp[:, :n], in0=hb[:, k, :n], scalar=Acol(k),
                                                          in1=rs[:, :n], op0=ALU.mult, op1=ALU.mult),
                  reads=[hb, rs, A1, A2], writes=[tmp])
            for ob in outs:
                kb.op('pool', lambda e: e.tensor_scalar(out=ob[:, k, :n], in0=tmp[:, :n], scalar1=Bcol(k), scalar2=None,
                                                        op0=ALU.add), reads=[tmp, modT], writes=[ob])

    for l in range(NL):
        last = (l == NL - 1)
        lam_init = 0.8 - 0.6 * math.exp(-0.3 * l)
        act_blocks = blocks[:n_lat_blocks] if last else blocks
        act_sblocks = sblocks[:n_lat_sblocks] if last else sblocks

        convert(w_in[l], w_in_b, D, IN_W)
        convert(conv_out_w[l], co_b, D, D)
        convert(w_out[l], wo_b, D, D)
        convert(pool_w[l], pw_b, D, 256)
        convert(peer_wq[l], wq_b, D, 2048)
        convert(keysT[l], keys_b, 128, 2048)
        convert(peer_uL[l], uT_b, N_EXP, D)
        convert(peer_v[l], v_b, N_EXP, D)

        kb.push()
        hb = kb.sbuf("hb", [128, KD, 512], F32)
        xn = kb.sbuf("xn", [128, KD, 512], BF16)
        sqr = Ring([kb.sbuf("sq%d" % i, [128, 512], F32) for i in range(2)])
        tmpr = Ring([kb.sbuf("tmp%d" % i, [128, 512], F32) for i in range(2)])
        rs = kb.sbuf("rs", [128, 512], F32)
        rs2 = kb.sbuf("rs2", [128, 512], F32)
        qn = kb.sbuf("qn", [128, 512], F32)
        t1 = kb.sbuf("t1", [128, 512], F32)
        t2 = kb.sbuf("t2", [128, 512], F32)
        cosb = kb.sbuf("cosb", [128, 512], F32)
        sinb = kb.sbuf("sinb", [128, 512], F32)
        qor = Ring([kb.sbuf("qo%d" % i, [128, 512], BF16) for i in range(2)])
        wr = Ring([kb.sbuf("wA%d" % i, [128, KD, 512], BF16) for i in range(3)])
        vbr = Ring([kb.sbuf("vb%d" % i, [128, 512], BF16) for i in range(2)])
        c1r = Ring([kb.sbuf("c1%d" % i, [128, 512], F32) for i in range(2)])
        psn = kb.psum("psn", [128, 512])
        psr = Ring([kb.psum("psA%d" % i, [128, 512]) for i in range(3)])
        ps2 = kb.psum("ps2", [128, 512])
        ps3 = kb.psum("ps3", [128, 512])
        psc2 = kb.psum("psc2", [128, 512])

        def loadw(col0):
            w = wr.next()
            kb.dma('sp', w[:], fm(w_in_b, col0, 512), writes=[w])
            return w

        for bi, (t0, n) in enumerate(blocks):
            nt = n // 128
            mc = 0 if bi < n_lat_blocks else 1
            kb.dma('sp', hb[:, :, :n], fm(hT, t0, n), writes=[hb])
            kb.dma('sp', cosb[:, :n], cosT[:, t0:t0 + n], writes=[cosb])
            kb.dma('sp', sinb[:, :n], sinT[:, t0:t0 + n], writes=[sinb])
            norm_block(hb, n, lambda k: A1[:, l, k, mc:mc + 1], lambda k: modT[:, l, k, mc:mc + 1], [xn],
                       sqr, psn, rs, tmpr)
            kb.dma('sp', fm(xnT, t0, n), xn[:, :, :n], reads=[xn])
            for (coloff, dst, gcol) in ((Q_OFF, qT, 0), (K_OFF, kT, 1)):
                for wt in range(2):
                    w = loadw(coloff + wt * 512)
                    for hh in range(4):
                        h = wt * 4 + hh
                        ps = psr.next()
                        for k in range(KD):
                            kb.op('pe', lambda e: e.matmul(ps[:, :n], lhsT=w[:, k, hh * 128:(hh + 1) * 128], rhs=xn[:, k, :n],
                                                           start=(k == 0), stop=(k == KD - 1)), reads=[w, xn], writes=[ps])
                        sq = sqr.next()
                        kb.op('act', lambda e: e.activation(out=sq[:, :n], in_=ps[:, :n], func=AF.Square), reads=[ps], writes=[sq])
                        kb.op('pe', lambda e: e.matmul(ps2[:, :n], lhsT=bd_s[:], rhs=sq[:, :n], start=True, stop=True),
                              reads=[bd_s, sq], writes=[ps2])
                        rsqrt_eps(rs2, rs2[:, :n], ps2, ps2[:, :n], EPS)
                        kb.op('dve', lambda e: e.scalar_tensor_tensor(out=qn[:, :n], in0=ps[:, :n], scalar=qkg[:, l, gcol:gcol + 1],
                                                                      in1=rs2[:, :n], op0=ALU.mult, op1=ALU.mult),
                              reads=[ps, rs2, qkg], writes=[qn])
                        kb.op('pe', lambda e: e.matmul(ps3[:, :n], lhsT=rot_s[:], rhs=qn[:, :n], start=True, stop=True),
                              reads=[rot_s, qn], writes=[ps3])
                        kb.op('pool', lambda e: e.tensor_tensor(out=t1[:, :n], in0=qn[:, :n], in1=cosb[:, :n], op=ALU.mult),
                              reads=[qn, cosb], writes=[t1])
                        kb.op('dve', lambda e: e.tensor_tensor(out=t2[:, :n], in0=ps3[:, :n], in1=sinb[:, :n], op=ALU.mult),
                              reads=[ps3, sinb], writes=[t2])
                        qo = qor.next()
                        kb.op('pool', lambda e: e.tensor_tensor(out=qo[:, :n], in0=t1[:, :n], in1=t2[:, :n], op=ALU.add),
                              reads=[t1, t2], writes=[qo])
                        kb.dma('sp', dst[h * 128:(h + 1) * 128, t0:t0 + n], qo[:, :n], reads=[qo])
            for (coloff, dstap, dcol) in ((V_OFF, Vt, 0), (POOL_OFF, uh, 0)):
                for wt in range(2):
                    w = loadw(coloff + wt * 512)
                    for ti in range(nt):
                        ps = psr.next()
                        for k in range(KD):
                            kb.op('pe', lambda e: e.matmul(ps[:], lhsT=xn[:, k, ti * 128:(ti + 1) * 128], rhs=w[:, k, :],
                                                           start=(k == 0), stop=(k == KD - 1)), reads=[w, xn], writes=[ps])
                        vb = vbr.next()
                        kb.op('act', lambda e: e.copy(out=vb[:], in_=ps[:]), reads=[ps], writes=[vb])
                        r0 = t0 + ti * 128
                        kb.dma('sp', dstap[r0:r0 + 128, dcol + wt * 512: dcol + (wt + 1) * 512], vb[:], reads=[vb])
            for wt in range(2):
                w1 = loadw(CIN_OFF + wt * 512)
                w2 = loadw(CC_OFF + wt * 512)
                for ti in range(nt):
                    ps = psr.next()
                    for k in range(KD):
                        kb.op('pe', lambda e: e.matmul(ps[:], lhsT=xn[:, k, ti * 128:(ti + 1) * 128], rhs=w1[:, k, :],
                                                       start=(k == 0), stop=(k == KD - 1)), reads=[w1, xn], writes=[ps])
                    for k in range(KD):
                        kb.op('pe', lambda e: e.matmul(psc2[:], lhsT=xn[:, k, ti * 128:(ti + 1) * 128], rhs=w2[:, k, :],
                                                       start=(k == 0), stop=(k == KD - 1)), reads=[w2, xn], writes=[psc2])
                    c1 = c1r.next()
                    kb.op('act', lambda e: e.copy(out=c1[:], in_=ps[:]), reads=[ps], writes=[c1])
                    vb = vbr.next()
                    kb.op('dve', lambda e: e.tensor_tensor(out=vb[:], in0=psc2[:], in1=c1[:], op=ALU.mult),
                          reads=[psc2, c1], writes=[vb])
                    r0 = t0 + ti * 128
                    kb.dma('sp', uh[r0:r0 + 128, D + wt * 512: D + (wt + 1) * 512], vb[:], reads=[vb])
        kb.pop("A%d" % l)

        kb.push()
        lvb = kb.sbuf("lvb", [128, 256], F32)
        lprod = kb.sbuf("lprod", [128, 128], F32)
        lsum = kb.sbuf("lsum", [128, 2], F32)
        lexp = kb.sbuf("lexp", [128, 2], F32)
        nlam = kb.sbuf("nlam", [128, 1], F32)
        gvT = kb.sbuf("gvT", [128, 1], F32)
        ones1 = kb.sbuf("ones1", [128, 128], F32)
        kb.op('pool', lambda e: e.memset(ones1[:], 1.0), writes=[ones1])
        kb.dma('sp', lvb[:], lam_v[l:l + 1, :].partition_broadcast(128), writes=[lvb])
        kb.dma('sp', gvT[:], attn_g[l:l + 1, :].rearrange("o e -> e o"), writes=[gvT])
        lv4 = lvb[:].rearrange("p (a d) -> p a d", a=4)
        kb.op('dve', lambda e: e.tensor_tensor(out=lprod[:, 0:64], in0=lv4[:, 0, :], in1=lv4[:, 1, :], op=ALU.mult),
              reads=[lvb], writes=[lprod])
        kb.op('dve', lambda e: e.tensor_tensor(out=lprod[:, 64:128], in0=lv4[:, 2, :], in1=lv4[:, 3, :], op=ALU.mult),
              reads=[lvb, lprod], writes=[lprod])
        kb.op('dve', lambda e: e.tensor_reduce(out=lsum[:], in_=lprod[:].rearrange("p (a d) -> p a d", a=2), axis=AX.X, op=ALU.add),
              reads=[lprod], writes=[lsum])
        kb.op('act', lambda e: e.activation(out=lexp[:], in_=lsum[:], func=AF.Exp), reads=[lsum], writes=[lexp])
        kb.op('dve', lambda e: e.tensor_tensor(out=nlam[:], in0=lexp[:, 1:2], in1=lexp[:, 0:1], op=ALU.subtract),
              reads=[lexp], writes=[nlam])
        kb.op('dve', lambda e: e.tensor_scalar(out=nlam[:], in0=nlam[:], scalar1=-lam_init, scalar2=None, op0=ALU.add),
              reads=[nlam], writes=[nlam])
        kb.op('dve', lambda e: e.tensor_scalar(out=gvT[:], in0=gvT[:], scalar1=math.sqrt(128.0) * (1.0 - lam_init),
                                               scalar2=None, op0=ALU.mult), reads=[gvT], writes=[gvT])
        kTr = Ring([kb.sbuf("kTh%d" % i, [128, T], BF16) for i in range(2)])
        qTr = Ring([kb.sbuf("qTh%d" % i, [128, T], BF16) for i in range(2)])
        var = Ring([kb.sbuf("vah%d" % i, [128, NT, 128], BF16) for i in range(2)])
        ptr = Ring([kb.sbuf("pt%d" % i, [128, 1024], BF16) for i in range(3)])
        zacc = kb.sbuf("zacc", [128, 1024], F32)
        rzb = kb.sbuf("rzb", [128, 1024], F32)
        osb = [kb.sbuf("osb%d" % i, [128, 1024], F32) for i in range(2)]
        oc = kb.sbuf("oc", [128, 1024], F32)
        osq = kb.sbuf("osq", [128, 1024], F32)
        rsd = kb.sbuf("rsd", [128, 1024], F32)
        aor = Ring([kb.sbuf("ao%d" % i, [128, 1024], BF16) for i in range(2)])
        psS = Ring([kb.psum("pS%d" % i, [128, 2, 512]) for i in range(2)])
        psO = kb.psum("pO", [128, 2, 512])
        psZ = Ring([kb.psum("pZ%d" % i, [128, 512]) for i in range(2)])
        qgroups = []
        for g0 in range(0, S, 1024):
            qgroups.append([(g0 + j, 512) for j in range(0, min(1024, S - g0), 512)])
        if not last:
            qgroups.append([(S, CT)])
        for h in range(8):
            kh = kTr.next()
            qh = qTr.next()
            va = var.next()
            kb.dma('sp', kh[:], kT[h * 128:(h + 1) * 128, :], writes=[kh])
            kb.dma('sp', qh[:], qT[h * 128:(h + 1) * 128, :], writes=[qh])
            for c0 in range(0, NT, 16):
                cn = min(16, NT - c0)
                kb.dma('sp', va[:, c0:c0 + cn, :],
                       Vt[c0 * 128:(c0 + cn) * 128, h * 128:(h + 1) * 128].rearrange("(c p) e -> p c e", p=128), writes=[va])
            for subs_ in qgroups:
                nb = len(subs_)
                is_ctx = subs_[0][0] >= S
                kcs = list(range(S // 128, NT)) if is_ctx else list(range(NT))
                ntot = 512 * (nb - 1) + subs_[-1][1]
                for s in range(2):
                    for ci, kc in enumerate(kcs):
                        ps = psS.next()
                        for b_, (tq, n) in enumerate(subs_):
                            kb.op('pe', lambda e: e.matmul(ps[:, b_, :n], lhsT=kh[s * 64:(s + 1) * 64, kc * 128:(kc + 1) * 128],
                                                           rhs=qh[s * 64:(s + 1) * 64, tq:tq + n], start=True, stop=True),
                                  reads=[kh, qh], writes=[ps])
                        pt = ptr.next()
                        if nb == 2:
                            kb.op('act', lambda e: e.activation(out=pt[:].rearrange("p (b x) -> p b x", b=2), in_=ps[:], func=AF.Exp, scale=0.125),
                                  reads=[ps], writes=[pt])
                        else:
                            n = subs_[0][1]
                            kb.op('act', lambda e: e.activation(out=pt[:, :n], in_=ps[:, 0, :n], func=AF.Exp, scale=0.125),
                                  reads=[ps], writes=[pt])
                        for b_, (tq, n) in enumerate(subs_):
                            kb.op('pe', lambda e: e.matmul(psO[:, b_, :n], lhsT=va[:, kc, :], rhs=pt[:, b_ * 512:b_ * 512 + n],
                                                           start=(ci == 0), stop=(ci == len(kcs) - 1)), reads=[pt, va], writes=[psO])
                        if ci == 0:
                            kb.op('dve', lambda e: e.tensor_copy(out=zacc[:, :ntot], in_=pt[:, :ntot]), reads=[pt], writes=[zacc])
                        else:
                            kb.op('dve', lambda e: e.tensor_tensor(out=zacc[:, :ntot], in0=zacc[:, :ntot], in1=pt[:, :ntot], op=ALU.add),
                                  reads=[pt, zacc], writes=[zacc])
                    for b_, (tq, n) in enumerate(subs_):
                        sl = slice(b_ * 512, b_ * 512 + n)
                        pz = psZ.next()
                        kb.op('pe', lambda e: e.matmul(pz[:, :n], lhsT=ones1[:], rhs=zacc[:, sl], start=True, stop=True),
                              reads=[ones1, zacc], writes=[pz])
                        kb.op('dve', lambda e: e.reciprocal(out=rzb[:, sl], in_=pz[:, :n]), reads=[pz], writes=[rzb])
                        kb.op('dve', lambda e: e.tensor_tensor(out=osb[s][:, sl], in0=psO[:, b_, :n], in1=rzb[:, sl], op=ALU.mult),
                              reads=[psO, rzb], writes=[osb[s]])
                ao = aor.next()
                for b_, (tq, n) in enumerate(subs_):
                    sl = slice(b_ * 512, b_ * 512 + n)
                    kb.op('dve', lambda e: e.scalar_tensor_tensor(out=oc[:, sl], in0=osb[1][:, sl], scalar=nlam[:, 0:1], in1=osb[0][:, sl],
                                                                  op0=ALU.mult, op1=ALU.add), reads=[osb[0], osb[1], nlam], writes=[oc])
                    kb.op('act', lambda e: e.activation(out=osq[:, sl], in_=oc[:, sl], func=AF.Square), reads=[oc], writes=[osq])
                    pz = psZ.next()
                    kb.op('pe', lambda e: e.matmul(pz[:, :n], lhsT=ones1[:], rhs=osq[:, sl], start=True, stop=True),
                          reads=[ones1, osq], writes=[pz])
                    rsqrt_eps(rsd, rsd[:, sl], pz, pz[:, :n], 128.0 * EPS)
                    kb.op('dve', lambda e: e.scalar_tensor_tensor(out=ao[:, sl], in0=oc[:, sl], scalar=gvT[:, 0:1], in1=rsd[:, sl],
                                                                  op0=ALU.mult, op1=ALU.mult), reads=[oc, gvT, rsd], writes=[ao])
                    kb.dma('sp', attnT[h * 128:(h + 1) * 128, tq:tq + n], ao[:, sl], reads=[ao])
        kb.pop("B1_%d" % l)

        kb.push()
        cow = kb.sbuf("cow", [128, KD, D], BF16)
        wow = kb.sbuf("wow", [128, KD, D], BF16)
        pww = kb.sbuf("pww", [128, KD, 256], BF16)
        kb.dma('sp', cow[:], fm(co_b, 0, D), writes=[cow])
        kb.dma('sp', wow[:], fm(wo_b, 0, D), writes=[wow])
        kb.dma('sp', pww[:], fm(pw_b, 0, 256), writes=[pww])
        xn = kb.sbuf("xnB", [128, KD, SB], BF16)
        uht = kb.sbuf("uht", [128, 4, 2 * D], BF16)
        att = kb.sbuf("att", [128, KD, SB], BF16)
        cbT = kb.sbuf("cbT", [128, KD, SB], BF16)
        plT = kb.sbuf("plT", [128, KD, SB], BF16)
        bcT = kb.sbuf("bcT", [128, KD, SB], BF16)
        mgT = kb.sbuf("mgT", [128, KD, SB], BF16)
        wcr = Ring([kb.sbuf("wcb%d" % i, [128, KD, 512], BF16) for i in range(2)])
        gwr = Ring([kb.sbuf("gw%d" % i, [128, KD, 3, 128], BF16) for i in range(2)])
        gTr = Ring([kb.sbuf("gT%d" % i, [128, 3, SB], F32) for i in range(2)])
        hcr = Ring([kb.sbuf("hcb%d" % i, [128, SB], F32) for i in range(2)])
        hnr = Ring([kb.sbuf("hnb%d" % i, [128, SB], F32) for i in range(2)])
        m0 = kb.sbuf("m0", [128, SB], F32)
        m1 = kb.sbuf("m1", [128, SB], F32)
        m2 = kb.sbuf("m2", [128, SB], F32)
        cacc = Ring([kb.sbuf("cacc%d" % i, [128, 128], F32) for i in range(2)])
        psr = Ring([kb.psum("psB%d" % i, [128, 512]) for i in range(4)])
        pcv = Ring([kb.psum("pcv%d" % i, [128, 512]) for i in range(2)])
        for bi, (t0, n) in enumerate(act_sblocks):
            nt = n // 128
            tile0 = t0 // 128
            is_ctx = bi >= n_lat_sblocks
            mc = 1 if is_ctx else 0
            seq_first = (tile0 == 0) or (tile0 == S // 128)
            seq_last_tile = (S // 128 - 1) if not is_ctx else (NT - 1)
            kb.dma('sp', xn[:, :, :n], fm(xnT, t0, n), writes=[xn])
            kb.dma('sp', att[:, :, :n], fm(attnT, t0, n), writes=[att])
            lo_t = tile0 if seq_first else tile0 - 1
            hi_t = min(tile0 + nt, seq_last_tile)
            kb.dma('sp', uht[:, lo_t - (tile0 - 1): hi_t - (tile0 - 1) + 1, :],
                   uh[lo_t * 128:(hi_t + 1) * 128, :].rearrange("(q p) e -> p q e", p=128), writes=[uht])
            for wt in range(2):
                w = wcr.next()
                kb.dma('sp', w[:], fm(w_in_b, CB_OFF + wt * 512, 512), writes=[w])
                for cc in range(4):
                    ps = psr.next()
                    for k in range(KD):
                        kb.op('pe', lambda e: e.matmul(ps[:, :n], lhsT=w[:, k, cc * 128:(cc + 1) * 128], rhs=xn[:, k, :n],
                                                       start=(k == 0), stop=(k == KD - 1)), reads=[w, xn], writes=[ps])
                    kb.op('act', lambda e: e.copy(out=cbT[:, wt * 4 + cc, :n], in_=ps[:, :n]), reads=[ps], writes=[cbT])
            for c in range(KD):
                wi = c // 2
                ps = psr.next()
                for ti in range(nt):
                    tg = tile0 + ti
                    first = (tg == 0) or (tg == S // 128)
                    lastt = (tg == seq_last_tile)
                    terms = []
                    if not first:
                        terms.append((ti, wi * 5 + 0))
                    terms.append((ti + 1, wi * 5 + (3 if first else (4 if lastt else 1))))
                    if not lastt:
                        terms.append((ti + 2, wi * 5 + 2))
                    for j, (slot, kind) in enumerate(terms):
                        kb.op('pe', lambda e: e.matmul(ps[:, ti * 128:(ti + 1) * 128], lhsT=uht[:, slot, c * 128:(c + 1) * 128],
                                                       rhs=band[:, kind, :], start=(j == 0), stop=(j == len(terms) - 1)),
                              reads=[uht, band], writes=[ps])
                kb.op('act', lambda e: e.copy(out=plT[:, c, :n], in_=ps[:, :n]), reads=[ps], writes=[plT])
            for c in range(KD):
                for ti in range(nt):
                    tg = tile0 + ti
                    first = (tg == 0) or (tg == S // 128)
                    lastt = (tg == seq_last_tile)
                    terms = [(ti + 1, 1)]
                    if not first:
                        terms.append((ti, 0))
                    if not lastt:
                        terms.append((ti + 2, 2))
                    pc = pcv.next()
                    for j, (slot, kind) in enumerate(terms):
                        kb.op('pe', lambda e: e.matmul(pc[:, 0:384], lhsT=uht[:, slot, D + c * 128: D + (c + 1) * 128],
                                                       rhs=scat[:, kind, :], start=(j == 0), stop=(j == len(terms) - 1)),
                              reads=[uht, scat], writes=[pc])
                    ca = cacc.next()
                    kb.op('dve', lambda e: e.tensor_scalar(out=ca[:], in0=pc[:, 0:128], scalar1=cws[:, l, c, 0:1], scalar2=None,
                                                           op0=ALU.mult), reads=[pc, cws], writes=[ca])
                    kb.op('dve', lambda e: e.scalar_tensor_tensor(out=ca[:], in0=pc[:, 128:256], scalar=cws[:, l, c, 1:2], in1=ca[:],
                                                                  op0=ALU.mult, op1=ALU.add), reads=[pc, cws, ca], writes=[ca])
                    kb.op('dve', lambda e: e.scalar_tensor_tensor(out=ca[:], in0=pc[:, 256:384], scalar=cws[:, l, c, 2:3], in1=ca[:],
                                                                  op0=ALU.mult, op1=ALU.add), reads=[pc, cws, ca], writes=[ca])
                    kb.op('pool', lambda e: e.tensor_tensor(out=bcT[:, c, ti * 128:(ti + 1) * 128], in0=ca[:],
                                                            in1=cbT[:, c, ti * 128:(ti + 1) * 128], op=ALU.mult),
                          reads=[ca, cbT], writes=[bcT])
            for dc in range(KD):
                gw = gwr.next()
                for j in range(3):
                    kb.dma('sp', gw[:, :, j, :], fm(w_in_b, GATE_OFF + j * D + dc * 128, 128), writes=[gw])
                gT = gTr.next()
                for j in range(3):
                    ps = psr.next()
                    for k in range(KD):
                        kb.op('pe', lambda e: e.matmul(ps[:, :n], lhsT=gw[:, k, j, :], rhs=xn[:, k, :n],
                                                       start=(k == 0), stop=(k == KD - 1)), reads=[gw, xn], writes=[ps])
                    kb.op('act', lambda e: e.activation(out=gT[:, j, :n], in_=ps[:, :n], func=AF.Sigmoid), reads=[ps], writes=[gT])
                kb.op('dve', lambda e: e.tensor_tensor(out=m0[:, :n], in0=att[:, dc, :n], in1=gT[:, 0, :n], op=ALU.mult),
                      reads=[att, gT], writes=[m0])
                ps = psr.next()
                g = dc // 2
                dh = dc % 2
                for kk in range(2):
                    kb.op('pe', lambda e: e.matmul(ps[:, :n], lhsT=pww[:, g * 2 + kk, dh * 128:(dh + 1) * 128], rhs=plT[:, g * 2 + kk, :n],
                                                   start=(kk == 0), stop=(kk == 1)), reads=[pww, plT], writes=[ps])
                kb.op('dve', lambda e: e.scalar_tensor_tensor(out=m1[:, :n], in0=ps[:, :n], scalar=psc[:, l, dc:dc + 1], in1=gT[:, 1, :n],
                                                              op0=ALU.mult, op1=ALU.mult), reads=[ps, psc, gT], writes=[m1])
                ps = psr.next()
                for k in range(KD):
                    kb.op('pe', lambda e: e.matmul(ps[:, :n], lhsT=cow[:, k, dc * 128:(dc + 1) * 128], rhs=bcT[:, k, :n],
                                                   start=(k == 0), stop=(k == KD - 1)), reads=[cow, bcT], writes=[ps])
                kb.op('dve', lambda e: e.tensor_tensor(out=m2[:, :n], in0=ps[:, :n], in1=gT[:, 2, :n], op=ALU.mult),
                      reads=[ps, gT], writes=[m2])
                kb.op('pool', lambda e: e.tensor_tensor(out=m0[:, :n], in0=m0[:, :n], in1=m1[:, :n], op=ALU.add),
                      reads=[m0, m1], writes=[m0])
                kb.op('pool', lambda e: e.tensor_tensor(out=mgT[:, dc, :n], in0=m0[:, :n], in1=m2[:, :n], op=ALU.add),
                      reads=[m0, m2], writes=[mgT])
            for dc in range(KD):
                ps = psr.next()
                for k in range(KD):
                    kb.op('pe', lambda e: e.matmul(ps[:, :n], lhsT=wow[:, k, dc * 128:(dc + 1) * 128], rhs=mgT[:, k, :n],
                                                   start=(k == 0), stop=(k == KD - 1)), reads=[wow, mgT], writes=[ps])
                hc_ = hcr.next()
                kb.dma('sp', hc_[:, :n], hT[dc * 128:(dc + 1) * 128, t0:t0 + n], writes=[hc_])
                hn = hnr.next()
                kb.op('dve', lambda e: e.scalar_tensor_tensor(out=hn[:, :n], in0=ps[:, :n], scalar=modT[:, l, 16 + dc, mc:mc + 1],
                                                              in1=hc_[:, :n], op0=ALU.mult, op1=ALU.add),
                      reads=[ps, modT, hc_], writes=[hn])
                kb.dma('sp', hT[dc * 128:(dc + 1) * 128, t0:t0 + n], hn[:, :n], reads=[hn])
        kb.pop("B2_%d" % l)

        kb.push()
        kys = kb.sbuf("kys", [128, 16, 128], BF16)
        kb.dma('sp', kys[:], keys_b.rearrange("p (g k) -> p g k", g=16), writes=[kys])
        hb = kb.sbuf("hbC", [128, KD, SB], F32)
        xb = kb.sbuf("xbC", [128, KD, SB], BF16)
        sqr = Ring([kb.sbuf("sqC%d" % i, [128, SB], F32) for i in range(2)])
        tmpr = Ring([kb.sbuf("tmpC%d" % i, [128, SB], F32) for i in range(2)])
        rs = kb.sbuf("rsC", [128, SB], F32)
        wqr = Ring([kb.sbuf("wq%d" % i, [128, KD, 512], BF16) for i in range(2)])
        qp = kb.sbuf("qp", [128, 16, SB], BF16)
        bufA = kb.sbuf("bufA", [128, 2048], F32)
        bufB = kb.sbuf("bufB", [128, 2048], F32)
        stop_ = kb.sbuf("stop", [128, 16, 16], F32)
        itop = kb.sbuf("itop", [128, 16, 16], U32)
        itf = kb.sbuf("itf", [128, 16, 16], F32)
        tops = kb.sbuf("tops", [128, 8, 16], F32)
        pos = kb.sbuf("pos", [128, 8, 16], U32)
        pa = kb.sbuf("pa", [128, 8, 16], U32)
        pbb = kb.sbuf("pbb", [128, 8, 16], U32)
        paf = kb.sbuf("paf", [128, 8, 16], F32)
        pbf = kb.sbuf("pbf", [128, 8, 16], F32)
        sel3 = kb.sbuf("sel3", [128, 3, 128], F32)
        ex = kb.sbuf("ex", [128, 8, 16], F32)
        esum = kb.sbuf("esum", [128, 8], F32)
        itT = kb.sbuf("itT", [128, 3, SB], F32)
        ohA = kb.sbuf("ohA", [128, 16, 128], BF16)
        Ar = Ring([kb.sbuf("Aoh%d" % i, [128, 16, 128], BF16) for i in range(2)])
        Br = Ring([kb.sbuf("Boh%d" % i, [128, 16, 128], BF16) for i in range(2)])
        GT = kb.sbuf("GT", [128, 128, SB], BF16)
        ucr = Ring([kb.sbuf("uc%d" % i, [128, 2, D], BF16) for i in range(2)])
        vcr = Ring([kb.sbuf("vc%d" % i, [128, 2, D], BF16) for i in range(2)])
        x2r = Ring([kb.sbuf("x2_%d" % i, [128, SB], F32) for i in range(2)])
        ttr = Ring([kb.sbuf("tt_%d" % i, [128, SB], F32) for i in range(2)])
        sgr = Ring([kb.sbuf("sg_%d" % i, [128, SB], F32) for i in range(2)])
        xgr = Ring([kb.sbuf("xg_%d" % i, [128, SB], F32) for i in range(2)])
        Wr = Ring([kb.sbuf("W_%d" % i, [128, SB], BF16) for i in range(3)])
        hnr = Ring([kb.sbuf("hnC%d" % i, [128, SB], F32) for i in range(2)])
        psn = kb.psum("psnC", [128, 512])
        psq = Ring([kb.psum("psq%d" % i, [128, 512]) for i in range(2)])
        big4 = kb.psum("big4", [128, 4, 512])
        sS = bufA[:].rearrange("p (g k) -> p g k", g=16)
        sS2 = bufB[:].rearrange("p (g k) -> p g k", g=16)
        cand = bufA[:].rearrange("p (h c) -> p h c", h=8)
        cand2 = bufB[:].rearrange("p (h c) -> p h c", h=8)
        oh = bufB[:].rearrange("p (h k a) -> p h k a", h=8, k=16)
        GC = 1.5957691216057308
        for bi, (t0, n) in enumerate(act_sblocks):
            nt = n // 128
            mc = 1 if bi >= n_lat_sblocks else 0
            kb.dma('sp', hb[:, :, :n], fm(hT, t0, n), writes=[hb])
            norm_block(hb, n, lambda k: A2[:, l, k, mc:mc + 1], lambda k: modT[:, l, 24 + k, mc:mc + 1], [xb],
                       sqr, psn, rs, tmpr)
            for wt in range(4):
                wq = wqr.next()
                kb.dma('sp', wq[:], fm(wq_b, wt * 512, 512), writes=[wq])
                for gg in range(4):
                    g = wt * 4 + gg
                    ps = psq.next()
                    for k in range(KD):
                        kb.op('pe', lambda e: e.matmul(ps[:, :n], lhsT=wq[:, k, gg * 128:(gg + 1) * 128], rhs=xb[:, k, :n],
                                                       start=(k == 0), stop=(k == KD - 1)), reads=[wq, xb], writes=[ps])
                    kb.op('act', lambda e: e.copy(out=qp[:, g, :n], in_=ps[:, :n]), reads=[ps], writes=[qp])
            for ti in range(nt):
                tsl = slice(ti * 128, (ti + 1) * 128)
                for g in range(16):
                    kb.op('pe', lambda e: e.matmul(big4[:, g // 4, (g % 4) * 128:(g % 4 + 1) * 128], lhsT=qp[:, g, tsl], rhs=kys[:, g, :],
                                                   start=True, stop=True), reads=[qp, kys], writes=[big4])
                kb.op('act', lambda e: e.copy(out=bufA[:].rearrange("p (a x) -> p a x", a=4), in_=big4[:]), reads=[big4], writes=[bufA])
                for g in range(16):
                    kb.op('dve', lambda e: e.max(out=stop_[:, g, 0:8], in_=sS[:, g, :]), reads=[bufA], writes=[stop_])
                    kb.op('dve', lambda e: e.max_index(out=itop[:, g, 0:8], in_max=stop_[:, g, 0:8], in_values=sS[:, g, :]),
                          reads=[bufA, stop_], writes=[itop])
                    kb.op('dve', lambda e: e.match_replace(out=sS2[:, g, :], in_to_replace=stop_[:, g, 0:8], in_values=sS[:, g, :],
                                                           imm_value=-1e30), reads=[bufA, stop_], writes=[bufB])
                    kb.op('dve', lambda e: e.max(out=stop_[:, g, 8:16], in_=sS2[:, g, :]), reads=[bufB], writes=[stop_])
                    kb.op('dve', lambda e: e.max_index(out=itop[:, g, 8:16], in_max=stop_[:, g, 8:16], in_values=sS2[:, g, :]),
                          reads=[bufB, stop_], writes=[itop])
                kb.op('dve', lambda e: e.tensor_copy(out=itf[:], in_=itop[:]), reads=[itop], writes=[itf])
                s4 = stop_[:].rearrange("p (h i) a -> p h i a", i=2)
                kb.op('dve', lambda e: e.tensor_tensor(
                    out=cand.rearrange("p h (a b) -> p h a b", a=16),
                    in0=s4[:, :, 0, :].unsqueeze(3).to_broadcast([128, 8, 16, 16]),
                    in1=s4[:, :, 1, :].unsqueeze(2).to_broadcast([128, 8, 16, 16]), op=ALU.add), reads=[stop_], writes=[bufA])
                for h in range(8):
                    kb.op('dve', lambda e: e.max(out=tops[:, h, 0:8], in_=cand[:, h, :]), reads=[bufA], writes=[tops])
                    kb.op('dve', lambda e: e.max_index(out=pos[:, h, 0:8], in_max=tops[:, h, 0:8], in_values=cand[:, h, :]),
                          reads=[bufA, tops], writes=[pos])
                    kb.op('dve', lambda e: e.match_replace(out=cand2[:, h, :], in_to_replace=tops[:, h, 0:8], in_values=cand[:, h, :],
                                                           imm_value=-1e30), reads=[bufA, tops], writes=[bufB])
                    kb.op('dve', lambda e: e.max(out=tops[:, h, 8:16], in_=cand2[:, h, :]), reads=[bufB], writes=[tops])
                    kb.op('dve', lambda e: e.max_index(out=pos[:, h, 8:16], in_max=tops[:, h, 8:16], in_values=cand2[:, h, :]),
                          reads=[bufB, tops], writes=[pos])
                kb.op('dve', lambda e: e.tensor_single_scalar(out=pa[:], in_=pos[:], scalar=4, op=ALU.logical_shift_right),
                      reads=[pos], writes=[pa])
                kb.op('dve', lambda e: e.tensor_single_scalar(out=pbb[:], in_=pos[:], scalar=15, op=ALU.bitwise_and),
                      reads=[pos], writes=[pbb])
                kb.op('dve', lambda e: e.tensor_copy(out=paf[:], in_=pa[:]), reads=[pa], writes=[paf])
                kb.op('dve', lambda e: e.tensor_copy(out=pbf[:], in_=pbb[:]), reads=[pbb], writes=[pbf])
                i4 = itf[:].rearrange("p (h i) a -> p h i a", i=2)
                for (pf, ii) in ((paf, 0), (pbf, 1)):
                    kb.op('dve', lambda e: e.tensor_tensor(
                        out=oh, in0=pf[:].unsqueeze(3).to_broadcast([128, 8, 16, 16]),
                        in1=iota16[:].unsqueeze(1).unsqueeze(1).to_broadcast([128, 8, 16, 16]), op=ALU.is_equal),
                        reads=[pf, iota16], writes=[bufB])
                    kb.op('dve', lambda e: e.tensor_tensor(
                        out=oh, in0=oh, in1=i4[:, :, ii, :].unsqueeze(2).to_broadcast([128, 8, 16, 16]), op=ALU.mult),
                        reads=[bufB, itf], writes=[bufB])
                    kb.op('dve', lambda e: e.tensor_reduce(out=sel3[:, ii, :].rearrange("p (h k) -> p h k", h=8), in_=oh, axis=AX.X, op=ALU.add),
                          reads=[bufB], writes=[sel3])
                kb.op('dve', lambda e: e.tensor_tensor(out=ex[:], in0=tops[:], in1=tops[:, :, 0:1].to_broadcast([128, 8, 16]),
                                                       op=ALU.subtract), reads=[tops], writes=[ex])
                kb.op('act', lambda e: e.activation(out=ex[:], in_=ex[:], func=AF.Exp), reads=[ex], writes=[ex])
                kb.op('dve', lambda e: e.tensor_reduce(out=esum[:], in_=ex[:], axis=AX.X, op=ALU.add), reads=[ex], writes=[esum])
                kb.op('dve', lambda e: e.reciprocal(out=esum[:], in_=esum[:]), reads=[esum], writes=[esum])
                kb.op('dve', lambda e: e.tensor_tensor(out=sel3[:, 2, :].rearrange("p (h k) -> p h k", h=8), in0=ex[:],
                                                       in1=esum[:].unsqueeze(2).to_broadcast([128, 8, 16]), op=ALU.mult),
                      reads=[ex, esum], writes=[sel3])
                for x3 in range(3):
                    kb.op('pe', lambda e: e.transpose(psn[:, x3 * 128:(x3 + 1) * 128], sel3[:, x3, :], idf[:]), reads=[sel3, idf], writes=[psn])
                kb.op('act', lambda e: e.copy(out=itT[:, :, tsl], in_=psn[:, 0:384].rearrange("p (x t) -> p x t", x=3)), reads=[psn], writes=[itT])
            for tg in range(0, n, 16):
                A_ = Ar.next()
                B_ = Br.next()
                io_b = iota128[:].unsqueeze(1).to_broadcast([128, 16, 128])
                kb.op('dve', lambda e: e.tensor_tensor(out=ohA[:], in0=io_b, in1=itT[:, 0, tg:tg + 16].unsqueeze(2).to_broadcast([128, 16, 128]),
                                                       op=ALU.is_equal), reads=[iota128, itT], writes=[ohA])
                kb.op('dve', lambda e: e.tensor_tensor(out=A_[:], in0=ohA[:], in1=itT[:, 2, tg:tg + 16].unsqueeze(2).to_broadcast([128, 16, 128]),
                                                       op=ALU.mult), reads=[ohA, itT], writes=[A_])
                kb.op('dve', lambda e: e.tensor_tensor(out=B_[:], in0=io_b, in1=itT[:, 1, tg:tg + 16].unsqueeze(2).to_broadcast([128, 16, 128]),
                                                       op=ALU.is_equal), reads=[iota128, itT], writes=[B_])
                for t4 in range(4):
                    pg = psq.next()
                    for tt in range(4):
                        t_ = t4 * 4 + tt
                        kb.op('pe', lambda e: e.matmul(pg[:, tt * 128:(tt + 1) * 128], lhsT=B_[:, t_, :], rhs=A_[:, t_, :], start=True, stop=True),
                              reads=[A_, B_], writes=[pg])
                    tok0 = tg + t4 * 4
                    kb.op('act', lambda e: e.copy(out=GT[:, :, tok0:tok0 + 4].rearrange("p i t -> p t i"),
                                                  in_=pg[:].rearrange("p (t i) -> p t i", t=4)), reads=[pg], writes=[GT])
            for ig in range(0, 128, 2):
                uc = ucr.next()
                vc = vcr.next()
                kb.dma('sp', uc[:], uT_b[ig * 128:(ig + 2) * 128, :].rearrange("(c p) x -> p c x", p=128), writes=[uc])
                kb.dma('sp', vc[:], v_b[ig * 128:(ig + 2) * 128, :].rearrange("(c p) x -> p c x", p=128), writes=[vc])
                for c in range(2):
                    i = ig + c
                    ph = psq.next()
                    for k in range(KD):
                        kb.op('pe', lambda e: e.matmul(ph[:, :n], lhsT=uc[:, c, k * 128:(k + 1) * 128], rhs=xb[:, k, :n],
                                                       start=(k == 0), stop=(k == KD - 1)), reads=[uc, xb], writes=[ph])
                    x2 = x2r.next()
                    tt_ = ttr.next()
                    sg = sgr.next()
                    xg = xgr.next()
                    W_ = Wr.next()
                    kb.op('act', lambda e: e.activation(out=x2[:, :n], in_=ph[:, :n], func=AF.Square), reads=[ph], writes=[x2])
                    kb.op('dve', lambda e: e.tensor_scalar(out=tt_[:, :n], in0=x2[:, :n], scalar1=0.044715, scalar2=1.0,
                                                           op0=ALU.mult, op1=ALU.add), reads=[x2], writes=[tt_])
                    kb.op('dve', lambda e: e.tensor_tensor(out=tt_[:, :n], in0=ph[:, :n], in1=tt_[:, :n], op=ALU.mult),
                          reads=[ph, tt_], writes=[tt_])
                    kb.op('act', lambda e: e.activation(out=sg[:, :n], in_=tt_[:, :n], func=AF.Sigmoid, scale=GC), reads=[tt_], writes=[sg])
                    kb.op('dve', lambda e: e.tensor_tensor(out=xg[:, :n], in0=ph[:, :n], in1=GT[:, i, :n], op=ALU.mult),
                          reads=[ph, GT], writes=[xg])
                    kb.op('pool', lambda e: e.tensor_tensor(out=W_[:, :n], in0=xg[:, :n], in1=sg[:, :n], op=ALU.mult),
                          reads=[xg, sg], writes=[W_])
                    for dc in range(KD):
                        kb.op('pe', lambda e: e.matmul(big4[:, dc // 2, (dc % 2) * 256:(dc % 2) * 256 + n], lhsT=vc[:, c, dc * 128:(dc + 1) * 128],
                                                       rhs=W_[:, :n], start=(i == 0), stop=(i == 127)), reads=[vc, W_], writes=[big4])
            for dc in range(KD):
                hn = hnr.next()
                kb.op('dve', lambda e: e.scalar_tensor_tensor(out=hn[:, :n], in0=big4[:, dc // 2, (dc % 2) * 256:(dc % 2) * 256 + n],
                                                              scalar=modT[:, l, 40 + dc, mc:mc + 1], in1=hb[:, dc, :n],
                                                              op0=ALU.mult, op1=ALU.add), reads=[big4, modT, hb], writes=[hn])
                if last:
                    kb.dma('sp', out_hT[dc * 128:(dc + 1) * 128, t0:t0 + n], hn[:, :n], reads=[hn])
                else:
                    kb.dma('sp', hT[dc * 128:(dc + 1) * 128, t0:t0 + n], hn[:, :n], reads=[hn])
        kb.pop("C%d" % l)

    return kb.finish(), kb


def _fmaj(v):
    v = np.asarray(v, np.float32)
    lead = v.shape[:-1]
    r = v.reshape(lead + (KD, 128))
    r = np.moveaxis(r, -1, 0)
    return np.ascontiguousarray(r)


def prep_inputs(S, CT, NL, b, x, c, ctx, c_ctx, norm1_g, norm2_g, w_mod, b_mod, w_in, q_norm_g, k_norm_g,
                lam_vecs, attn_norm_g, pool_w, pool_scale, conv_w, conv_out_w, w_out, peer_wq, peer_keys, peer_u, peer_v,
                shared=None):
    f = np.float32
    if shared is None:
        cos, sin = rope_tabs(S, CT)
        shared = {
            "w_mod": np.ascontiguousarray(w_mod, f),
            "b_modT": np.ascontiguousarray(np.asarray(b_mod, f).reshape(NL, 48, 128).transpose(2, 0, 1)),
            "g1T": _fmaj(norm1_g), "g2T": _fmaj(norm2_g),
            "w_in": np.ascontiguousarray(w_in, f),
            "qk_g": np.ascontiguousarray(np.stack([np.tile(np.asarray(q_norm_g, f), (1, 2)), np.tile(np.asarray(k_norm_g, f), (1, 2))], -1).transpose(1, 0, 2)),
            "lam_v": np.ascontiguousarray(np.asarray(lam_vecs, f).reshape(NL, 256)),
            "attn_g": np.ascontiguousarray(attn_norm_g, f),
            "pool_w": np.ascontiguousarray(np.asarray(pool_w, f).reshape(NL, D, 256)),
            "pscT": _fmaj(pool_scale),
            "cwT": np.ascontiguousarray(np.moveaxis(_fmaj(conv_w), 2, 3)),
            "conv_out_w": np.ascontiguousarray(conv_out_w, f),
            "w_out": np.ascontiguousarray(w_out, f),
            "peer_wq": np.ascontiguousarray(peer_wq, f),
            "keysT": np.ascontiguousarray(np.asarray(peer_keys, f).reshape(NL, 16, 128, 128).transpose(0, 3, 1, 2).reshape(NL, 128, 2048)),
            "peer_uL": np.ascontiguousarray(np.asarray(peer_u, f).reshape(NL, 128, 128, KD, 128).transpose(0, 1, 4, 3, 2).reshape(NL, N_EXP, D)),
            "peer_v": np.ascontiguousarray(peer_v, f),
            "c_iota128": np.ascontiguousarray(np.tile(np.arange(128, dtype=f), (128, 1))),
            "cosT": cos, "sinT": sin,
            "c_rot": rot_mat(), "c_bd": bd_mat(), "c_idf": np.eye(128, dtype=f),
            "c_band": band_mats(), "c_scat": scat_mats(),
            "c_iota": np.ascontiguousarray(np.tile(np.arange(16, dtype=f), (128, 1))),
        }
    m = dict(shared)
    m["hT0"] = np.ascontiguousarray(np.concatenate([np.asarray(x[b], f).T, np.asarray(ctx[b], f).T], axis=1))
    m["cvec"] = np.ascontiguousarray(np.stack([_fmaj(c[b]), _fmaj(c_ctx)], -1))
    return m, shared


_CACHE = {}


def kernel(**inputs):
    x = np.asarray(inputs["x"])
    B, S, _ = x.shape
    CT = inputs["ctx"].shape[1]
    NL = inputs["w_mod"].shape[0]
    key = (S, CT, NL)
    if key not in _CACHE:
        _CACHE[key] = build(S, CT, NL)[0]
    nc = _CACHE[key]
    in_maps = []
    shared = None
    for core in range(8):
        m, shared = prep_inputs(S, CT, NL, core % B, shared=shared, **inputs)
        in_maps.append(m)
    res = run_bass_kernel_spmd(nc, in_maps, core_ids=list(range(8)))
    out = np.stack([np.ascontiguousarray(np.asarray(res.results[b]["out_hT"]).T) for b in range(B)], 0)
    return out.astype(np.float32)
```

```python
import math
from contextlib import ExitStack
import numpy as np
import ml_dtypes
import concourse.bass as bass
import concourse.mybir as mybir
from concourse.bass_utils import run_bass_kernel_spmd

F32 = mybir.dt.float32
BF16 = mybir.dt.bfloat16
I32 = mybir.dt.int32
U32 = mybir.dt.uint32
ALU = mybir.AluOpType
AF = mybir.ActivationFunctionType
AX = mybir.AxisListType

D = 1024
KD = 8
EPS = 1e-6
Q_OFF, K_OFF, V_OFF, POOL_OFF, CIN_OFF, CB_OFF, CC_OFF, GATE_OFF = 0, 1024, 2048, 3072, 4096, 5120, 6144, 7168
IN_W = 10240
POOL_SIZES = (2, 4, 8, 16)
N_EXP = 16384


class Buf:
    _n = 0

    def __init__(self, t, name):
        self.t = t
        self.name = name
        self.last_w = None
        self.readers = []
        self.dsem = None
        Buf._n += 1
        self.id = Buf._n

    def __getitem__(self, idx):
        return self.t[idx]


class KB:
    ENG = ('pe', 'act', 'dve', 'pool', 'sp')

    def __init__(self, n_dsem=96):
        self.nc = bass.Bass("TRN2", target_bir_lowering=False)
        nc = self.nc
        self.es = ExitStack()
        self.E = {'pe': nc.tensor, 'act': nc.scalar, 'dve': nc.vector, 'pool': nc.gpsimd, 'sp': nc.sync}
        self.sems = {}
        self.cnt = {}
        for e in self.ENG:
            self.sems[('e', e)] = self.es.enter_context(nc.semaphore("p_" + e))
            self.cnt[('e', e)] = 0
        self.free_dsems = []
        for i in range(n_dsem):
            k = ('d', i)
            self.sems[k] = self.es.enter_context(nc.semaphore("d_%d" % i))
            self.cnt[k] = 0
            self.free_dsems.append(k)
        self.seen = {}
        self.n_ins = 0
        self.stack = [(self.es, [])]

    def push(self):
        self.stack.append((ExitStack(), []))

    def pop(self, tag=None):
        if tag is not None:
            self.log = getattr(self, "log", [])
            self.log.append((tag, self.n_ins))
        self.barrier()
        es, bufs = self.stack.pop()
        for b in bufs:
            if b.dsem is not None:
                self.free_dsems.append(b.dsem)
        es.close()

    def sbuf(self, name, shape, dt):
        es, bufs = self.stack[-1]
        t = es.enter_context(self.nc.sbuf_tensor(name + "_%d" % Buf._n, list(shape), dt))
        b = Buf(t, name)
        bufs.append(b)
        return b

    def psum(self, name, shape, dt=F32):
        es, bufs = self.stack[-1]
        t = es.enter_context(self.nc.psum_tensor(name + "_%d" % Buf._n, list(shape), dt))
        b = Buf(t, name)
        bufs.append(b)
        return b

    def _wait(self, eng, key, val):
        k = (eng, key)
        if self.seen.get(k, 0) >= val:
            return
        self.E[eng].wait_ge(self.sems[key], val)
        self.seen[k] = val

    def _deps(self, eng, reads, writes, is_dma=False):
        deps = {}
        me = ('e', eng)

        def add(st):
            key, val = st
            if deps.get(key, 0) < val:
                deps[key] = val
        for b in reads:
            if b.last_w is not None:
                if b.last_w[0] == me and eng == 'pe' and not is_dma:
                    continue
                add(b.last_w)
        for b in writes:
            if b.last_w is not None and (is_dma or b.last_w[0] != me):
                add(b.last_w)
            for r in b.readers:
                if r[0] == me and not is_dma:
                    continue
                add(r)
        for key, val in deps.items():
            self._wait(eng, key, val)

    def _stamp(self, st, reads, writes):
        for b in reads:
            b.readers.append(st)
            if len(b.readers) > 16:
                m = {}
                for k, v in b.readers:
                    if m.get(k, 0) < v:
                        m[k] = v
                b.readers = list(m.items())
        for b in writes:
            b.last_w = st
            b.readers = []

    def op(self, eng, fn, reads=(), writes=()):
        self._deps(eng, reads, writes)
        ins = fn(self.E[eng])
        key = ('e', eng)
        self.cnt[key] += 1
        ins.then_inc(self.sems[key], 1)
        self._stamp((key, self.cnt[key]), reads, writes)
        self.n_ins += 1
        return ins

    def dma(self, q, out, in_, reads=(), writes=(), indirect=None):
        self._deps(q, reads, writes, is_dma=True)
        b = (list(writes) + list(reads))[0]
        if b.dsem is None:
            b.dsem = self.free_dsems.pop(0)
        if indirect is None:
            ins = self.E[q].dma_start(out=out, in_=in_)
        else:
            ins = self.E[q].indirect_dma_start(out=out, out_offset=None, in_=in_, in_offset=indirect)
        self.cnt[b.dsem] += 16
        ins.then_inc(self.sems[b.dsem], 16)
        self._stamp((b.dsem, self.cnt[b.dsem]), reads, writes)
        self.n_ins += 1
        return ins

    def barrier(self):
        for f in self.ENG:
            for key, val in self.cnt.items():
                if val > 0:
                    self._wait(f, key, val)

    def finish(self):
        self.barrier()
        while len(self.stack) > 1:
            self.pop()
        return self.nc


class Ring:
    def __init__(self, bufs):
        self.bufs = bufs
        self.i = 0

    def next(self):
        b = self.bufs[self.i % len(self.bufs)]
        self.i += 1
        return b


def band_mats():
    out = np.zeros((128, 20, 128), np.float32)
    for wi, w in enumerate(POOL_SIZES):
        lo = w // 2
        hi = w - 1 - lo
        for t in range(128):
            for tp in range(-128, 256):
                if t - lo <= tp <= t + hi:
                    if tp < 0:
                        out[tp + 128, wi * 5 + 0, t] = 1.0 / w
                    elif tp < 128:
                        out[tp, wi * 5 + 1, t] = 1.0 / w
                    else:
                        out[tp - 128, wi * 5 + 2, t] = 1.0 / w
            s0, e0 = max(t - lo, 0), t + hi
            cnt = e0 - s0 + 1
            for tp in range(s0, min(e0, 127) + 1):
                out[tp, wi * 5 + 3, t] = 1.0 / cnt
            s1, e1 = t - lo, min(t + hi, 127)
            cnt = e1 - s1 + 1
            for tp in range(max(s1, 0), e1 + 1):
                out[tp, wi * 5 + 4, t] = 1.0 / cnt
        for kind in (1, 3, 4):
            out[:, wi * 5 + kind, :] -= np.eye(128, dtype=np.float32)
    return out


def scat_mats():
    out = np.zeros((128, 3, 384), np.float32)
    for t in range(128):
        if t - 1 >= 0:
            out[t - 1, 1, 0 * 128 + t] = 1.0
        else:
            out[127, 0, 0 * 128 + t] = 1.0
        out[t, 1, 1 * 128 + t] = 1.0
        if t + 1 < 128:
            out[t + 1, 1, 2 * 128 + t] = 1.0
        else:
            out[0, 2, 2 * 128 + t] = 1.0
    return out


def rope_tabs(S, CT):
    n_rows = S // 64
    rows = np.repeat(np.arange(n_rows, dtype=np.float32), 64)
    cols = np.tile(np.arange(64, dtype=np.float32), n_rows)
    inv = (10000.0 ** (-np.arange(16, dtype=np.float32) / 16)).astype(np.float32)
    ang = np.concatenate([rows[:, None] * inv, cols[:, None] * inv], axis=-1)
    cos = np.concatenate([np.cos(ang), np.ones((CT, 32), np.float32)], 0).astype(np.float32)
    sin = np.concatenate([np.sin(ang), np.zeros((CT, 32), np.float32)], 0).astype(np.float32)
    idx = np.arange(128) % 32
    return np.ascontiguousarray(cos[:, idx].T), np.ascontiguousarray(sin[:, idx].T)


def rot_mat():
    R = np.zeros((128, 128), np.float32)
    for p in range(128):
        if (p % 64) < 32:
            R[p + 32, p] = -1.0
        else:
            R[p - 32, p] = 1.0
    return R


def bd_mat():
    M = np.zeros((128, 128), np.float32)
    M[:64, :64] = 1.0 / 64
    M[64:, 64:] = 1.0 / 64
    return M


def build(S, CT, NL, dbg=False):
    kb = KB()
    nc = kb.nc
    T = S + CT
    NT = T // 128
    blocks = [(i * 512, 512) for i in range(S // 512)] + [(S, CT)]
    n_lat_blocks = S // 512
    SB = 256
    sblocks = [(i * SB, SB) for i in range(S // SB)] + [(S, CT)]
    n_lat_sblocks = S // SB

    def din(name, shape, dt=F32):
        return nc.dram_tensor(name, list(shape), dt, kind="ExternalInput").ap()

    def dscr(name, shape, dt):
        return nc.dram_tensor(name, list(shape), dt, kind="ExternalOutput" if dbg else "Internal").ap()

    hT0 = din("hT0", [D, T])
    cvec = din("cvec", [128, KD, 2])
    w_mod = din("w_mod", [NL, D, 6 * D])
    b_modT = din("b_modT", [128, NL, 48])
    g1T = din("g1T", [128, NL, KD])
    g2T = din("g2T", [128, NL, KD])
    w_in = din("w_in", [NL, D, IN_W])
    qk_g = din("qk_g", [128, NL, 2])
    lam_v = din("lam_v", [NL, 256])
    attn_g = din("attn_g", [NL, 128])
    pool_w = din("pool_w", [NL, D, 256])
    pscT = din("pscT", [128, NL, KD])
    cwT = din("cwT", [128, NL, KD, 3])
    conv_out_w = din("conv_out_w", [NL, D, D])
    w_out = din("w_out", [NL, D, D])
    peer_wq = din("peer_wq", [NL, D, 2048])
    keysT = din("keysT", [NL, 128, 2048])
    peer_uL = din("peer_uL", [NL, N_EXP, D])
    peer_v = din("peer_v", [NL, N_EXP, D])
    c_iota128 = din("c_iota128", [128, 128])
    cosT = din("cosT", [128, T])
    sinT = din("sinT", [128, T])
    c_rot = din("c_rot", [128, 128])
    c_bd = din("c_bd", [128, 128])
    c_idf = din("c_idf", [128, 128])
    c_band = din("c_band", [128, 20, 128])
    c_scat = din("c_scat", [128, 3, 384])
    c_iota = din("c_iota", [128, 16])
    out_hT = nc.dram_tensor("out_hT", [D, S], F32, kind="ExternalOutput").ap()

    hT = dscr("s_hT", [D, T], F32)
    xnT = dscr("s_xnT", [D, T], BF16)
    qT = dscr("s_qT", [D, T], BF16)
    kT = dscr("s_kT", [D, T], BF16)
    Vt = dscr("s_Vt", [T, D], BF16)
    uh = dscr("s_uh", [T, 2 * D], BF16)
    attnT = dscr("s_attnT", [D, T], BF16)
    w_in_b = dscr("s_w_in_b", [D, IN_W], BF16)
    co_b = dscr("s_co_b", [D, D], BF16)
    wo_b = dscr("s_wo_b", [D, D], BF16)
    pw_b = dscr("s_pw_b", [D, 256], BF16)
    wq_b = dscr("s_wq_b", [D, 2048], BF16)
    keys_b = dscr("s_keys_b", [128, 2048], BF16)
    uT_b = dscr("s_uT_b", [N_EXP, D], BF16)
    v_b = dscr("s_v_b", [N_EXP, D], BF16)

    def fm(ap2d, c0, n):
        return ap2d[:, c0:c0 + n].rearrange("(k p) t -> p k t", p=128)

    ones_m = kb.sbuf("ones_m", [128, 128], F32)
    rot_s = kb.sbuf("rot_s", [128, 128], F32)
    bd_s = kb.sbuf("bd_s", [128, 128], F32)
    idf = kb.sbuf("idf", [128, 128], F32)
    idb = kb.sbuf("idb", [128, 128], BF16)
    modT = kb.sbuf("modT", [128, NL, 48, 2], F32)
    A1 = kb.sbuf("A1", [128, NL, KD, 2], F32)
    A2 = kb.sbuf("A2", [128, NL, KD, 2], F32)
    g1s = kb.sbuf("g1s", [128, NL, KD], F32)
    g2s = kb.sbuf("g2s", [128, NL, KD], F32)
    qkg = kb.sbuf("qkg", [128, NL, 2], F32)
    psc = kb.sbuf("psc", [128, NL, KD], F32)
    cws = kb.sbuf("cws", [128, NL, KD, 3], F32)
    iota16 = kb.sbuf("iota16", [128, 16], F32)
    iota128 = kb.sbuf("iota128", [128, 128], F32)

    kb.op('dve', lambda e: e.memset(ones_m[:], 1.0 / D), writes=[ones_m])
    for sb, src in ((rot_s, c_rot), (bd_s, c_bd), (idf, c_idf), (g1s, g1T), (g2s, g2T), (qkg, qk_g),
                    (psc, pscT), (cws, cwT), (iota16, c_iota), (iota128, c_iota128)):
        kb.dma('sp', sb[:], src, writes=[sb])
    kb.op('dve', lambda e: e.tensor_copy(out=idb[:], in_=idf[:]), reads=[idf], writes=[idb])
    band = kb.sbuf("band", [128, 20, 128], BF16)
    scat = kb.sbuf("scat", [128, 3, 384], BF16)
    kb.push()
    bandf = kb.sbuf("bandf", [128, 20, 128], F32)
    scatf = kb.sbuf("scatf", [128, 3, 384], F32)
    kb.dma('sp', bandf[:], c_band, writes=[bandf])
    kb.dma('sp', scatf[:], c_scat, writes=[scatf])
    kb.op('dve', lambda e: e.tensor_copy(out=band[:], in_=bandf[:]), reads=[bandf], writes=[band])
    kb.op('dve', lambda e: e.tensor_copy(out=scat[:], in_=scatf[:]), reads=[scatf], writes=[scat])
    kb.pop()

    kb.push()
    stg = Ring([kb.sbuf("cp%d" % i, [128, KD, 512], F32) for i in range(2)])
    for (t0, n) in blocks:
        b = stg.next()
        kb.dma('sp', b[:, :, :n], fm(hT0, t0, n), writes=[b])
        kb.dma('sp', fm(hT, t0, n), b[:, :, :n], reads=[b])
    sc = kb.sbuf("sc", [128, KD, 2], F32)
    scs = kb.sbuf("scs", [128, KD, 2], F32)
    bm = kb.sbuf("bm", [128, NL, 48], F32)
    kb.dma('sp', sc[:], cvec, writes=[sc])
    kb.dma('sp', bm[:], b_modT, writes=[bm])
    kb.op('act', lambda e: e.activation(out=scs[:], in_=sc[:], func=AF.Silu), reads=[sc], writes=[scs])
    wmr = Ring([kb.sbuf("wm%d" % i, [128, KD, 512], F32) for i in range(2)])
    mps = kb.psum("mps", [128, 48, 2])
    for l in range(NL):
        for jt in range(12):
            wm = wmr.next()
            kb.dma('sp', wm[:], fm(w_mod[l], jt * 512, 512), writes=[wm])
            for jj in range(4):
                j = jt * 4 + jj
                for k in range(KD):
                    kb.op('pe', lambda e: e.matmul(mps[:, j, :], lhsT=wm[:, k, jj * 128:(jj + 1) * 128],
                                                   rhs=scs[:, k, :], start=(k == 0), stop=(k == KD - 1)),
                          reads=[wm, scs], writes=[mps])
        kb.op('dve', lambda e: e.tensor_tensor(out=modT[:, l], in0=mps[:],
                                               in1=bm[:, l, :].unsqueeze(2).to_broadcast([128, 48, 2]), op=ALU.add),
              reads=[mps, bm], writes=[modT])
        kb.op('dve', lambda e: e.scalar_tensor_tensor(
            out=A1[:, l], in0=modT[:, l, 8:16, :], scalar=1.0,
            in1=g1s[:, l, :].unsqueeze(2).to_broadcast([128, KD, 2]), op0=ALU.add, op1=ALU.mult),
            reads=[modT, g1s], writes=[A1])
        kb.op('dve', lambda e: e.scalar_tensor_tensor(
            out=A2[:, l], in0=modT[:, l, 32:40, :], scalar=1.0,
            in1=g2s[:, l, :].unsqueeze(2).to_broadcast([128, KD, 2]), op0=ALU.add, op1=ALU.mult),
            reads=[modT, g2s], writes=[A2])
    kb.pop()

    def convert(src2d, dst2d, R, C):
        kb.push()
        sf = Ring([kb.sbuf("cvf%d" % i, [128, 2048], F32) for i in range(3)])
        sb_ = Ring([kb.sbuf("cvb%d" % i, [128, 2048], BF16) for i in range(3)])
        engs = ['pool', 'act', 'dve']
        i = 0
        for r0 in range(0, R, 128):
            for c0 in range(0, C, 2048):
                cw = min(2048, C - c0)
                a = sf.next()
                b = sb_.next()
                kb.dma('sp', a[:, :cw], src2d[r0:r0 + 128, c0:c0 + cw], writes=[a])
                eng = engs[i % 3]
                i += 1
                if eng == 'act':
                    kb.op('act', lambda e: e.copy(out=b[:, :cw], in_=a[:, :cw]), reads=[a], writes=[b])
                else:
                    kb.op(eng, lambda e: e.tensor_copy(out=b[:, :cw], in_=a[:, :cw]), reads=[a], writes=[b])
                kb.dma('act', dst2d[r0:r0 + 128, c0:c0 + cw], b[:, :cw], reads=[b])
        kb.pop()

    def rsqrt_eps(ob, oap, ib, iap, eps):
        kb.op('dve', lambda e: e.tensor_scalar(out=oap, in0=iap, scalar1=eps, scalar2=None, op0=ALU.add), reads=[ib], writes=[ob])
        kb.op('act', lambda e: e.activation(out=oap, in_=oap, func=AF.Sqrt), reads=[ob], writes=[ob])
        kb.op('dve', lambda e: e.reciprocal(out=oap, in_=oap), reads=[ob], writes=[ob])

    def norm_block(hb, n, Acol, Bcol, outs, sqr, psn, rs, tmpr):
        for k in range(KD):
            sq = sqr.next()
            kb.op('act', lambda e: e.activation(out=sq[:, :n], in_=hb[:, k, :n], func=AF.Square), reads=[hb], writes=[sq])
            kb.op('pe', lambda e: e.matmul(psn[:, :n], lhsT=ones_m[:], rhs=sq[:, :n], start=(k == 0), stop=(k == KD - 1)),
                  reads=[ones_m, sq], writes=[psn])
        rsqrt_eps(rs, rs[:, :n], psn, psn[:, :n], EPS)
        for k in range(KD):
            tmp = tmpr.next()
            kb.op('dve', lambda e: e.scalar_tensor_tensor(out=tmp[:, :n], in0=hb[:, k, :n], scalar=Acol(k),
                                                          in1=rs[:, :n], op0=ALU.mult, op1=ALU.mult),
                  reads=[hb, rs, A1, A2], writes=[tmp])
            for ob in outs:
                kb.op('pool', lambda e: e.tensor_scalar(out=ob[:, k, :n], in0=tmp[:, :n], scalar1=Bcol(k), scalar2=None,
                                                        op0=ALU.add), reads=[tmp, modT], writes=[ob])

    for l in range(NL):
        last = (l == NL - 1)
        lam_init = 0.8 - 0.6 * math.exp(-0.3 * l)
        act_blocks = blocks[:n_lat_blocks] if last else blocks
        act_sblocks = sblocks[:n_lat_sblocks] if last else sblocks

        convert(w_in[l], w_in_b, D, IN_W)
        convert(conv_out_w[l], co_b, D, D)
        convert(w_out[l], wo_b, D, D)
        convert(pool_w[l], pw_b, D, 256)
        convert(peer_wq[l], wq_b, D, 2048)
        convert(keysT[l], keys_b, 128, 2048)
        convert(peer_uL[l], uT_b, N_EXP, D)
        convert(peer_v[l], v_b, N_EXP, D)

        kb.push()
        hb = kb.sbuf("hb", [128, KD, 512], F32)
        xn = kb.sbuf("xn", [128, KD, 512], BF16)
        sqr = Ring([kb.sbuf("sq%d" % i, [128, 512], F32) for i in range(2)])
        tmpr = Ring([kb.sbuf("tmp%d" % i, [128, 512], F32) for i in range(2)])
        rs = kb.sbuf("rs", [128, 512], F32)
        rs2 = kb.sbuf("rs2", [128, 512], F32)
        qn = kb.sbuf("qn", [128, 512], F32)
        t1 = kb.sbuf("t1", [128, 512], F32)
        t2 = kb.sbuf("t2", [128, 512], F32)
        cosb = kb.sbuf("cosb", [128, 512], F32)
        sinb = kb.sbuf("sinb", [128, 512], F32)
        qor = Ring([kb.sbuf("qo%d" % i, [128, 512], BF16) for i in range(2)])
        wr = Ring([kb.sbuf("wA%d" % i, [128, KD, 512], BF16) for i in range(3)])
        vbr = Ring([kb.sbuf("vb%d" % i, [128, 512], BF16) for i in range(2)])
        c1r = Ring([kb.sbuf("c1%d" % i, [128, 512], F32) for i in range(2)])
        psn = kb.psum("psn", [128, 512])
        psr = Ring([kb.psum("psA%d" % i, [128, 512]) for i in range(3)])
        ps2 = kb.psum("ps2", [128, 512])
        ps3 = kb.psum("ps3", [128, 512])
        psc2 = kb.psum("psc2", [128, 512])

        def loadw(col0):
            w = wr.next()
            kb.dma('sp', w[:], fm(w_in_b, col0, 512), writes=[w])
            return w

        for bi, (t0, n) in enumerate(blocks):
            nt = n // 128
            mc = 0 if bi < n_lat_blocks else 1
            kb.dma('sp', hb[:, :, :n], fm(hT, t0, n), writes=[hb])
            kb.dma('sp', cosb[:, :n], cosT[:, t0:t0 + n], writes=[cosb])
            kb.dma('sp', sinb[:, :n], sinT[:, t0:t0 + n], writes=[sinb])
            norm_block(hb, n, lambda k: A1[:, l, k, mc:mc + 1], lambda k: modT[:, l, k, mc:mc + 1], [xn],
                       sqr, psn, rs, tmpr)
            kb.dma('sp', fm(xnT, t0, n), xn[:, :, :n], reads=[xn])
            for (coloff, dst, gcol) in ((Q_OFF, qT, 0), (K_OFF, kT, 1)):
                for wt in range(2):
                    w = loadw(coloff + wt * 512)
                    for hh in range(4):
                        h = wt * 4 + hh
                        ps = psr.next()
                        for k in range(KD):
                            kb.op('pe', lambda e: e.matmul(ps[:, :n], lhsT=w[:, k, hh * 128:(hh + 1) * 128], rhs=xn[:, k, :n],
                                                           start=(k == 0), stop=(k == KD - 1)), reads=[w, xn], writes=[ps])
                        sq = sqr.next()
                        kb.op('act', lambda e: e.activation(out=sq[:, :n], in_=ps[:, :n], func=AF.Square), reads=[ps], writes=[sq])
                        kb.op('pe', lambda e: e.matmul(ps2[:, :n], lhsT=bd_s[:], rhs=sq[:, :n], start=True, stop=True),
                              reads=[bd_s, sq], writes=[ps2])
                        rsqrt_eps(rs2, rs2[:, :n], ps2, ps2[:, :n], EPS)
                        kb.op('dve', lambda e: e.scalar_tensor_tensor(out=qn[:, :n], in0=ps[:, :n], scalar=qkg[:, l, gcol:gcol + 1],
                                                                      in1=rs2[:, :n], op0=ALU.mult, op1=ALU.mult),
                              reads=[ps, rs2, qkg], writes=[qn])
                        kb.op('pe', lambda e: e.matmul(ps3[:, :n], lhsT=rot_s[:], rhs=qn[:, :n], start=True, stop=True),
                              reads=[rot_s, qn], writes=[ps3])
                        kb.op('pool', lambda e: e.tensor_tensor(out=t1[:, :n], in0=qn[:, :n], in1=cosb[:, :n], op=ALU.mult),
                              reads=[qn, cosb], writes=[t1])
                        kb.op('dve', lambda e: e.tensor_tensor(out=t2[:, :n], in0=ps3[:, :n], in1=sinb[:, :n], op=ALU.mult),
                              reads=[ps3, sinb], writes=[t2])
                        qo = qor.next()
                        kb.op('pool', lambda e: e.tensor_tensor(out=qo[:, :n], in0=t1[:, :n], in1=t2[:, :n], op=ALU.add),
                              reads=[t1, t2], writes=[qo])
                        kb.dma('sp', dst[h * 128:(h + 1) * 128, t0:t0 + n], qo[:, :n], reads=[qo])
            for (coloff, dstap, dcol) in ((V_OFF, Vt, 0), (POOL_OFF, uh, 0)):
                for wt in range(2):
                    w = loadw(coloff + wt * 512)
                    for ti in range(nt):
                        ps = psr.next()
                        for k in range(KD):
                            kb.op('pe', lambda e: e.matmul(ps[:], lhsT=xn[:, k, ti * 128:(ti + 1) * 128], rhs=w[:, k, :],
                                                           start=(k == 0), stop=(k == KD - 1)), reads=[w, xn], writes=[ps])
                        vb = vbr.next()
                        kb.op('act', lambda e: e.copy(out=vb[:], in_=ps[:]), reads=[ps], writes=[vb])
                        r0 = t0 + ti * 128
                        kb.dma('sp', dstap[r0:r0 + 128, dcol + wt * 512: dcol + (wt + 1) * 512], vb[:], reads=[vb])
            for wt in range(2):
                w1 = loadw(CIN_OFF + wt * 512)
                w2 = loadw(CC_OFF + wt * 512)
                for ti in range(nt):
                    ps = psr.next()
                    for k in range(KD):
                        kb.op('pe', lambda e: e.matmul(ps[:], lhsT=xn[:, k, ti * 128:(ti + 1) * 128], rhs=w1[:, k, :],
                                                       start=(k == 0), stop=(k == KD - 1)), reads=[w1, xn], writes=[ps])
                    for k in range(KD):
                        kb.op('pe', lambda e: e.matmul(psc2[:], lhsT=xn[:, k, ti * 128:(ti + 1) * 128], rhs=w2[:, k, :],
                                                       start=(k == 0), stop=(k == KD - 1)), reads=[w2, xn], writes=[psc2])
                    c1 = c1r.next()
                    kb.op('act', lambda e: e.copy(out=c1[:], in_=ps[:]), reads=[ps], writes=[c1])
                    vb = vbr.next()
                    kb.op('dve', lambda e: e.tensor_tensor(out=vb[:], in0=psc2[:], in1=c1[:], op=ALU.mult),
                          reads=[psc2, c1], writes=[vb])
                    r0 = t0 + ti * 128
                    kb.dma('sp', uh[r0:r0 + 128, D + wt * 512: D + (wt + 1) * 512], vb[:], reads=[vb])
        kb.pop("A%d" % l)

        kb.push()
        lvb = kb.sbuf("lvb", [128, 256], F32)
        lprod = kb.sbuf("lprod", [128, 128], F32)
        lsum = kb.sbuf("lsum", [128, 2], F32)
        lexp = kb.sbuf("lexp", [128, 2], F32)
        nlam = kb.sbuf("nlam", [128, 1], F32)
        gvT = kb.sbuf("gvT", [128, 1], F32)
        ones1 = kb.sbuf("ones1", [128, 128], F32)
        kb.op('pool', lambda e: e.memset(ones1[:], 1.0), writes=[ones1])
        kb.dma('sp', lvb[:], lam_v[l:l + 1, :].partition_broadcast(128), writes=[lvb])
        kb.dma('sp', gvT[:], attn_g[l:l + 1, :].rearrange("o e -> e o"), writes=[gvT])
        lv4 = lvb[:].rearrange("p (a d) -> p a d", a=4)
        kb.op('dve', lambda e: e.tensor_tensor(out=lprod[:, 0:64], in0=lv4[:, 0, :], in1=lv4[:, 1, :], op=ALU.mult),
              reads=[lvb], writes=[lprod])
        kb.op('dve', lambda e: e.tensor_tensor(out=lprod[:, 64:128], in0=lv4[:, 2, :], in1=lv4[:, 3, :], op=ALU.mult),
              reads=[lvb, lprod], writes=[lprod])
        kb.op('dve', lambda e: e.tensor_reduce(out=lsum[:], in_=lprod[:].rearrange("p (a d) -> p a d", a=2), axis=AX.X, op=ALU.add),
              reads=[lprod], writes=[lsum])
        kb.op('act', lambda e: e.activation(out=lexp[:], in_=lsum[:], func=AF.Exp), reads=[lsum], writes=[lexp])
        kb.op('dve', lambda e: e.tensor_tensor(out=nlam[:], in0=lexp[:, 1:2], in1=lexp[:, 0:1], op=ALU.subtract),
              reads=[lexp], writes=[nlam])
        kb.op('dve', lambda e: e.tensor_scalar(out=nlam[:], in0=nlam[:], scalar1=-lam_init, scalar2=None, op0=ALU.add),
              reads=[nlam], writes=[nlam])
        kb.op('dve', lambda e: e.tensor_scalar(out=gvT[:], in0=gvT[:], scalar1=math.sqrt(128.0) * (1.0 - lam_init),
                                               scalar2=None, op0=ALU.mult), reads=[gvT], writes=[gvT])
        kTr = Ring([kb.sbuf("kTh%d" % i, [128, T], BF16) for i in range(2)])
        qTr = Ring([kb.sbuf("qTh%d" % i, [128, T], BF16) for i in range(2)])
        var = Ring([kb.sbuf("vah%d" % i, [128, NT, 128], BF16) for i in range(2)])
        ptr = Ring([kb.sbuf("pt%d" % i, [128, 1024], BF16) for i in range(3)])
        zacc = kb.sbuf("zacc", [128, 1024], F32)
        rzb = kb.sbuf("rzb", [128, 1024], F32)
        osb = [kb.sbuf("osb%d" % i, [128, 1024], F32) for i in range(2)]
        oc = kb.sbuf("oc", [128, 1024], F32)
        osq = kb.sbuf("osq", [128, 1024], F32)
        rsd = kb.sbuf("rsd", [128, 1024], F32)
        aor = Ring([kb.sbuf("ao%d" % i, [128, 1024], BF16) for i in range(2)])
        psS = Ring([kb.psum("pS%d" % i, [128, 2, 512]) for i in range(2)])
        psO = kb.psum("pO", [128, 2, 512])
        psZ = Ring([kb.psum("pZ%d" % i, [128, 512]) for i in range(2)])
        qgroups = []
        for g0 in range(0, S, 1024):
            qgroups.append([(g0 + j, 512) for j in range(0, min(1024, S - g0), 512)])
        if not last:
            qgroups.append([(S, CT)])
        for h in range(8):
            kh = kTr.next()
            qh = qTr.next()
            va = var.next()
            kb.dma('sp', kh[:], kT[h * 128:(h + 1) * 128, :], writes=[kh])
            kb.dma('sp', qh[:], qT[h * 128:(h + 1) * 128, :], writes=[qh])
            for c0 in range(0, NT, 16):
                cn = min(16, NT - c0)
                kb.dma('sp', va[:, c0:c0 + cn, :],
                       Vt[c0 * 128:(c0 + cn) * 128, h * 128:(h + 1) * 128].rearrange("(c p) e -> p c e", p=128), writes=[va])
            for subs_ in qgroups:
                nb = len(subs_)
                is_ctx = subs_[0][0] >= S
                kcs = list(range(S // 128, NT)) if is_ctx else list(range(NT))
                ntot = 512 * (nb - 1) + subs_[-1][1]
                for s in range(2):
                    def emit_qk(kc):
                        ps = psS.next()
                        for b_, (tq, n) in enumerate(subs_):
                            kb.op('pe', lambda e: e.matmul(ps[:, b_, :n], lhsT=kh[s * 64:(s + 1) * 64, kc * 128:(kc + 1) * 128],
                                                           rhs=qh[s * 64:(s + 1) * 64, tq:tq + n], start=True, stop=True),
                                  reads=[kh, qh], writes=[ps])
                        return ps

                    def emit_rest(ci, kc, ps):
                        pt = ptr.next()
                        if nb == 2:
                            kb.op('act', lambda e: e.activation(out=pt[:].rearrange("p (b x) -> p b x", b=2), in_=ps[:], func=AF.Exp, scale=0.125),
                                  reads=[ps], writes=[pt])
                        else:
                            n = subs_[0][1]
                            kb.op('act', lambda e: e.activation(out=pt[:, :n], in_=ps[:, 0, :n], func=AF.Exp, scale=0.125),
                                  reads=[ps], writes=[pt])
                        for b_, (tq, n) in enumerate(subs_):
                            kb.op('pe', lambda e: e.matmul(psO[:, b_, :n], lhsT=va[:, kc, :], rhs=pt[:, b_ * 512:b_ * 512 + n],
                                                           start=(ci == 0), stop=(ci == len(kcs) - 1)), reads=[pt, va], writes=[psO])
                        if ci == 0:
                            kb.op('dve', lambda e: e.tensor_copy(out=zacc[:, :ntot], in_=pt[:, :ntot]), reads=[pt], writes=[zacc])
                        else:
                            kb.op('dve', lambda e: e.tensor_tensor(out=zacc[:, :ntot], in0=zacc[:, :ntot], in1=pt[:, :ntot], op=ALU.add),
                                  reads=[pt, zacc], writes=[zacc])
                    prev = None
                    for ci, kc in enumerate(kcs):
                        ps = emit_qk(kc)
                        if prev is not None:
                            emit_rest(*prev)
                        prev = (ci, kc, ps)
                    emit_rest(*prev)
                    for b_, (tq, n) in enumerate(subs_):
                        sl = slice(b_ * 512, b_ * 512 + n)
                        pz = psZ.next()
                        kb.op('pe', lambda e: e.matmul(pz[:, :n], lhsT=ones1[:], rhs=zacc[:, sl], start=True, stop=True),
                              reads=[ones1, zacc], writes=[pz])
                        kb.op('dve', lambda e: e.reciprocal(out=rzb[:, sl], in_=pz[:, :n]), reads=[pz], writes=[rzb])
                        kb.op('dve', lambda e: e.tensor_tensor(out=osb[s][:, sl], in0=psO[:, b_, :n], in1=rzb[:, sl], op=ALU.mult),
                              reads=[psO, rzb], writes=[osb[s]])
                ao = aor.next()
                for b_, (tq, n) in enumerate(subs_):
                    sl = slice(b_ * 512, b_ * 512 + n)
                    kb.op('dve', lambda e: e.scalar_tensor_tensor(out=oc[:, sl], in0=osb[1][:, sl], scalar=nlam[:, 0:1], in1=osb[0][:, sl],
                                                                  op0=ALU.mult, op1=ALU.add), reads=[osb[0], osb[1], nlam], writes=[oc])
                    kb.op('act', lambda e: e.activation(out=osq[:, sl], in_=oc[:, sl], func=AF.Square), reads=[oc], writes=[osq])
                    pz = psZ.next()
                    kb.op('pe', lambda e: e.matmul(pz[:, :n], lhsT=ones1[:], rhs=osq[:, sl], start=True, stop=True),
                          reads=[ones1, osq], writes=[pz])
                    rsqrt_eps(rsd, rsd[:, sl], pz, pz[:, :n], 128.0 * EPS)
                    kb.op('dve', lambda e: e.scalar_tensor_tensor(out=ao[:, sl], in0=oc[:, sl], scalar=gvT[:, 0:1], in1=rsd[:, sl],
                                                                  op0=ALU.mult, op1=ALU.mult), reads=[oc, gvT, rsd], writes=[ao])
                    kb.dma('sp', attnT[h * 128:(h + 1) * 128, tq:tq + n], ao[:, sl], reads=[ao])
        kb.pop("B1_%d" % l)

        kb.push()
        cow = kb.sbuf("cow", [128, KD, D], BF16)
        wow = kb.sbuf("wow", [128, KD, D], BF16)
        pww = kb.sbuf("pww", [128, KD, 256], BF16)
        kb.dma('sp', cow[:], fm(co_b, 0, D), writes=[cow])
        kb.dma('sp', wow[:], fm(wo_b, 0, D), writes=[wow])
        kb.dma('sp', pww[:], fm(pw_b, 0, 256), writes=[pww])
        xn = kb.sbuf("xnB", [128, KD, SB], BF16)
        uht = kb.sbuf("uht", [128, 4, 2 * D], BF16)
        att = kb.sbuf("att", [128, KD, SB], BF16)
        cbT = kb.sbuf("cbT", [128, KD, SB], BF16)
        plT = kb.sbuf("plT", [128, KD, SB], BF16)
        bcT = kb.sbuf("bcT", [128, KD, SB], BF16)
        mgT = kb.sbuf("mgT", [128, KD, SB], BF16)
        wcr = Ring([kb.sbuf("wcb%d" % i, [128, KD, 512], BF16) for i in range(2)])
        gwr = Ring([kb.sbuf("gw%d" % i, [128, KD, 3, 128], BF16) for i in range(2)])
        gTr = Ring([kb.sbuf("gT%d" % i, [128, 3, SB], F32) for i in range(2)])
        hcr = Ring([kb.sbuf("hcb%d" % i, [128, SB], F32) for i in range(2)])
        hnr = Ring([kb.sbuf("hnb%d" % i, [128, SB], F32) for i in range(2)])
        m0 = kb.sbuf("m0", [128, SB], F32)
        m1 = kb.sbuf("m1", [128, SB], F32)
        m2 = kb.sbuf("m2", [128, SB], F32)
        cacc = Ring([kb.sbuf("cacc%d" % i, [128, 128], F32) for i in range(2)])
        psr = Ring([kb.psum("psB%d" % i, [128, 512]) for i in range(4)])
        pcv = Ring([kb.psum("pcv%d" % i, [128, 512]) for i in range(2)])
        for bi, (t0, n) in enumerate(act_sblocks):
            nt = n // 128
            tile0 = t0 // 128
            is_ctx = bi >= n_lat_sblocks
            mc = 1 if is_ctx else 0
            seq_first = (tile0 == 0) or (tile0 == S // 128)
            seq_last_tile = (S // 128 - 1) if not is_ctx else (NT - 1)
            kb.dma('sp', xn[:, :, :n], fm(xnT, t0, n), writes=[xn])
            kb.dma('sp', att[:, :, :n], fm(attnT, t0, n), writes=[att])
            lo_t = tile0 if seq_first else tile0 - 1
            hi_t = min(tile0 + nt, seq_last_tile)
            kb.dma('sp', uht[:, lo_t - (tile0 - 1): hi_t - (tile0 - 1) + 1, :],
                   uh[lo_t * 128:(hi_t + 1) * 128, :].rearrange("(q p) e -> p q e", p=128), writes=[uht])
            for wt in range(2):
                w = wcr.next()
                kb.dma('sp', w[:], fm(w_in_b, CB_OFF + wt * 512, 512), writes=[w])
                for cc in range(4):
                    ps = psr.next()
                    for k in range(KD):
                        kb.op('pe', lambda e: e.matmul(ps[:, :n], lhsT=w[:, k, cc * 128:(cc + 1) * 128], rhs=xn[:, k, :n],
                                                       start=(k == 0), stop=(k == KD - 1)), reads=[w, xn], writes=[ps])
                    kb.op('act', lambda e: e.copy(out=cbT[:, wt * 4 + cc, :n], in_=ps[:, :n]), reads=[ps], writes=[cbT])
            for c in range(KD):
                wi = c // 2
                ps = psr.next()
                for ti in range(nt):
                    tg = tile0 + ti
                    first = (tg == 0) or (tg == S // 128)
                    lastt = (tg == seq_last_tile)
                    terms = []
                    if not first:
                        terms.append((ti, wi * 5 + 0))
                    terms.append((ti + 1, wi * 5 + (3 if first else (4 if lastt else 1))))
                    if not lastt:
                        terms.append((ti + 2, wi * 5 + 2))
                    for j, (slot, kind) in enumerate(terms):
                        kb.op('pe', lambda e: e.matmul(ps[:, ti * 128:(ti + 1) * 128], lhsT=uht[:, slot, c * 128:(c + 1) * 128],
                                                       rhs=band[:, kind, :], start=(j == 0), stop=(j == len(terms) - 1)),
                              reads=[uht, band], writes=[ps])
                kb.op('act', lambda e: e.copy(out=plT[:, c, :n], in_=ps[:, :n]), reads=[ps], writes=[plT])
            for c in range(KD):
                for ti in range(nt):
                    tg = tile0 + ti
                    first = (tg == 0) or (tg == S // 128)
                    lastt = (tg == seq_last_tile)
                    terms = [(ti + 1, 1)]
                    if not first:
                        terms.append((ti, 0))
                    if not lastt:
                        terms.append((ti + 2, 2))
                    pc = pcv.next()
                    for j, (slot, kind) in enumerate(terms):
                        kb.op('pe', lambda e: e.matmul(pc[:, 0:384], lhsT=uht[:, slot, D + c * 128: D + (c + 1) * 128],
                                                       rhs=scat[:, kind, :], start=(j == 0), stop=(j == len(terms) - 1)),
                              reads=[uht, scat], writes=[pc])
                    ca = cacc.next()
                    kb.op('dve', lambda e: e.tensor_scalar(out=ca[:], in0=pc[:, 0:128], scalar1=cws[:, l, c, 0:1], scalar2=None,
                                                           op0=ALU.mult), reads=[pc, cws], writes=[ca])
                    kb.op('dve', lambda e: e.scalar_tensor_tensor(out=ca[:], in0=pc[:, 128:256], scalar=cws[:, l, c, 1:2], in1=ca[:],
                                                                  op0=ALU.mult, op1=ALU.add), reads=[pc, cws, ca], writes=[ca])
                    kb.op('dve', lambda e: e.scalar_tensor_tensor(out=ca[:], in0=pc[:, 256:384], scalar=cws[:, l, c, 2:3], in1=ca[:],
                                                                  op0=ALU.mult, op1=ALU.add), reads=[pc, cws, ca], writes=[ca])
                    kb.op('pool', lambda e: e.tensor_tensor(out=bcT[:, c, ti * 128:(ti + 1) * 128], in0=ca[:],
                                                            in1=cbT[:, c, ti * 128:(ti + 1) * 128], op=ALU.mult),
                          reads=[ca, cbT], writes=[bcT])
            for dc in range(KD):
                gw = gwr.next()
                for j in range(3):
                    kb.dma('sp', gw[:, :, j, :], fm(w_in_b, GATE_OFF + j * D + dc * 128, 128), writes=[gw])
                gT = gTr.next()
                for j in range(3):
                    ps = psr.next()
                    for k in range(KD):
                        kb.op('pe', lambda e: e.matmul(ps[:, :n], lhsT=gw[:, k, j, :], rhs=xn[:, k, :n],
                                                       start=(k == 0), stop=(k == KD - 1)), reads=[gw, xn], writes=[ps])
                    kb.op('act', lambda e: e.activation(out=gT[:, j, :n], in_=ps[:, :n], func=AF.Sigmoid), reads=[ps], writes=[gT])
                kb.op('dve', lambda e: e.tensor_tensor(out=m0[:, :n], in0=att[:, dc, :n], in1=gT[:, 0, :n], op=ALU.mult),
                      reads=[att, gT], writes=[m0])
                ps = psr.next()
                g = dc // 2
                dh = dc % 2
                for kk in range(2):
                    kb.op('pe', lambda e: e.matmul(ps[:, :n], lhsT=pww[:, g * 2 + kk, dh * 128:(dh + 1) * 128], rhs=plT[:, g * 2 + kk, :n],
                                                   start=(kk == 0), stop=(kk == 1)), reads=[pww, plT], writes=[ps])
                kb.op('dve', lambda e: e.scalar_tensor_tensor(out=m1[:, :n], in0=ps[:, :n], scalar=psc[:, l, dc:dc + 1], in1=gT[:, 1, :n],
                                                              op0=ALU.mult, op1=ALU.mult), reads=[ps, psc, gT], writes=[m1])
                ps = psr.next()
                for k in range(KD):
                    kb.op('pe', lambda e: e.matmul(ps[:, :n], lhsT=cow[:, k, dc * 128:(dc + 1) * 128], rhs=bcT[:, k, :n],
                                                   start=(k == 0), stop=(k == KD - 1)), reads=[cow, bcT], writes=[ps])
                kb.op('dve', lambda e: e.tensor_tensor(out=m2[:, :n], in0=ps[:, :n], in1=gT[:, 2, :n], op=ALU.mult),
                      reads=[ps, gT], writes=[m2])
                kb.op('pool', lambda e: e.tensor_tensor(out=m0[:, :n], in0=m0[:, :n], in1=m1[:, :n], op=ALU.add),
                      reads=[m0, m1], writes=[m0])
                kb.op('pool', lambda e: e.tensor_tensor(out=mgT[:, dc, :n], in0=m0[:, :n], in1=m2[:, :n], op=ALU.add),
                      reads=[m0, m2], writes=[mgT])
            for dc in range(KD):
                ps = psr.next()
                for k in range(KD):
                    kb.op('pe', lambda e: e.matmul(ps[:, :n], lhsT=wow[:, k, dc * 128:(dc + 1) * 128], rhs=mgT[:, k, :n],
                                                   start=(k == 0), stop=(k == KD - 1)), reads=[wow, mgT], writes=[ps])
                hc_ = hcr.next()
                kb.dma('sp', hc_[:, :n], hT[dc * 128:(dc + 1) * 128, t0:t0 + n], writes=[hc_])
                hn = hnr.next()
                kb.op('dve', lambda e: e.scalar_tensor_tensor(out=hn[:, :n], in0=ps[:, :n], scalar=modT[:, l, 16 + dc, mc:mc + 1],
                                                              in1=hc_[:, :n], op0=ALU.mult, op1=ALU.add),
                      reads=[ps, modT, hc_], writes=[hn])
                kb.dma('sp', hT[dc * 128:(dc + 1) * 128, t0:t0 + n], hn[:, :n], reads=[hn])
        kb.pop("B2_%d" % l)

        kb.push()
        kys = kb.sbuf("kys", [128, 16, 128], BF16)
        kb.dma('sp', kys[:], keys_b.rearrange("p (g k) -> p g k", g=16), writes=[kys])
        hb = kb.sbuf("hbC", [128, KD, SB], F32)
        xb = kb.sbuf("xbC", [128, KD, SB], BF16)
        sqr = Ring([kb.sbuf("sqC%d" % i, [128, SB], F32) for i in range(2)])
        tmpr = Ring([kb.sbuf("tmpC%d" % i, [128, SB], F32) for i in range(2)])
        rs = kb.sbuf("rsC", [128, SB], F32)
        wqr = Ring([kb.sbuf("wq%d" % i, [128, KD, 512], BF16) for i in range(2)])
        qp = kb.sbuf("qp", [128, 16, SB], BF16)
        bufA = kb.sbuf("bufA", [128, 2048], F32)
        bufB = kb.sbuf("bufB", [128, 2048], F32)
        stop_ = kb.sbuf("stop", [128, 16, 16], F32)
        itop = kb.sbuf("itop", [128, 16, 16], U32)
        itf = kb.sbuf("itf", [128, 16, 16], F32)
        tops = kb.sbuf("tops", [128, 8, 16], F32)
        pos = kb.sbuf("pos", [128, 8, 16], U32)
        pa = kb.sbuf("pa", [128, 8, 16], U32)
        pbb = kb.sbuf("pbb", [128, 8, 16], U32)
        paf = kb.sbuf("paf", [128, 8, 16], F32)
        pbf = kb.sbuf("pbf", [128, 8, 16], F32)
        sel3 = kb.sbuf("sel3", [128, 3, 128], F32)
        ex = kb.sbuf("ex", [128, 8, 16], F32)
        esum = kb.sbuf("esum", [128, 8], F32)
        itT = kb.sbuf("itT", [128, 3, SB], F32)
        ohA = kb.sbuf("ohA", [128, 16, 128], BF16)
        Ar = Ring([kb.sbuf("Aoh%d" % i, [128, 16, 128], BF16) for i in range(2)])
        Br = Ring([kb.sbuf("Boh%d" % i, [128, 16, 128], BF16) for i in range(2)])
        GT = kb.sbuf("GT", [128, 128, SB], BF16)
        ucr = Ring([kb.sbuf("uc%d" % i, [128, 2, D], BF16) for i in range(3)])
        vcr = Ring([kb.sbuf("vc%d" % i, [128, 2, D], BF16) for i in range(3)])
        x2r = Ring([kb.sbuf("x2_%d" % i, [128, SB], F32) for i in range(2)])
        ttr = Ring([kb.sbuf("tt_%d" % i, [128, SB], F32) for i in range(2)])
        sgr = Ring([kb.sbuf("sg_%d" % i, [128, SB], F32) for i in range(2)])
        xgr = Ring([kb.sbuf("xg_%d" % i, [128, SB], F32) for i in range(2)])
        Wr = Ring([kb.sbuf("W_%d" % i, [128, SB], BF16) for i in range(3)])
        hnr = Ring([kb.sbuf("hnC%d" % i, [128, SB], F32) for i in range(2)])
        psn = kb.psum("psnC", [128, 512])
        psq = Ring([kb.psum("psq%d" % i, [128, 512]) for i in range(2)])
        big4 = kb.psum("big4", [128, 4, 512])
        sS = bufA[:].rearrange("p (g k) -> p g k", g=16)
        sS2 = bufB[:].rearrange("p (g k) -> p g k", g=16)
        cand = bufA[:].rearrange("p (h c) -> p h c", h=8)
        cand2 = bufB[:].rearrange("p (h c) -> p h c", h=8)
        oh = bufB[:].rearrange("p (h k a) -> p h k a", h=8, k=16)
        GC = 1.5957691216057308
        for bi, (t0, n) in enumerate(act_sblocks):
            nt = n // 128
            mc = 1 if bi >= n_lat_sblocks else 0
            kb.dma('sp', hb[:, :, :n], fm(hT, t0, n), writes=[hb])
            norm_block(hb, n, lambda k: A2[:, l, k, mc:mc + 1], lambda k: modT[:, l, 24 + k, mc:mc + 1], [xb],
                       sqr, psn, rs, tmpr)
            for wt in range(4):
                wq = wqr.next()
                kb.dma('sp', wq[:], fm(wq_b, wt * 512, 512), writes=[wq])
                for gg in range(4):
                    g = wt * 4 + gg
                    ps = psq.next()
                    for k in range(KD):
                        kb.op('pe', lambda e: e.matmul(ps[:, :n], lhsT=wq[:, k, gg * 128:(gg + 1) * 128], rhs=xb[:, k, :n],
                                                       start=(k == 0), stop=(k == KD - 1)), reads=[wq, xb], writes=[ps])
                    kb.op('act', lambda e: e.copy(out=qp[:, g, :n], in_=ps[:, :n]), reads=[ps], writes=[qp])
            for ti in range(nt):
                tsl = slice(ti * 128, (ti + 1) * 128)
                for g in range(16):
                    kb.op('pe', lambda e: e.matmul(big4[:, g // 4, (g % 4) * 128:(g % 4 + 1) * 128], lhsT=qp[:, g, tsl], rhs=kys[:, g, :],
                                                   start=True, stop=True), reads=[qp, kys], writes=[big4])
                kb.op('act', lambda e: e.copy(out=bufA[:].rearrange("p (a x) -> p a x", a=4), in_=big4[:]), reads=[big4], writes=[bufA])
                for g in range(16):
                    kb.op('dve', lambda e: e.max(out=stop_[:, g, 0:8], in_=sS[:, g, :]), reads=[bufA], writes=[stop_])
                    kb.op('dve', lambda e: e.max_index(out=itop[:, g, 0:8], in_max=stop_[:, g, 0:8], in_values=sS[:, g, :]),
                          reads=[bufA, stop_], writes=[itop])
                    kb.op('dve', lambda e: e.match_replace(out=sS2[:, g, :], in_to_replace=stop_[:, g, 0:8], in_values=sS[:, g, :],
                                                           imm_value=-1e30), reads=[bufA, stop_], writes=[bufB])
                    kb.op('dve', lambda e: e.max(out=stop_[:, g, 8:16], in_=sS2[:, g, :]), reads=[bufB], writes=[stop_])
                    kb.op('dve', lambda e: e.max_index(out=itop[:, g, 8:16], in_max=stop_[:, g, 8:16], in_values=sS2[:, g, :]),
                          reads=[bufB, stop_], writes=[itop])
                kb.op('dve', lambda e: e.tensor_copy(out=itf[:], in_=itop[:]), reads=[itop], writes=[itf])
                s4 = stop_[:].rearrange("p (h i) a -> p h i a", i=2)
                kb.op('dve', lambda e: e.tensor_tensor(
                    out=cand.rearrange("p h (a b) -> p h a b", a=16),
                    in0=s4[:, :, 0, :].unsqueeze(3).to_broadcast([128, 8, 16, 16]),
                    in1=s4[:, :, 1, :].unsqueeze(2).to_broadcast([128, 8, 16, 16]), op=ALU.add), reads=[stop_], writes=[bufA])
                for h in range(8):
                    kb.op('dve', lambda e: e.max(out=tops[:, h, 0:8], in_=cand[:, h, :]), reads=[bufA], writes=[tops])
                    kb.op('dve', lambda e: e.max_index(out=pos[:, h, 0:8], in_max=tops[:, h, 0:8], in_values=cand[:, h, :]),
                          reads=[bufA, tops], writes=[pos])
                    kb.op('dve', lambda e: e.match_replace(out=cand2[:, h, :], in_to_replace=tops[:, h, 0:8], in_values=cand[:, h, :],
                                                           imm_value=-1e30), reads=[bufA, tops], writes=[bufB])
                    kb.op('dve', lambda e: e.max(out=tops[:, h, 8:16], in_=cand2[:, h, :]), reads=[bufB], writes=[tops])
                    kb.op('dve', lambda e: e.max_index(out=pos[:, h, 8:16], in_max=tops[:, h, 8:16], in_values=cand2[:, h, :]),
                          reads=[bufB, tops], writes=[pos])
                kb.op('dve', lambda e: e.tensor_single_scalar(out=pa[:], in_=pos[:], scalar=4, op=ALU.logical_shift_right),
                      reads=[pos], writes=[pa])
                kb.op('dve', lambda e: e.tensor_single_scalar(out=pbb[:], in_=pos[:], scalar=15, op=ALU.bitwise_and),
                      reads=[pos], writes=[pbb])
                kb.op('dve', lambda e: e.tensor_copy(out=paf[:], in_=pa[:]), reads=[pa], writes=[paf])
                kb.op('dve', lambda e: e.tensor_copy(out=pbf[:], in_=pbb[:]), reads=[pbb], writes=[pbf])
                i4 = itf[:].rearrange("p (h i) a -> p h i a", i=2)
                for (pf, ii) in ((paf, 0), (pbf, 1)):
                    kb.op('dve', lambda e: e.tensor_tensor(
                        out=oh, in0=pf[:].unsqueeze(3).to_broadcast([128, 8, 16, 16]),
                        in1=iota16[:].unsqueeze(1).unsqueeze(1).to_broadcast([128, 8, 16, 16]), op=ALU.is_equal),
                        reads=[pf, iota16], writes=[bufB])
                    kb.op('dve', lambda e: e.tensor_tensor(
                        out=oh, in0=oh, in1=i4[:, :, ii, :].unsqueeze(2).to_broadcast([128, 8, 16, 16]), op=ALU.mult),
                        reads=[bufB, itf], writes=[bufB])
                    kb.op('dve', lambda e: e.tensor_reduce(out=sel3[:, ii, :].rearrange("p (h k) -> p h k", h=8), in_=oh, axis=AX.X, op=ALU.add),
                          reads=[bufB], writes=[sel3])
                kb.op('dve', lambda e: e.tensor_tensor(out=ex[:], in0=tops[:], in1=tops[:, :, 0:1].to_broadcast([128, 8, 16]),
                                                       op=ALU.subtract), reads=[tops], writes=[ex])
                kb.op('act', lambda e: e.activation(out=ex[:], in_=ex[:], func=AF.Exp), reads=[ex], writes=[ex])
                kb.op('dve', lambda e: e.tensor_reduce(out=esum[:], in_=ex[:], axis=AX.X, op=ALU.add), reads=[ex], writes=[esum])
                kb.op('dve', lambda e: e.reciprocal(out=esum[:], in_=esum[:]), reads=[esum], writes=[esum])
                kb.op('dve', lambda e: e.tensor_tensor(out=sel3[:, 2, :].rearrange("p (h k) -> p h k", h=8), in0=ex[:],
                                                       in1=esum[:].unsqueeze(2).to_broadcast([128, 8, 16]), op=ALU.mult),
                      reads=[ex, esum], writes=[sel3])
                for x3 in range(3):
                    kb.op('pe', lambda e: e.transpose(psn[:, x3 * 128:(x3 + 1) * 128], sel3[:, x3, :], idf[:]), reads=[sel3, idf], writes=[psn])
                kb.op('act', lambda e: e.copy(out=itT[:, :, tsl], in_=psn[:, 0:384].rearrange("p (x t) -> p x t", x=3)), reads=[psn], writes=[itT])
            for tg in range(0, n, 16):
                A_ = Ar.next()
                B_ = Br.next()
                io_b = iota128[:].unsqueeze(1).to_broadcast([128, 16, 128])
                kb.op('dve', lambda e: e.tensor_tensor(out=ohA[:], in0=io_b, in1=itT[:, 0, tg:tg + 16].unsqueeze(2).to_broadcast([128, 16, 128]),
                                                       op=ALU.is_equal), reads=[iota128, itT], writes=[ohA])
                kb.op('dve', lambda e: e.tensor_tensor(out=A_[:], in0=ohA[:], in1=itT[:, 2, tg:tg + 16].unsqueeze(2).to_broadcast([128, 16, 128]),
                                                       op=ALU.mult), reads=[ohA, itT], writes=[A_])
                kb.op('dve', lambda e: e.tensor_tensor(out=B_[:], in0=io_b, in1=itT[:, 1, tg:tg + 16].unsqueeze(2).to_broadcast([128, 16, 128]),
                                                       op=ALU.is_equal), reads=[iota128, itT], writes=[B_])
                for t4 in range(4):
                    pg = psq.next()
                    for tt in range(4):
                        t_ = t4 * 4 + tt
                        kb.op('pe', lambda e: e.matmul(pg[:, tt * 128:(tt + 1) * 128], lhsT=B_[:, t_, :], rhs=A_[:, t_, :], start=True, stop=True),
                              reads=[A_, B_], writes=[pg])
                    tok0 = tg + t4 * 4
                    kb.op('act', lambda e: e.copy(out=GT[:, :, tok0:tok0 + 4].rearrange("p i t -> p t i"),
                                                  in_=pg[:].rearrange("p (t i) -> p t i", t=4)), reads=[pg], writes=[GT])
            def emit_hid(i, uc, c):
                ph = psq.next()
                for k in range(KD):
                    kb.op('pe', lambda e: e.matmul(ph[:, :n], lhsT=uc[:, c, k * 128:(k + 1) * 128], rhs=xb[:, k, :n],
                                                   start=(k == 0), stop=(k == KD - 1)), reads=[uc, xb], writes=[ph])
                return ph

            def emit_out(i, vc, c, ph):
                x2 = x2r.next()
                tt_ = ttr.next()
                sg = sgr.next()
                xg = xgr.next()
                W_ = Wr.next()
                kb.op('act', lambda e: e.activation(out=x2[:, :n], in_=ph[:, :n], func=AF.Square), reads=[ph], writes=[x2])
                kb.op('dve', lambda e: e.tensor_scalar(out=tt_[:, :n], in0=x2[:, :n], scalar1=0.044715, scalar2=1.0,
                                                       op0=ALU.mult, op1=ALU.add), reads=[x2], writes=[tt_])
                kb.op('dve', lambda e: e.tensor_tensor(out=tt_[:, :n], in0=ph[:, :n], in1=tt_[:, :n], op=ALU.mult),
                      reads=[ph, tt_], writes=[tt_])
                kb.op('act', lambda e: e.activation(out=sg[:, :n], in_=tt_[:, :n], func=AF.Sigmoid, scale=GC), reads=[tt_], writes=[sg])
                kb.op('dve', lambda e: e.tensor_tensor(out=xg[:, :n], in0=ph[:, :n], in1=GT[:, i, :n], op=ALU.mult),
                      reads=[ph, GT], writes=[xg])
                kb.op('pool', lambda e: e.tensor_tensor(out=W_[:, :n], in0=xg[:, :n], in1=sg[:, :n], op=ALU.mult),
                      reads=[xg, sg], writes=[W_])
                for dc in range(KD):
                    kb.op('pe', lambda e: e.matmul(big4[:, dc // 2, (dc % 2) * 256:(dc % 2) * 256 + n], lhsT=vc[:, c, dc * 128:(dc + 1) * 128],
                                                   rhs=W_[:, :n], start=(i == 0), stop=(i == 127)), reads=[vc, W_], writes=[big4])
            prevd = None
            for ig in range(0, 128, 2):
                uc = ucr.next()
                vc = vcr.next()
                kb.dma('sp', uc[:], uT_b[ig * 128:(ig + 2) * 128, :].rearrange("(c p) x -> p c x", p=128), writes=[uc])
                kb.dma('sp', vc[:], v_b[ig * 128:(ig + 2) * 128, :].rearrange("(c p) x -> p c x", p=128), writes=[vc])
                for c in range(2):
                    i = ig + c
                    ph = emit_hid(i, uc, c)
                    if prevd is not None:
                        emit_out(*prevd)
                    prevd = (i, vc, c, ph)
            emit_out(*prevd)
            for dc in range(KD):
                hn = hnr.next()
                kb.op('dve', lambda e: e.scalar_tensor_tensor(out=hn[:, :n], in0=big4[:, dc // 2, (dc % 2) * 256:(dc % 2) * 256 + n],
                                                              scalar=modT[:, l, 40 + dc, mc:mc + 1], in1=hb[:, dc, :n],
                                                              op0=ALU.mult, op1=ALU.add), reads=[big4, modT, hb], writes=[hn])
                if last:
                    kb.dma('sp', out_hT[dc * 128:(dc + 1) * 128, t0:t0 + n], hn[:, :n], reads=[hn])
                else:
                    kb.dma('sp', hT[dc * 128:(dc + 1) * 128, t0:t0 + n], hn[:, :n], reads=[hn])
        kb.pop("C%d" % l)

    return kb.finish(), kb


def _fmaj(v):
    v = np.asarray(v, np.float32)
    lead = v.shape[:-1]
    r = v.reshape(lead + (KD, 128))
    r = np.moveaxis(r, -1, 0)
    return np.ascontiguousarray(r)


def prep_inputs(S, CT, NL, b, x, c, ctx, c_ctx, norm1_g, norm2_g, w_mod, b_mod, w_in, q_norm_g, k_norm_g,
                lam_vecs, attn_norm_g, pool_w, pool_scale, conv_w, conv_out_w, w_out, peer_wq, peer_keys, peer_u, peer_v,
                shared=None):
    f = np.float32
    if shared is None:
        cos, sin = rope_tabs(S, CT)
        shared = {
            "w_mod": np.ascontiguousarray(w_mod, f),
            "b_modT": np.ascontiguousarray(np.asarray(b_mod, f).reshape(NL, 48, 128).transpose(2, 0, 1)),
            "g1T": _fmaj(norm1_g), "g2T": _fmaj(norm2_g),
            "w_in": np.ascontiguousarray(w_in, f),
            "qk_g": np.ascontiguousarray(np.stack([np.tile(np.asarray(q_norm_g, f), (1, 2)), np.tile(np.asarray(k_norm_g, f), (1, 2))], -1).transpose(1, 0, 2)),
            "lam_v": np.ascontiguousarray(np.asarray(lam_vecs, f).reshape(NL, 256)),
            "attn_g": np.ascontiguousarray(attn_norm_g, f),
            "pool_w": np.ascontiguousarray(np.asarray(pool_w, f).reshape(NL, D, 256)),
            "pscT": _fmaj(pool_scale),
            "cwT": np.ascontiguousarray(np.moveaxis(_fmaj(conv_w), 2, 3)),
            "conv_out_w": np.ascontiguousarray(conv_out_w, f),
            "w_out": np.ascontiguousarray(w_out, f),
            "peer_wq": np.ascontiguousarray(peer_wq, f),
            "keysT": np.ascontiguousarray(np.asarray(peer_keys, f).reshape(NL, 16, 128, 128).transpose(0, 3, 1, 2).reshape(NL, 128, 2048)),
            "peer_uL": np.ascontiguousarray(np.asarray(peer_u, f).reshape(NL, 128, 128, KD, 128).transpose(0, 1, 4, 3, 2).reshape(NL, N_EXP, D)),
            "peer_v": np.ascontiguousarray(peer_v, f),
            "c_iota128": np.ascontiguousarray(np.tile(np.arange(128, dtype=f), (128, 1))),
            "cosT": cos, "sinT": sin,
            "c_rot": rot_mat(), "c_bd": bd_mat(), "c_idf": np.eye(128, dtype=f),
            "c_band": band_mats(), "c_scat": scat_mats(),
            "c_iota": np.ascontiguousarray(np.tile(np.arange(16, dtype=f), (128, 1))),
        }
    m = dict(shared)
    m["hT0"] = np.ascontiguousarray(np.concatenate([np.asarray(x[b], f).T, np.asarray(ctx[b], f).T], axis=1))
    m["cvec"] = np.ascontiguousarray(np.stack([_fmaj(c[b]), _fmaj(c_ctx)], -1))
    return m, shared


_CACHE = {}


def kernel(**inputs):
    x = np.asarray(inputs["x"])
    B, S, _ = x.shape
    CT = inputs["ctx"].shape[1]
    NL = inputs["w_mod"].shape[0]
    key = (S, CT, NL)
    if key not in _CACHE:
        _CACHE[key] = build(S, CT, NL)[0]
    nc = _CACHE[key]
    in_maps = []
    shared = None
    for core in range(8):
        m, shared = prep_inputs(S, CT, NL, core % B, shared=shared, **inputs)
        in_maps.append(m)
    res = run_bass_kernel_spmd(nc, in_maps, core_ids=list(range(8)))
    out = np.stack([np.ascontiguousarray(np.asarray(res.results[b]["out_hT"]).T) for b in range(B)], 0)
    return out.astype(np.float32)
```

```python
import math
from contextlib import ExitStack
import numpy as np
import ml_dtypes
import concourse.bass as bass
import concourse.mybir as mybir
from concourse.bass_utils import run_bass_kernel_spmd

F32 = mybir.dt.float32
BF16 = mybir.dt.bfloat16
I32 = mybir.dt.int32
U32 = mybir.dt.uint32
ALU = mybir.AluOpType
AF = mybir.ActivationFunctionType
AX = mybir.AxisListType

D = 1024
KD = 8
EPS = 1e-6
Q_OFF, K_OFF, V_OFF, POOL_OFF, CIN_OFF, CB_OFF, CC_OFF, GATE_OFF = 0, 1024, 2048, 3072, 4096, 5120, 6144, 7168
IN_W = 10240
POOL_SIZES = (2, 4, 8, 16)
N_EXP = 16384


class Buf:
    _n = 0

    def __init__(self, t, name):
        self.t = t
        self.name = name
        self.last_w = None
        self.readers = []
        self.dsem = None
        Buf._n += 1
        self.id = Buf._n

    def __getitem__(self, idx):
        return self.t[idx]


class KB:
    ENG = ('pe', 'act', 'dve', 'pool', 'sp')

    def __init__(self, n_dsem=96):
        self.nc = bass.Bass("TRN2", target_bir_lowering=False)
        nc = self.nc
        self.es = ExitStack()
        self.E = {'pe': nc.tensor, 'act': nc.scalar, 'dve': nc.vector, 'pool': nc.gpsimd, 'sp': nc.sync}
        self.sems = {}
        self.cnt = {}
        for e in self.ENG:
            self.sems[('e', e)] = self.es.enter_context(nc.semaphore("p_" + e))
            self.cnt[('e', e)] = 0
        self.free_dsems = []
        for i in range(n_dsem):
            k = ('d', i)
            self.sems[k] = self.es.enter_context(nc.semaphore("d_%d" % i))
            self.cnt[k] = 0
            self.free_dsems.append(k)
        self.seen = {}
        self.n_ins = 0
        self.stack = [(self.es, [])]

    def push(self):
        self.stack.append((ExitStack(), []))

    def pop(self, tag=None):
        if tag is not None:
            self.log = getattr(self, "log", [])
            self.log.append((tag, self.n_ins))
        self.barrier()
        es, bufs = self.stack.pop()
        for b in bufs:
            if b.dsem is not None:
                self.free_dsems.append(b.dsem)
        es.close()

    def sbuf(self, name, shape, dt):
        es, bufs = self.stack[-1]
        t = es.enter_context(self.nc.sbuf_tensor(name + "_%d" % Buf._n, list(shape), dt))
        b = Buf(t, name)
        bufs.append(b)
        return b

    def psum(self, name, shape, dt=F32):
        es, bufs = self.stack[-1]
        t = es.enter_context(self.nc.psum_tensor(name + "_%d" % Buf._n, list(shape), dt))
        b = Buf(t, name)
        bufs.append(b)
        return b

    def _wait(self, eng, key, val):
        k = (eng, key)
        if self.seen.get(k, 0) >= val:
            return
        self.E[eng].wait_ge(self.sems[key], val)
        self.seen[k] = val

    def _deps(self, eng, reads, writes, is_dma=False):
        deps = {}
        me = ('e', eng)

        def add(st):
            key, val = st
            if deps.get(key, 0) < val:
                deps[key] = val
        for b in reads:
            if b.last_w is not None:
                if b.last_w[0] == me and eng == 'pe' and not is_dma:
                    continue
                add(b.last_w)
        for b in writes:
            if b.last_w is not None and (is_dma or b.last_w[0] != me):
                add(b.last_w)
            for r in b.readers:
                if r[0] == me and not is_dma:
                    continue
                add(r)
        for key, val in deps.items():
            self._wait(eng, key, val)

    def _stamp(self, st, reads, writes):
        for b in reads:
            b.readers.append(st)
            if len(b.readers) > 16:
                m = {}
                for k, v in b.readers:
                    if m.get(k, 0) < v:
                        m[k] = v
                b.readers = list(m.items())
        for b in writes:
            b.last_w = st
            b.readers = []

    def op(self, eng, fn, reads=(), writes=()):
        self._deps(eng, reads, writes)
        ins = fn(self.E[eng])
        key = ('e', eng)
        self.cnt[key] += 1
        ins.then_inc(self.sems[key], 1)
        self._stamp((key, self.cnt[key]), reads, writes)
        self.n_ins += 1
        return ins

    def dma(self, q, out, in_, reads=(), writes=(), indirect=None):
        self._deps(q, reads, writes, is_dma=True)
        b = (list(writes) + list(reads))[0]
        if b.dsem is None:
            b.dsem = self.free_dsems.pop(0)
        if indirect is None:
            ins = self.E[q].dma_start(out=out, in_=in_)
        else:
            ins = self.E[q].indirect_dma_start(out=out, out_offset=None, in_=in_, in_offset=indirect)
        self.cnt[b.dsem] += 16
        ins.then_inc(self.sems[b.dsem], 16)
        self._stamp((b.dsem, self.cnt[b.dsem]), reads, writes)
        self.n_ins += 1
        return ins

    def barrier(self):
        for f in self.ENG:
            for key, val in self.cnt.items():
                if val > 0:
                    self._wait(f, key, val)

    def finish(self):
        self.barrier()
        while len(self.stack) > 1:
            self.pop()
        return self.nc


class Ring:
    def __init__(self, bufs):
        self.bufs = bufs
        self.i = 0

    def next(self):
        b = self.bufs[self.i % len(self.bufs)]
        self.i += 1
        return b


def band_mats():
    out = np.zeros((128, 20, 128), np.float32)
    for wi, w in enumerate(POOL_SIZES):
        lo = w // 2
        hi = w - 1 - lo
        for t in range(128):
            for tp in range(-128, 256):
                if t - lo <= tp <= t + hi:
                    if tp < 0:
                        out[tp + 128, wi * 5 + 0, t] = 1.0 / w
                    elif tp < 128:
                        out[tp, wi * 5 + 1, t] = 1.0 / w
                    else:
                        out[tp - 128, wi * 5 + 2, t] = 1.0 / w
            s0, e0 = max(t - lo, 0), t + hi
            cnt = e0 - s0 + 1
            for tp in range(s0, min(e0, 127) + 1):
                out[tp, wi * 5 + 3, t] = 1.0 / cnt
            s1, e1 = t - lo, min(t + hi, 127)
            cnt = e1 - s1 + 1
            for tp in range(max(s1, 0), e1 + 1):
                out[tp, wi * 5 + 4, t] = 1.0 / cnt
        for kind in (1, 3, 4):
            out[:, wi * 5 + kind, :] -= np.eye(128, dtype=np.float32)
    return out


def scat_mats():
    out = np.zeros((128, 3, 384), np.float32)
    for t in range(128):
        if t - 1 >= 0:
            out[t - 1, 1, 0 * 128 + t] = 1.0
        else:
            out[127, 0, 0 * 128 + t] = 1.0
        out[t, 1, 1 * 128 + t] = 1.0
        if t + 1 < 128:
            out[t + 1, 1, 2 * 128 + t] = 1.0
        else:
            out[0, 2, 2 * 128 + t] = 1.0
    return out


def rope_tabs(S, CT):
    n_rows = S // 64
    rows = np.repeat(np.arange(n_rows, dtype=np.float32), 64)
    cols = np.tile(np.arange(64, dtype=np.float32), n_rows)
    inv = (10000.0 ** (-np.arange(16, dtype=np.float32) / 16)).astype(np.float32)
    ang = np.concatenate([rows[:, None] * inv, cols[:, None] * inv], axis=-1)
    cos = np.concatenate([np.cos(ang), np.ones((CT, 32), np.float32)], 0).astype(np.float32)
    sin = np.concatenate([np.sin(ang), np.zeros((CT, 32), np.float32)], 0).astype(np.float32)
    idx = np.arange(128) % 32
    return np.ascontiguousarray(cos[:, idx].T), np.ascontiguousarray(sin[:, idx].T)


def rot_mat():
    R = np.zeros((128, 128), np.float32)
    for p in range(128):
        if (p % 64) < 32:
            R[p + 32, p] = -1.0
        else:
            R[p - 32, p] = 1.0
    return R


def bd_mat():
    M = np.zeros((128, 128), np.float32)
    M[:64, :64] = 1.0 / 64
    M[64:, 64:] = 1.0 / 64
    return M


def build(S, CT, NL, dbg=False):
    kb = KB()
    nc = kb.nc
    T = S + CT
    NT = T // 128
    blocks = [(i * 512, 512) for i in range(S // 512)] + [(S, CT)]
    n_lat_blocks = S // 512
    SB = 256
    sblocks = [(i * SB, SB) for i in range(S // SB)] + [(S, CT)]
    n_lat_sblocks = S // SB

    def din(name, shape, dt=F32):
        return nc.dram_tensor(name, list(shape), dt, kind="ExternalInput").ap()

    def dscr(name, shape, dt):
        return nc.dram_tensor(name, list(shape), dt, kind="ExternalOutput" if dbg else "Internal").ap()

    hT0 = din("hT0", [D, T])
    cvec = din("cvec", [128, KD, 2])
    w_mod = din("w_mod", [NL, D, 6 * D])
    b_modT = din("b_modT", [128, NL, 48])
    g1T = din("g1T", [128, NL, KD])
    g2T = din("g2T", [128, NL, KD])
    w_in = din("w_in", [NL, D, IN_W])
    qk_g = din("qk_g", [128, NL, 2])
    lam_v = din("lam_v", [NL, 256])
    attn_g = din("attn_g", [NL, 128])
    pool_w = din("pool_w", [NL, D, 256])
    pscT = din("pscT", [128, NL, KD])
    cwT = din("cwT", [128, NL, KD, 3])
    conv_out_w = din("conv_out_w", [NL, D, D])
    w_out = din("w_out", [NL, D, D])
    peer_wq = din("peer_wq", [NL, D, 2048])
    keysT = din("keysT", [NL, 128, 2048])
    peer_uL = din("peer_uL", [NL, N_EXP, D])
    peer_v = din("peer_v", [NL, N_EXP, D])
    c_iota128 = din("c_iota128", [128, 128])
    cosT = din("cosT", [128, T])
    sinT = din("sinT", [128, T])
    c_rot = din("c_rot", [128, 128])
    c_bd = din("c_bd", [128, 128])
    c_idf = din("c_idf", [128, 128])
    c_band = din("c_band", [128, 20, 128])
    c_scat = din("c_scat", [128, 3, 384])
    c_iota = din("c_iota", [128, 16])
    out_hT = nc.dram_tensor("out_hT", [D, S], F32, kind="ExternalOutput").ap()

    hT = dscr("s_hT", [D, T], F32)
    xnT = dscr("s_xnT", [D, T], BF16)
    qT = dscr("s_qT", [D, T], BF16)
    kT = dscr("s_kT", [D, T], BF16)
    Vt = dscr("s_Vt", [T, D], BF16)
    uh = dscr("s_uh", [T, 2 * D], BF16)
    attnT = dscr("s_attnT", [D, T], BF16)
    w_in_b = dscr("s_w_in_b", [D, IN_W], BF16)
    co_b = dscr("s_co_b", [D, D], BF16)
    wo_b = dscr("s_wo_b", [D, D], BF16)
    pw_b = dscr("s_pw_b", [D, 256], BF16)
    wq_b = dscr("s_wq_b", [D, 2048], BF16)
    keys_b = dscr("s_keys_b", [128, 2048], BF16)
    uT_b = dscr("s_uT_b", [N_EXP, D], BF16)
    v_b = dscr("s_v_b", [N_EXP, D], BF16)

    def fm(ap2d, c0, n):
        return ap2d[:, c0:c0 + n].rearrange("(k p) t -> p k t", p=128)

    ones_m = kb.sbuf("ones_m", [128, 128], F32)
    rot_s = kb.sbuf("rot_s", [128, 128], F32)
    bd_s = kb.sbuf("bd_s", [128, 128], F32)
    idf = kb.sbuf("idf", [128, 128], F32)
    idb = kb.sbuf("idb", [128, 128], BF16)
    modT = kb.sbuf("modT", [128, NL, 48, 2], F32)
    A1 = kb.sbuf("A1", [128, NL, KD, 2], F32)
    A2 = kb.sbuf("A2", [128, NL, KD, 2], F32)
    g1s = kb.sbuf("g1s", [128, NL, KD], F32)
    g2s = kb.sbuf("g2s", [128, NL, KD], F32)
    qkg = kb.sbuf("qkg", [128, NL, 2], F32)
    psc = kb.sbuf("psc", [128, NL, KD], F32)
    cws = kb.sbuf("cws", [128, NL, KD, 3], F32)
    iota16 = kb.sbuf("iota16", [128, 16], F32)
    iota128 = kb.sbuf("iota128", [128, 128], F32)

    kb.op('dve', lambda e: e.memset(ones_m[:], 1.0 / D), writes=[ones_m])
    for sb, src in ((rot_s, c_rot), (bd_s, c_bd), (idf, c_idf), (g1s, g1T), (g2s, g2T), (qkg, qk_g),
                    (psc, pscT), (cws, cwT), (iota16, c_iota), (iota128, c_iota128)):
        kb.dma('sp', sb[:], src, writes=[sb])
    kb.op('dve', lambda e: e.tensor_copy(out=idb[:], in_=idf[:]), reads=[idf], writes=[idb])
    band = kb.sbuf("band", [128, 20, 128], BF16)
    scat = kb.sbuf("scat", [128, 3, 384], BF16)
    kb.push()
    bandf = kb.sbuf("bandf", [128, 20, 128], F32)
    scatf = kb.sbuf("scatf", [128, 3, 384], F32)
    kb.dma('sp', bandf[:], c_band, writes=[bandf])
    kb.dma('sp', scatf[:], c_scat, writes=[scatf])
    kb.op('dve', lambda e: e.tensor_copy(out=band[:], in_=bandf[:]), reads=[bandf], writes=[band])
    kb.op('dve', lambda e: e.tensor_copy(out=scat[:], in_=scatf[:]), reads=[scatf], writes=[scat])
    kb.pop()

    kb.push()
    stg = Ring([kb.sbuf("cp%d" % i, [128, KD, 512], F32) for i in range(2)])
    for (t0, n) in blocks:
        b = stg.next()
        kb.dma('sp', b[:, :, :n], fm(hT0, t0, n), writes=[b])
        kb.dma('sp', fm(hT, t0, n), b[:, :, :n], reads=[b])
    sc = kb.sbuf("sc", [128, KD, 2], F32)
    scs = kb.sbuf("scs", [128, KD, 2], F32)
    bm = kb.sbuf("bm", [128, NL, 48], F32)
    kb.dma('sp', sc[:], cvec, writes=[sc])
    kb.dma('sp', bm[:], b_modT, writes=[bm])
    kb.op('act', lambda e: e.activation(out=scs[:], in_=sc[:], func=AF.Silu), reads=[sc], writes=[scs])
    wmr = Ring([kb.sbuf("wm%d" % i, [128, KD, 512], F32) for i in range(2)])
    mps = kb.psum("mps", [128, 48, 2])
    for l in range(NL):
        for jt in range(12):
            wm = wmr.next()
            kb.dma('sp', wm[:], fm(w_mod[l], jt * 512, 512), writes=[wm])
            for jj in range(4):
                j = jt * 4 + jj
                for k in range(KD):
                    kb.op('pe', lambda e: e.matmul(mps[:, j, :], lhsT=wm[:, k, jj * 128:(jj + 1) * 128],
                                                   rhs=scs[:, k, :], start=(k == 0), stop=(k == KD - 1)),
                          reads=[wm, scs], writes=[mps])
        kb.op('dve', lambda e: e.tensor_tensor(out=modT[:, l], in0=mps[:],
                                               in1=bm[:, l, :].unsqueeze(2).to_broadcast([128, 48, 2]), op=ALU.add),
              reads=[mps, bm], writes=[modT])
        kb.op('dve', lambda e: e.scalar_tensor_tensor(
            out=A1[:, l], in0=modT[:, l, 8:16, :], scalar=1.0,
            in1=g1s[:, l, :].unsqueeze(2).to_broadcast([128, KD, 2]), op0=ALU.add, op1=ALU.mult),
            reads=[modT, g1s], writes=[A1])
        kb.op('dve', lambda e: e.scalar_tensor_tensor(
            out=A2[:, l], in0=modT[:, l, 32:40, :], scalar=1.0,
            in1=g2s[:, l, :].unsqueeze(2).to_broadcast([128, KD, 2]), op0=ALU.add, op1=ALU.mult),
            reads=[modT, g2s], writes=[A2])
    kb.pop()

    def convert(src2d, dst2d, R, C):
        kb.push()
        sf = Ring([kb.sbuf("cvf%d" % i, [128, 2048], F32) for i in range(3)])
        sb_ = Ring([kb.sbuf("cvb%d" % i, [128, 2048], BF16) for i in range(3)])
        engs = ['pool', 'act', 'dve']
        i = 0
        for r0 in range(0, R, 128):
            for c0 in range(0, C, 2048):
                cw = min(2048, C - c0)
                a = sf.next()
                b = sb_.next()
                kb.dma('sp', a[:, :cw], src2d[r0:r0 + 128, c0:c0 + cw], writes=[a])
                eng = engs[i % 3]
                i += 1
                if eng == 'act':
                    kb.op('act', lambda e: e.copy(out=b[:, :cw], in_=a[:, :cw]), reads=[a], writes=[b])
                else:
                    kb.op(eng, lambda e: e.tensor_copy(out=b[:, :cw], in_=a[:, :cw]), reads=[a], writes=[b])
                kb.dma('act', dst2d[r0:r0 + 128, c0:c0 + cw], b[:, :cw], reads=[b])
        kb.pop()

    def rsqrt_eps(ob, oap, ib, iap, eps):
        kb.op('dve', lambda e: e.tensor_scalar(out=oap, in0=iap, scalar1=eps, scalar2=None, op0=ALU.add), reads=[ib], writes=[ob])
        kb.op('act', lambda e: e.activation(out=oap, in_=oap, func=AF.Sqrt), reads=[ob], writes=[ob])
        kb.op('dve', lambda e: e.reciprocal(out=oap, in_=oap), reads=[ob], writes=[ob])

    def norm_block(hb, n, Acol, Bcol, outs, sqr, psn, rs, tmpr):
        for k in range(KD):
            sq = sqr.next()
            kb.op('act', lambda e: e.activation(out=sq[:, :n], in_=hb[:, k, :n], func=AF.Square), reads=[hb], writes=[sq])
            kb.op('pe', lambda e: e.matmul(psn[:, :n], lhsT=ones_m[:], rhs=sq[:, :n], start=(k == 0), stop=(k == KD - 1)),
                  reads=[ones_m, sq], writes=[psn])
        rsqrt_eps(rs, rs[:, :n], psn, psn[:, :n], EPS)
        for k in range(KD):
            tmp = tmpr.next()
            kb.op('dve', lambda e: e.scalar_tensor_tensor(out=tmp[:, :n], in0=hb[:, k, :n], scalar=Acol(k),
                                                          in1=rs[:, :n], op0=ALU.mult, op1=ALU.mult),
                  reads=[hb, rs, A1, A2], writes=[tmp])
            for ob in outs:
                kb.op('pool', lambda e: e.tensor_scalar(out=ob[:, k, :n], in0=tmp[:, :n], scalar1=Bcol(k), scalar2=None,
                                                        op0=ALU.add), reads=[tmp, modT], writes=[ob])

    for l in range(NL):
        last = (l == NL - 1)
        lam_init = 0.8 - 0.6 * math.exp(-0.3 * l)
        act_blocks = blocks[:n_lat_blocks] if last else blocks
        act_sblocks = sblocks[:n_lat_sblocks] if last else sblocks

        convert(w_in[l], w_in_b, D, IN_W)
        convert(conv_out_w[l], co_b, D, D)
        convert(w_out[l], wo_b, D, D)
        convert(pool_w[l], pw_b, D, 256)
        convert(peer_wq[l], wq_b, D, 2048)
        convert(keysT[l], keys_b, 128, 2048)
        convert(peer_uL[l], uT_b, N_EXP, D)
        convert(peer_v[l], v_b, N_EXP, D)

        kb.push()
        hb = kb.sbuf("hb", [128, KD, 512], F32)
        xn = kb.sbuf("xn", [128, KD, 512], BF16)
        sqr = Ring([kb.sbuf("sq%d" % i, [128, 512], F32) for i in range(2)])
        tmpr = Ring([kb.sbuf("tmp%d" % i, [128, 512], F32) for i in range(2)])
        rs = kb.sbuf("rs", [128, 512], F32)
        rs2 = kb.sbuf("rs2", [128, 512], F32)
        qn = kb.sbuf("qn", [128, 512], F32)
        t1 = kb.sbuf("t1", [128, 512], F32)
        t2 = kb.sbuf("t2", [128, 512], F32)
        cosb = kb.sbuf("cosb", [128, 512], F32)
        sinb = kb.sbuf("sinb", [128, 512], F32)
        qor = Ring([kb.sbuf("qo%d" % i, [128, 512], BF16) for i in range(2)])
        wr = Ring([kb.sbuf("wA%d" % i, [128, KD, 512], BF16) for i in range(3)])
        vbr = Ring([kb.sbuf("vb%d" % i, [128, 512], BF16) for i in range(2)])
        c1r = Ring([kb.sbuf("c1%d" % i, [128, 512], F32) for i in range(2)])
        psn = kb.psum("psn", [128, 512])
        psr = Ring([kb.psum("psA%d" % i, [128, 512]) for i in range(3)])
        ps2 = kb.psum("ps2", [128, 512])
        ps3 = kb.psum("ps3", [128, 512])
        psc2 = kb.psum("psc2", [128, 512])

        def loadw(col0):
            w = wr.next()
            kb.dma('sp', w[:], fm(w_in_b, col0, 512), writes=[w])
            return w

        for bi, (t0, n) in enumerate(blocks):
            nt = n // 128
            mc = 0 if bi < n_lat_blocks else 1
            kb.dma('sp', hb[:, :, :n], fm(hT, t0, n), writes=[hb])
            kb.dma('sp', cosb[:, :n], cosT[:, t0:t0 + n], writes=[cosb])
            kb.dma('sp', sinb[:, :n], sinT[:, t0:t0 + n], writes=[sinb])
            norm_block(hb, n, lambda k: A1[:, l, k, mc:mc + 1], lambda k: modT[:, l, k, mc:mc + 1], [xn],
                       sqr, psn, rs, tmpr)
            kb.dma('sp', fm(xnT, t0, n), xn[:, :, :n], reads=[xn])
            for (coloff, dst, gcol) in ((Q_OFF, qT, 0), (K_OFF, kT, 1)):
                for wt in range(2):
                    w = loadw(coloff + wt * 512)
                    for hh in range(4):
                        h = wt * 4 + hh
                        ps = psr.next()
                        for k in range(KD):
                            kb.op('pe', lambda e: e.matmul(ps[:, :n], lhsT=w[:, k, hh * 128:(hh + 1) * 128], rhs=xn[:, k, :n],
                                                           start=(k == 0), stop=(k == KD - 1)), reads=[w, xn], writes=[ps])
                        sq = sqr.next()
                        kb.op('act', lambda e: e.activation(out=sq[:, :n], in_=ps[:, :n], func=AF.Square), reads=[ps], writes=[sq])
                        kb.op('pe', lambda e: e.matmul(ps2[:, :n], lhsT=bd_s[:], rhs=sq[:, :n], start=True, stop=True),
                              reads=[bd_s, sq], writes=[ps2])
                        rsqrt_eps(rs2, rs2[:, :n], ps2, ps2[:, :n], EPS)
                        kb.op('dve', lambda e: e.scalar_tensor_tensor(out=qn[:, :n], in0=ps[:, :n], scalar=qkg[:, l, gcol:gcol + 1],
                                                                      in1=rs2[:, :n], op0=ALU.mult, op1=ALU.mult),
                              reads=[ps, rs2, qkg], writes=[qn])
                        kb.op('pe', lambda e: e.matmul(ps3[:, :n], lhsT=rot_s[:], rhs=qn[:, :n], start=True, stop=True),
                              reads=[rot_s, qn], writes=[ps3])
                        kb.op('pool', lambda e: e.tensor_tensor(out=t1[:, :n], in0=qn[:, :n], in1=cosb[:, :n], op=ALU.mult),
                              reads=[qn, cosb], writes=[t1])
                        kb.op('dve', lambda e: e.tensor_tensor(out=t2[:, :n], in0=ps3[:, :n], in1=sinb[:, :n], op=ALU.mult),
                              reads=[ps3, sinb], writes=[t2])
                        qo = qor.next()
                        kb.op('pool', lambda e: e.tensor_tensor(out=qo[:, :n], in0=t1[:, :n], in1=t2[:, :n], op=ALU.add),
                              reads=[t1, t2], writes=[qo])
                        kb.dma('sp', dst[h * 128:(h + 1) * 128, t0:t0 + n], qo[:, :n], reads=[qo])
            for (coloff, dstap, dcol) in ((V_OFF, Vt, 0), (POOL_OFF, uh, 0)):
                for wt in range(2):
                    w = loadw(coloff + wt * 512)
                    for ti in range(nt):
                        ps = psr.next()
                        for k in range(KD):
                            kb.op('pe', lambda e: e.matmul(ps[:], lhsT=xn[:, k, ti * 128:(ti + 1) * 128], rhs=w[:, k, :],
                                                           start=(k == 0), stop=(k == KD - 1)), reads=[w, xn], writes=[ps])
                        vb = vbr.next()
                        kb.op('act', lambda e: e.copy(out=vb[:], in_=ps[:]), reads=[ps], writes=[vb])
                        r0 = t0 + ti * 128
                        kb.dma('sp', dstap[r0:r0 + 128, dcol + wt * 512: dcol + (wt + 1) * 512], vb[:], reads=[vb])
            for wt in range(2):
                w1 = loadw(CIN_OFF + wt * 512)
                w2 = loadw(CC_OFF + wt * 512)
                for ti in range(nt):
                    ps = psr.next()
                    for k in range(KD):
                        kb.op('pe', lambda e: e.matmul(ps[:], lhsT=xn[:, k, ti * 128:(ti + 1) * 128], rhs=w1[:, k, :],
                                                       start=(k == 0), stop=(k == KD - 1)), reads=[w1, xn], writes=[ps])
                    for k in range(KD):
                        kb.op('pe', lambda e: e.matmul(psc2[:], lhsT=xn[:, k, ti * 128:(ti + 1) * 128], rhs=w2[:, k, :],
                                                       start=(k == 0), stop=(k == KD - 1)), reads=[w2, xn], writes=[psc2])
                    c1 = c1r.next()
                    kb.op('act', lambda e: e.copy(out=c1[:], in_=ps[:]), reads=[ps], writes=[c1])
                    vb = vbr.next()
                    kb.op('dve', lambda e: e.tensor_tensor(out=vb[:], in0=psc2[:], in1=c1[:], op=ALU.mult),
                          reads=[psc2, c1], writes=[vb])
                    r0 = t0 + ti * 128
                    kb.dma('sp', uh[r0:r0 + 128, D + wt * 512: D + (wt + 1) * 512], vb[:], reads=[vb])
        kb.pop("A%d" % l)

        kb.push()
        lvb = kb.sbuf("lvb", [128, 256], F32)
        lprod = kb.sbuf("lprod", [128, 128], F32)
        lsum = kb.sbuf("lsum", [128, 2], F32)
        lexp = kb.sbuf("lexp", [128, 2], F32)
        nlam = kb.sbuf("nlam", [128, 1], F32)
        gvT = kb.sbuf("gvT", [128, 1], F32)
        ones1 = kb.sbuf("ones1", [128, 128], F32)
        kb.op('pool', lambda e: e.memset(ones1[:], 1.0), writes=[ones1])
        kb.dma('sp', lvb[:], lam_v[l:l + 1, :].partition_broadcast(128), writes=[lvb])
        kb.dma('sp', gvT[:], attn_g[l:l + 1, :].rearrange("o e -> e o"), writes=[gvT])
        lv4 = lvb[:].rearrange("p (a d) -> p a d", a=4)
        kb.op('dve', lambda e: e.tensor_tensor(out=lprod[:, 0:64], in0=lv4[:, 0, :], in1=lv4[:, 1, :], op=ALU.mult),
              reads=[lvb], writes=[lprod])
        kb.op('dve', lambda e: e.tensor_tensor(out=lprod[:, 64:128], in0=lv4[:, 2, :], in1=lv4[:, 3, :], op=ALU.mult),
              reads=[lvb, lprod], writes=[lprod])
        kb.op('dve', lambda e: e.tensor_reduce(out=lsum[:], in_=lprod[:].rearrange("p (a d) -> p a d", a=2), axis=AX.X, op=ALU.add),
              reads=[lprod], writes=[lsum])
        kb.op('act', lambda e: e.activation(out=lexp[:], in_=lsum[:], func=AF.Exp), reads=[lsum], writes=[lexp])
        kb.op('dve', lambda e: e.tensor_tensor(out=nlam[:], in0=lexp[:, 1:2], in1=lexp[:, 0:1], op=ALU.subtract),
              reads=[lexp], writes=[nlam])
        kb.op('dve', lambda e: e.tensor_scalar(out=nlam[:], in0=nlam[:], scalar1=-lam_init, scalar2=None, op0=ALU.add),
              reads=[nlam], writes=[nlam])
        kb.op('dve', lambda e: e.tensor_scalar(out=gvT[:], in0=gvT[:], scalar1=math.sqrt(128.0) * (1.0 - lam_init),
                                               scalar2=None, op0=ALU.mult), reads=[gvT], writes=[gvT])
        kTr = Ring([kb.sbuf("kTh%d" % i, [128, T], BF16) for i in range(2)])
        qTr = Ring([kb.sbuf("qTh%d" % i, [128, T], BF16) for i in range(2)])
        var = Ring([kb.sbuf("vah%d" % i, [128, NT, 128], BF16) for i in range(2)])
        ptr = Ring([kb.sbuf("pt%d" % i, [128, 1024], BF16) for i in range(4)])
        zacc = kb.sbuf("zacc", [128, 1024], F32)
        zaccP = kb.sbuf("zaccP", [128, 1024], F32)
        rzb = kb.sbuf("rzb", [128, 1024], F32)
        osb = [kb.sbuf("osb%d" % i, [128, 1024], F32) for i in range(2)]
        oc = kb.sbuf("oc", [128, 1024], F32)
        osq = kb.sbuf("osq", [128, 1024], F32)
        rsd = kb.sbuf("rsd", [128, 1024], F32)
        aor = Ring([kb.sbuf("ao%d" % i, [128, 1024], BF16) for i in range(2)])
        psS = Ring([kb.psum("pS%d" % i, [128, 2, 512]) for i in range(3)])
        psO = kb.psum("pO", [128, 2, 512])
        qblocks = [(g0, 512) for g0 in range(0, S, 512)]
        if not last:
            qblocks.append((S, CT))
        for h in range(8):
            kh = kTr.next()
            qh = qTr.next()
            va = var.next()
            kb.dma('sp', kh[:], kT[h * 128:(h + 1) * 128, :], writes=[kh])
            kb.dma('sp', qh[:], qT[h * 128:(h + 1) * 128, :], writes=[qh])
            for c0 in range(0, NT, 16):
                cn = min(16, NT - c0)
                kb.dma('sp', va[:, c0:c0 + cn, :],
                       Vt[c0 * 128:(c0 + cn) * 128, h * 128:(h + 1) * 128].rearrange("(c p) e -> p c e", p=128), writes=[va])
            for (tq, n) in qblocks:
                is_ctx = tq >= S
                kcs = list(range(S // 128, NT)) if is_ctx else list(range(NT))

                def emit_qk(kc):
                    ps = psS.next()
                    for s_ in range(2):
                        kb.op('pe', lambda e: e.matmul(ps[:, s_, :n], lhsT=kh[s_ * 64:(s_ + 1) * 64, kc * 128:(kc + 1) * 128],
                                                       rhs=qh[s_ * 64:(s_ + 1) * 64, tq:tq + n], start=True, stop=True),
                              reads=[kh, qh], writes=[ps])
                    return ps

                def emit_rest(ci, kc, ps):
                    pt = ptr.next()
                    ptv = pt[:].rearrange("p (b x) -> p b x", b=2)
                    kb.op('act', lambda e: e.activation(out=ptv[:, :, :n], in_=ps[:, :, :n], func=AF.Exp, scale=0.125),
                          reads=[ps], writes=[pt])
                    for s_ in range(2):
                        kb.op('pe', lambda e: e.matmul(psO[:, s_, :n], lhsT=va[:, kc, :], rhs=pt[:, s_ * 512:s_ * 512 + n],
                                                       start=(ci == 0), stop=(ci == len(kcs) - 1)), reads=[pt, va], writes=[psO])
                    zeng, zb = 'dve', zacc
                    first = (ci == 0)
                    zv = zb[:].rearrange("p (b x) -> p b x", b=2)
                    if first:
                        kb.op(zeng, lambda e: e.tensor_copy(out=zv[:, :, :n], in_=ptv[:, :, :n]), reads=[pt], writes=[zb])
                    else:
                        kb.op(zeng, lambda e: e.tensor_tensor(out=zv[:, :, :n], in0=zv[:, :, :n], in1=ptv[:, :, :n], op=ALU.add),
                              reads=[pt, zb], writes=[zb])
                pend = []
                for ci, kc in enumerate(kcs):
                    ps = emit_qk(kc)
                    pend.append((ci, kc, ps))
                    if len(pend) > 2:
                        emit_rest(*pend.pop(0))
                while pend:
                    emit_rest(*pend.pop(0))
                pzb = psS.next()
                for s_ in range(2):
                    sl = slice(s_ * 512, s_ * 512 + n)
                    use_p = False
                    kb.op('pe', lambda e: e.matmul(pzb[:, s_, :n], lhsT=ones1[:], rhs=zacc[:, sl], start=True, stop=(not use_p)),
                          reads=[ones1, zacc], writes=[pzb])
                    if use_p:
                        kb.op('pe', lambda e: e.matmul(pzb[:, s_, :n], lhsT=ones1[:], rhs=zaccP[:, sl], start=False, stop=True),
                              reads=[ones1, zaccP], writes=[pzb])
                    kb.op('dve', lambda e: e.reciprocal(out=rzb[:, sl], in_=pzb[:, s_, :n]), reads=[pzb], writes=[rzb])
                    kb.op('dve', lambda e: e.tensor_tensor(out=osb[s_][:, :n], in0=psO[:, s_, :n], in1=rzb[:, sl], op=ALU.mult),
                          reads=[psO, rzb], writes=[osb[s_]])
                ao = aor.next()
                sl = slice(0, n)
                kb.op('dve', lambda e: e.scalar_tensor_tensor(out=oc[:, sl], in0=osb[1][:, sl], scalar=nlam[:, 0:1], in1=osb[0][:, sl],
                                                              op0=ALU.mult, op1=ALU.add), reads=[osb[0], osb[1], nlam], writes=[oc])
                kb.op('act', lambda e: e.activation(out=osq[:, sl], in_=oc[:, sl], func=AF.Square), reads=[oc], writes=[osq])
                pz = psS.next()
                kb.op('pe', lambda e: e.matmul(pz[:, 0, :n], lhsT=ones1[:], rhs=osq[:, sl], start=True, stop=True),
                      reads=[ones1, osq], writes=[pz])
                rsqrt_eps(rsd, rsd[:, sl], pz, pz[:, 0, :n], 128.0 * EPS)
                kb.op('dve', lambda e: e.scalar_tensor_tensor(out=ao[:, sl], in0=oc[:, sl], scalar=gvT[:, 0:1], in1=rsd[:, sl],
                                                              op0=ALU.mult, op1=ALU.mult), reads=[oc, gvT, rsd], writes=[ao])
                kb.dma('sp', attnT[h * 128:(h + 1) * 128, tq:tq + n], ao[:, sl], reads=[ao])
        kb.pop("B1_%d" % l)

        kb.push()
        cow = kb.sbuf("cow", [128, KD, D], BF16)
        wow = kb.sbuf("wow", [128, KD, D], BF16)
        pww = kb.sbuf("pww", [128, KD, 256], BF16)
        kb.dma('sp', cow[:], fm(co_b, 0, D), writes=[cow])
        kb.dma('sp', wow[:], fm(wo_b, 0, D), writes=[wow])
        kb.dma('sp', pww[:], fm(pw_b, 0, 256), writes=[pww])
        xn = kb.sbuf("xnB", [128, KD, SB], BF16)
        uht = kb.sbuf("uht", [128, 4, 2 * D], BF16)
        att = kb.sbuf("att", [128, KD, SB], BF16)
        cbT = kb.sbuf("cbT", [128, KD, SB], BF16)
        plT = kb.sbuf("plT", [128, KD, SB], BF16)
        bcT = kb.sbuf("bcT", [128, KD, SB], BF16)
        mgT = kb.sbuf("mgT", [128, KD, SB], BF16)
        wcr = Ring([kb.sbuf("wcb%d" % i, [128, KD, 512], BF16) for i in range(2)])
        gwr = Ring([kb.sbuf("gw%d" % i, [128, KD, 3, 128], BF16) for i in range(2)])
        gTr = Ring([kb.sbuf("gT%d" % i, [128, 3, SB], F32) for i in range(2)])
        hcr = Ring([kb.sbuf("hcb%d" % i, [128, SB], F32) for i in range(2)])
        hnr = Ring([kb.sbuf("hnb%d" % i, [128, SB], F32) for i in range(2)])
        m0 = kb.sbuf("m0", [128, SB], F32)
        m1 = kb.sbuf("m1", [128, SB], F32)
        m2 = kb.sbuf("m2", [128, SB], F32)
        cacc = Ring([kb.sbuf("cacc%d" % i, [128, 128], F32) for i in range(2)])
        psr = Ring([kb.psum("psB%d" % i, [128, 512]) for i in range(4)])
        pcv = Ring([kb.psum("pcv%d" % i, [128, 512]) for i in range(2)])
        for bi, (t0, n) in enumerate(act_sblocks):
            nt = n // 128
            tile0 = t0 // 128
            is_ctx = bi >= n_lat_sblocks
            mc = 1 if is_ctx else 0
            seq_first = (tile0 == 0) or (tile0 == S // 128)
            seq_last_tile = (S // 128 - 1) if not is_ctx else (NT - 1)
            kb.dma('sp', xn[:, :, :n], fm(xnT, t0, n), writes=[xn])
            kb.dma('sp', att[:, :, :n], fm(attnT, t0, n), writes=[att])
            lo_t = tile0 if seq_first else tile0 - 1
            hi_t = min(tile0 + nt, seq_last_tile)
            kb.dma('sp', uht[:, lo_t - (tile0 - 1): hi_t - (tile0 - 1) + 1, :],
                   uh[lo_t * 128:(hi_t + 1) * 128, :].rearrange("(q p) e -> p q e", p=128), writes=[uht])
            for wt in range(2):
                w = wcr.next()
                kb.dma('sp', w[:], fm(w_in_b, CB_OFF + wt * 512, 512), writes=[w])
                for cc in range(4):
                    ps = psr.next()
                    for k in range(KD):
                        kb.op('pe', lambda e: e.matmul(ps[:, :n], lhsT=w[:, k, cc * 128:(cc + 1) * 128], rhs=xn[:, k, :n],
                                                       start=(k == 0), stop=(k == KD - 1)), reads=[w, xn], writes=[ps])
                    kb.op('act', lambda e: e.copy(out=cbT[:, wt * 4 + cc, :n], in_=ps[:, :n]), reads=[ps], writes=[cbT])
            for c in range(KD):
                wi = c // 2
                ps = psr.next()
                for ti in range(nt):
                    tg = tile0 + ti
                    first = (tg == 0) or (tg == S // 128)
                    lastt = (tg == seq_last_tile)
                    terms = []
                    if not first:
                        terms.append((ti, wi * 5 + 0))
                    terms.append((ti + 1, wi * 5 + (3 if first else (4 if lastt else 1))))
                    if not lastt:
                        terms.append((ti + 2, wi * 5 + 2))
                    for j, (slot, kind) in enumerate(terms):
                        kb.op('pe', lambda e: e.matmul(ps[:, ti * 128:(ti + 1) * 128], lhsT=uht[:, slot, c * 128:(c + 1) * 128],
                                                       rhs=band[:, kind, :], start=(j == 0), stop=(j == len(terms) - 1)),
                              reads=[uht, band], writes=[ps])
                kb.op('act', lambda e: e.copy(out=plT[:, c, :n], in_=ps[:, :n]), reads=[ps], writes=[plT])
            for c in range(KD):
                for ti in range(nt):
                    tg = tile0 + ti
                    first = (tg == 0) or (tg == S // 128)
                    lastt = (tg == seq_last_tile)
                    terms = [(ti + 1, 1)]
                    if not first:
                        terms.append((ti, 0))
                    if not lastt:
                        terms.append((ti + 2, 2))
                    pc = pcv.next()
                    for j, (slot, kind) in enumerate(terms):
                        kb.op('pe', lambda e: e.matmul(pc[:, 0:384], lhsT=uht[:, slot, D + c * 128: D + (c + 1) * 128],
                                                       rhs=scat[:, kind, :], start=(j == 0), stop=(j == len(terms) - 1)),
                              reads=[uht, scat], writes=[pc])
                    ca = cacc.next()
                    kb.op('dve', lambda e: e.tensor_scalar(out=ca[:], in0=pc[:, 0:128], scalar1=cws[:, l, c, 0:1], scalar2=None,
                                                           op0=ALU.mult), reads=[pc, cws], writes=[ca])
                    kb.op('dve', lambda e: e.scalar_tensor_tensor(out=ca[:], in0=pc[:, 128:256], scalar=cws[:, l, c, 1:2], in1=ca[:],
                                                                  op0=ALU.mult, op1=ALU.add), reads=[pc, cws, ca], writes=[ca])
                    kb.op('dve', lambda e: e.scalar_tensor_tensor(out=ca[:], in0=pc[:, 256:384], scalar=cws[:, l, c, 2:3], in1=ca[:],
                                                                  op0=ALU.mult, op1=ALU.add), reads=[pc, cws, ca], writes=[ca])
                    kb.op('pool', lambda e: e.tensor_tensor(out=bcT[:, c, ti * 128:(ti + 1) * 128], in0=ca[:],
                                                            in1=cbT[:, c, ti * 128:(ti + 1) * 128], op=ALU.mult),
                          reads=[ca, cbT], writes=[bcT])
            for dc in range(KD):
                gw = gwr.next()
                for j in range(3):
                    kb.dma('sp', gw[:, :, j, :], fm(w_in_b, GATE_OFF + j * D + dc * 128, 128), writes=[gw])
                gT = gTr.next()
                for j in range(3):
                    ps = psr.next()
                    for k in range(KD):
                        kb.op('pe', lambda e: e.matmul(ps[:, :n], lhsT=gw[:, k, j, :], rhs=xn[:, k, :n],
                                                       start=(k == 0), stop=(k == KD - 1)), reads=[gw, xn], writes=[ps])
                    kb.op('act', lambda e: e.activation(out=gT[:, j, :n], in_=ps[:, :n], func=AF.Sigmoid), reads=[ps], writes=[gT])
                kb.op('dve', lambda e: e.tensor_tensor(out=m0[:, :n], in0=att[:, dc, :n], in1=gT[:, 0, :n], op=ALU.mult),
                      reads=[att, gT], writes=[m0])
                ps = psr.next()
                g = dc // 2
                dh = dc % 2
                for kk in range(2):
                    kb.op('pe', lambda e: e.matmul(ps[:, :n], lhsT=pww[:, g * 2 + kk, dh * 128:(dh + 1) * 128], rhs=plT[:, g * 2 + kk, :n],
                                                   start=(kk == 0), stop=(kk == 1)), reads=[pww, plT], writes=[ps])
                kb.op('dve', lambda e: e.scalar_tensor_tensor(out=m1[:, :n], in0=ps[:, :n], scalar=psc[:, l, dc:dc + 1], in1=gT[:, 1, :n],
                                                              op0=ALU.mult, op1=ALU.mult), reads=[ps, psc, gT], writes=[m1])
                ps = psr.next()
                for k in range(KD):
                    kb.op('pe', lambda e: e.matmul(ps[:, :n], lhsT=cow[:, k, dc * 128:(dc + 1) * 128], rhs=bcT[:, k, :n],
                                                   start=(k == 0), stop=(k == KD - 1)), reads=[cow, bcT], writes=[ps])
                kb.op('dve', lambda e: e.tensor_tensor(out=m2[:, :n], in0=ps[:, :n], in1=gT[:, 2, :n], op=ALU.mult),
                      reads=[ps, gT], writes=[m2])
                kb.op('pool', lambda e: e.tensor_tensor(out=m0[:, :n], in0=m0[:, :n], in1=m1[:, :n], op=ALU.add),
                      reads=[m0, m1], writes=[m0])
                kb.op('pool', lambda e: e.tensor_tensor(out=mgT[:, dc, :n], in0=m0[:, :n], in1=m2[:, :n], op=ALU.add),
                      reads=[m0, m2], writes=[mgT])
            for dc in range(KD):
                ps = psr.next()
                for k in range(KD):
                    kb.op('pe', lambda e: e.matmul(ps[:, :n], lhsT=wow[:, k, dc * 128:(dc + 1) * 128], rhs=mgT[:, k, :n],
                                                   start=(k == 0), stop=(k == KD - 1)), reads=[wow, mgT], writes=[ps])
                hc_ = hcr.next()
                kb.dma('sp', hc_[:, :n], hT[dc * 128:(dc + 1) * 128, t0:t0 + n], writes=[hc_])
                hn = hnr.next()
                kb.op('dve', lambda e: e.scalar_tensor_tensor(out=hn[:, :n], in0=ps[:, :n], scalar=modT[:, l, 16 + dc, mc:mc + 1],
                                                              in1=hc_[:, :n], op0=ALU.mult, op1=ALU.add),
                      reads=[ps, modT, hc_], writes=[hn])
                kb.dma('sp', hT[dc * 128:(dc + 1) * 128, t0:t0 + n], hn[:, :n], reads=[hn])
        kb.pop("B2_%d" % l)

        kb.push()
        kys = kb.sbuf("kys", [128, 16, 128], BF16)
        kb.dma('sp', kys[:], keys_b.rearrange("p (g k) -> p g k", g=16), writes=[kys])
        hbs = [kb.sbuf("hbC%d" % i, [128, KD, SB], F32) for i in range(2)]
        xbs = [kb.sbuf("xbC%d" % i, [128, KD, SB], BF16) for i in range(2)]
        itTs = [kb.sbuf("itT%d" % i, [128, 3, SB], F32) for i in range(2)]
        sqr = Ring([kb.sbuf("sqC%d" % i, [128, SB], F32) for i in range(2)])
        tmpr = Ring([kb.sbuf("tmpC%d" % i, [128, SB], F32) for i in range(2)])
        rs = kb.sbuf("rsC", [128, SB], F32)
        wqr = Ring([kb.sbuf("wq%d" % i, [128, KD, 512], BF16) for i in range(2)])
        qp = kb.sbuf("qp", [128, 16, SB], BF16)
        bufA = kb.sbuf("bufA", [128, 2048], F32)
        bufB = kb.sbuf("bufB", [128, 2048], F32)
        stop_ = kb.sbuf("stop", [128, 16, 16], F32)
        itop = kb.sbuf("itop", [128, 16, 16], U32)
        itf = kb.sbuf("itf", [128, 16, 16], F32)
        tops = kb.sbuf("tops", [128, 8, 16], F32)
        pos = kb.sbuf("pos", [128, 8, 16], U32)
        pa = kb.sbuf("pa", [128, 8, 16], U32)
        pbb = kb.sbuf("pbb", [128, 8, 16], U32)
        paf = kb.sbuf("paf", [128, 8, 16], F32)
        pbf = kb.sbuf("pbf", [128, 8, 16], F32)
        sel3 = kb.sbuf("sel3", [128, 3, 128], F32)
        ex = kb.sbuf("ex", [128, 8, 16], F32)
        esum = kb.sbuf("esum", [128, 8], F32)
        Ar = Ring([kb.sbuf("Aoh%d" % i, [128, 16, 128], BF16) for i in range(2)])
        Br = Ring([kb.sbuf("Boh%d" % i, [128, 16, 128], BF16) for i in range(2)])
        GT = kb.sbuf("GT", [128, 128, SB], BF16)
        ucr = Ring([kb.sbuf("uc%d" % i, [128, 2, D], BF16) for i in range(2)])
        vcr = Ring([kb.sbuf("vc%d" % i, [128, 2, D], BF16) for i in range(2)])
        x2r = Ring([kb.sbuf("x2_%d" % i, [128, SB], F32) for i in range(2)])
        ttr = Ring([kb.sbuf("tt_%d" % i, [128, SB], F32) for i in range(2)])
        sgr = Ring([kb.sbuf("sg_%d" % i, [128, SB], F32) for i in range(2)])
        xgr = Ring([kb.sbuf("xg_%d" % i, [128, SB], F32) for i in range(2)])
        Wr = Ring([kb.sbuf("W_%d" % i, [128, SB], BF16) for i in range(2)])
        hnr = Ring([kb.sbuf("hnC%d" % i, [128, SB], F32) for i in range(2)])
        psn = kb.psum("psnC", [128, 512])
        psr_ = kb.psum("psrC", [128, 512])
        psq = Ring([kb.psum("psq%d" % i, [128, 512]) for i in range(2)])
        big4 = kb.psum("big4", [128, 4, 512])
        sS = bufA[:].rearrange("p (g k) -> p g k", g=16)
        sS2 = bufB[:].rearrange("p (g k) -> p g k", g=16)
        cand = bufA[:].rearrange("p (h c) -> p h c", h=8)
        cand2 = bufB[:].rearrange("p (h c) -> p h c", h=8)
        oh = bufB[:].rearrange("p (h k a) -> p h k a", h=8, k=16)
        GC = 1.5957691216057308

        def routing_gen(bi, t0, n):
            hb = hbs[bi % 2]
            xb = xbs[bi % 2]
            itT = itTs[bi % 2]
            nt = n // 128
            mc = 1 if bi >= n_lat_sblocks else 0
            kb.dma('sp', hb[:, :, :n], fm(hT, t0, n), writes=[hb])
            norm_block(hb, n, lambda k: A2[:, l, k, mc:mc + 1], lambda k: modT[:, l, 24 + k, mc:mc + 1], [xb],
                       sqr, psn, rs, tmpr)
            yield
            for wt in range(4):
                wq = wqr.next()
                kb.dma('sp', wq[:], fm(wq_b, wt * 512, 512), writes=[wq])
                for gg in range(4):
                    g = wt * 4 + gg
                    for k in range(KD):
                        kb.op('pe', lambda e: e.matmul(psr_[:, :n], lhsT=wq[:, k, gg * 128:(gg + 1) * 128], rhs=xb[:, k, :n],
                                                       start=(k == 0), stop=(k == KD - 1)), reads=[wq, xb], writes=[psr_])
                    kb.op('act', lambda e: e.copy(out=qp[:, g, :n], in_=psr_[:, :n]), reads=[psr_], writes=[qp])
                    yield
            for ti in range(nt):
                tsl = slice(ti * 128, (ti + 1) * 128)
                for a4 in range(4):
                    for gg in range(4):
                        g = a4 * 4 + gg
                        kb.op('pe', lambda e: e.matmul(psn[:, gg * 128:(gg + 1) * 128], lhsT=qp[:, g, tsl], rhs=kys[:, g, :],
                                                       start=True, stop=True), reads=[qp, kys], writes=[psn])
                    kb.op('act', lambda e: e.copy(out=bufA[:, a4 * 512:(a4 + 1) * 512], in_=psn[:]), reads=[psn], writes=[bufA])
                yield
                for g in range(16):
                    kb.op('dve', lambda e: e.max(out=stop_[:, g, 0:8], in_=sS[:, g, :]), reads=[bufA], writes=[stop_])
                    kb.op('dve', lambda e: e.max_index(out=itop[:, g, 0:8], in_max=stop_[:, g, 0:8], in_values=sS[:, g, :]),
                          reads=[bufA, stop_], writes=[itop])
                    kb.op('dve', lambda e: e.match_replace(out=sS2[:, g, :], in_to_replace=stop_[:, g, 0:8], in_values=sS[:, g, :],
                                                           imm_value=-1e30), reads=[bufA, stop_], writes=[bufB])
                    kb.op('dve', lambda e: e.max(out=stop_[:, g, 8:16], in_=sS2[:, g, :]), reads=[bufB], writes=[stop_])
                    kb.op('dve', lambda e: e.max_index(out=itop[:, g, 8:16], in_max=stop_[:, g, 8:16], in_values=sS2[:, g, :]),
                          reads=[bufB, stop_], writes=[itop])
                    yield
                kb.op('dve', lambda e: e.tensor_copy(out=itf[:], in_=itop[:]), reads=[itop], writes=[itf])
                s4 = stop_[:].rearrange("p (h i) a -> p h i a", i=2)
                kb.op('dve', lambda e: e.tensor_tensor(
                    out=cand.rearrange("p h (a b) -> p h a b", a=16),
                    in0=s4[:, :, 0, :].unsqueeze(3).to_broadcast([128, 8, 16, 16]),
                    in1=s4[:, :, 1, :].unsqueeze(2).to_broadcast([128, 8, 16, 16]), op=ALU.add), reads=[stop_], writes=[bufA])
                yield
                for h in range(8):
                    kb.op('dve', lambda e: e.max(out=tops[:, h, 0:8], in_=cand[:, h, :]), reads=[bufA], writes=[tops])
                    kb.op('dve', lambda e: e.max_index(out=pos[:, h, 0:8], in_max=tops[:, h, 0:8], in_values=cand[:, h, :]),
                          reads=[bufA, tops], writes=[pos])
                    kb.op('dve', lambda e: e.match_replace(out=cand2[:, h, :], in_to_replace=tops[:, h, 0:8], in_values=cand[:, h, :],
                                                           imm_value=-1e30), reads=[bufA, tops], writes=[bufB])
                    kb.op('dve', lambda e: e.max(out=tops[:, h, 8:16], in_=cand2[:, h, :]), reads=[bufB], writes=[tops])
                    kb.op('dve', lambda e: e.max_index(out=pos[:, h, 8:16], in_max=tops[:, h, 8:16], in_values=cand2[:, h, :]),
                          reads=[bufB, tops], writes=[pos])
                    yield
                kb.op('dve', lambda e: e.tensor_single_scalar(out=pa[:], in_=pos[:], scalar=4, op=ALU.logical_shift_right),
                      reads=[pos], writes=[pa])
                kb.op('dve', lambda e: e.tensor_single_scalar(out=pbb[:], in_=pos[:], scalar=15, op=ALU.bitwise_and),
                      reads=[pos], writes=[pbb])
                kb.op('dve', lambda e: e.tensor_copy(out=paf[:], in_=pa[:]), reads=[pa], writes=[paf])
                kb.op('dve', lambda e: e.tensor_copy(out=pbf[:], in_=pbb[:]), reads=[pbb], writes=[pbf])
                yield
                i4 = itf[:].rearrange("p (h i) a -> p h i a", i=2)
                for (pf, ii) in ((paf, 0), (pbf, 1)):
                    kb.op('dve', lambda e: e.tensor_tensor(
                        out=oh, in0=pf[:].unsqueeze(3).to_broadcast([128, 8, 16, 16]),
                        in1=iota16[:].unsqueeze(1).unsqueeze(1).to_broadcast([128, 8, 16, 16]), op=ALU.is_equal),
                        reads=[pf, iota16], writes=[bufB])
                    kb.op('dve', lambda e: e.tensor_tensor(
                        out=oh, in0=oh, in1=i4[:, :, ii, :].unsqueeze(2).to_broadcast([128, 8, 16, 16]), op=ALU.mult),
                        reads=[bufB, itf], writes=[bufB])
                    kb.op('dve', lambda e: e.tensor_reduce(out=sel3[:, ii, :].rearrange("p (h k) -> p h k", h=8), in_=oh, axis=AX.X, op=ALU.add),
                          reads=[bufB], writes=[sel3])
                    yield
                kb.op('dve', lambda e: e.tensor_tensor(out=ex[:], in0=tops[:], in1=tops[:, :, 0:1].to_broadcast([128, 8, 16]),
                                                       op=ALU.subtract), reads=[tops], writes=[ex])
                kb.op('act', lambda e: e.activation(out=ex[:], in_=ex[:], func=AF.Exp), reads=[ex], writes=[ex])
                kb.op('dve', lambda e: e.tensor_reduce(out=esum[:], in_=ex[:], axis=AX.X, op=ALU.add), reads=[ex], writes=[esum])
                kb.op('dve', lambda e: e.reciprocal(out=esum[:], in_=esum[:]), reads=[esum], writes=[esum])
                kb.op('dve', lambda e: e.tensor_tensor(out=sel3[:, 2, :].rearrange("p (h k) -> p h k", h=8), in0=ex[:],
                                                       in1=esum[:].unsqueeze(2).to_broadcast([128, 8, 16]), op=ALU.mult),
                      reads=[ex, esum], writes=[sel3])
                for x3 in range(3):
                    kb.op('pe', lambda e: e.transpose(psn[:, x3 * 128:(x3 + 1) * 128], sel3[:, x3, :], idf[:]), reads=[sel3, idf], writes=[psn])
                kb.op('act', lambda e: e.copy(out=itT[:, :, tsl], in_=psn[:, 0:384].rearrange("p (x t) -> p x t", x=3)), reads=[psn], writes=[itT])
                yield

        def g_build(bi, n):
            itT = itTs[bi % 2]
            for tg in range(0, n, 16):
                A_ = Ar.next()
                B_ = Br.next()
                io_b = iota128[:].unsqueeze(1).to_broadcast([128, 16, 128])
                kb.op('dve', lambda e: e.tensor_tensor(out=A_[:], in0=io_b, in1=itT[:, 0, tg:tg + 16].unsqueeze(2).to_broadcast([128, 16, 128]),
                                                       op=ALU.is_equal), reads=[iota128, itT], writes=[A_])
                kb.op('dve', lambda e: e.tensor_tensor(out=A_[:], in0=A_[:], in1=itT[:, 2, tg:tg + 16].unsqueeze(2).to_broadcast([128, 16, 128]),
                                                       op=ALU.mult), reads=[A_, itT], writes=[A_])
                kb.op('dve', lambda e: e.tensor_tensor(out=B_[:], in0=io_b, in1=itT[:, 1, tg:tg + 16].unsqueeze(2).to_broadcast([128, 16, 128]),
                                                       op=ALU.is_equal), reads=[iota128, itT], writes=[B_])
                for t4 in range(4):
                    pg = psq.next()
                    for tt in range(4):
                        t_ = t4 * 4 + tt
                        kb.op('pe', lambda e: e.matmul(pg[:].rearrange("p (i t) -> p i t", t=4)[:, :, tt], lhsT=B_[:, t_, :], rhs=A_[:, t_, :],
                                                       start=True, stop=True), reads=[A_, B_], writes=[pg])
                    tok0 = tg + t4 * 4
                    kb.op('act', lambda e: e.copy(out=GT[:, :, tok0:tok0 + 4], in_=pg[:].rearrange("p (i t) -> p i t", t=4)),
                          reads=[pg], writes=[GT])

        def dense(bi, n, gen):
            xb = xbs[bi % 2]

            def emit_hid(i, uc, c):
                ph = psq.next()
                for k in range(KD):
                    kb.op('pe', lambda e: e.matmul(ph[:, :n], lhsT=uc[:, c, k * 128:(k + 1) * 128], rhs=xb[:, k, :n],
                                                   start=(k == 0), stop=(k == KD - 1)), reads=[uc, xb], writes=[ph])
                return ph

            def emit_out(i, vc, c, ph):
                x2 = x2r.next()
                tt_ = ttr.next()
                sg = sgr.next()
                xg = xgr.next()
                W_ = Wr.next()
                kb.op('act', lambda e: e.activation(out=x2[:, :n], in_=ph[:, :n], func=AF.Square), reads=[ph], writes=[x2])
                kb.op('dve', lambda e: e.tensor_scalar(out=tt_[:, :n], in0=x2[:, :n], scalar1=0.044715, scalar2=1.0,
                                                       op0=ALU.mult, op1=ALU.add), reads=[x2], writes=[tt_])
                kb.op('dve', lambda e: e.tensor_tensor(out=tt_[:, :n], in0=ph[:, :n], in1=tt_[:, :n], op=ALU.mult),
                      reads=[ph, tt_], writes=[tt_])
                kb.op('act', lambda e: e.activation(out=sg[:, :n], in_=tt_[:, :n], func=AF.Sigmoid, scale=GC), reads=[tt_], writes=[sg])
                kb.op('dve', lambda e: e.tensor_tensor(out=xg[:, :n], in0=ph[:, :n], in1=GT[:, i, :n], op=ALU.mult),
                      reads=[ph, GT], writes=[xg])
                kb.op('pool', lambda e: e.tensor_tensor(out=W_[:, :n], in0=xg[:, :n], in1=sg[:, :n], op=ALU.mult),
                      reads=[xg, sg], writes=[W_])
                for dc in range(KD):
                    kb.op('pe', lambda e: e.matmul(big4[:, dc // 2, (dc % 2) * 256:(dc % 2) * 256 + n], lhsT=vc[:, c, dc * 128:(dc + 1) * 128],
                                                   rhs=W_[:, :n], start=(i == 0), stop=(i == 127)), reads=[vc, W_], writes=[big4])
            prevd = None
            for ig in range(0, 128, 2):
                uc = ucr.next()
                vc = vcr.next()
                kb.dma('sp', uc[:], uT_b[ig * 128:(ig + 2) * 128, :].rearrange("(c p) x -> p c x", p=128), writes=[uc])
                kb.dma('sp', vc[:], v_b[ig * 128:(ig + 2) * 128, :].rearrange("(c p) x -> p c x", p=128), writes=[vc])
                for c in range(2):
                    i = ig + c
                    ph = emit_hid(i, uc, c)
                    if prevd is not None:
                        emit_out(*prevd)
                    prevd = (i, vc, c, ph)
                    if gen is not None and i >= 2:
                        next(gen, None)
            emit_out(*prevd)
            if gen is not None:
                for _ in gen:
                    pass

        nsb = len(act_sblocks)
        for _ in routing_gen(0, *act_sblocks[0]):
            pass
        for bi, (t0, n) in enumerate(act_sblocks):
            mc = 1 if bi >= n_lat_sblocks else 0
            g_build(bi, n)
            gen = routing_gen(bi + 1, *act_sblocks[bi + 1]) if bi + 1 < nsb else None
            dense(bi, n, gen)
            hb = hbs[bi % 2]
            for dc in range(KD):
                hn = hnr.next()
                kb.op('dve', lambda e: e.scalar_tensor_tensor(out=hn[:, :n], in0=big4[:, dc // 2, (dc % 2) * 256:(dc % 2) * 256 + n],
                                                              scalar=modT[:, l, 40 + dc, mc:mc + 1], in1=hb[:, dc, :n],
                                                              op0=ALU.mult, op1=ALU.add), reads=[big4, modT, hb], writes=[hn])
                if last:
                    kb.dma('sp', out_hT[dc * 128:(dc + 1) * 128, t0:t0 + n], hn[:, :n], reads=[hn])
                else:
                    kb.dma('sp', hT[dc * 128:(dc + 1) * 128, t0:t0 + n], hn[:, :n], reads=[hn])
        kb.pop("C%d" % l)

    return kb.finish(), kb


def _fmaj(v):
    v = np.asarray(v, np.float32)
    lead = v.shape[:-1]
    r = v.reshape(lead + (KD, 128))
    r = np.moveaxis(r, -1, 0)
    return np.ascontiguousarray(r)


def prep_inputs(S, CT, NL, b, x, c, ctx, c_ctx, norm1_g, norm2_g, w_mod, b_mod, w_in, q_norm_g, k_norm_g,
                lam_vecs, attn_norm_g, pool_w, pool_scale, conv_w, conv_out_w, w_out, peer_wq, peer_keys, peer_u, peer_v,
                shared=None):
    f = np.float32
    if shared is None:
        cos, sin = rope_tabs(S, CT)
        shared = {
            "w_mod": np.ascontiguousarray(w_mod, f),
            "b_modT": np.ascontiguousarray(np.asarray(b_mod, f).reshape(NL, 48, 128).transpose(2, 0, 1)),
            "g1T": _fmaj(norm1_g), "g2T": _fmaj(norm2_g),
            "w_in": np.ascontiguousarray(w_in, f),
            "qk_g": np.ascontiguousarray(np.stack([np.tile(np.asarray(q_norm_g, f), (1, 2)), np.tile(np.asarray(k_norm_g, f), (1, 2))], -1).transpose(1, 0, 2)),
            "lam_v": np.ascontiguousarray(np.asarray(lam_vecs, f).reshape(NL, 256)),
            "attn_g": np.ascontiguousarray(attn_norm_g, f),
            "pool_w": np.ascontiguousarray(np.asarray(pool_w, f).reshape(NL, D, 256)),
            "pscT": _fmaj(pool_scale),
            "cwT": np.ascontiguousarray(np.moveaxis(_fmaj(conv_w), 2, 3)),
            "conv_out_w": np.ascontiguousarray(conv_out_w, f),
            "w_out": np.ascontiguousarray(w_out, f),
            "peer_wq": np.ascontiguousarray(peer_wq, f),
            "keysT": np.ascontiguousarray(np.asarray(peer_keys, f).reshape(NL, 16, 128, 128).transpose(0, 3, 1, 2).reshape(NL, 128, 2048)),
            "peer_uL": np.ascontiguousarray(np.asarray(peer_u, f).reshape(NL, 128, 128, KD, 128).transpose(0, 1, 4, 3, 2).reshape(NL, N_EXP, D)),
            "peer_v": np.ascontiguousarray(peer_v, f),
            "c_iota128": np.ascontiguousarray(np.tile(np.arange(128, dtype=f), (128, 1))),
            "cosT": cos, "sinT": sin,
            "c_rot": rot_mat(), "c_bd": bd_mat(), "c_idf": np.eye(128, dtype=f),
            "c_band": band_mats(), "c_scat": scat_mats(),
            "c_iota": np.ascontiguousarray(np.tile(np.arange(16, dtype=f), (128, 1))),
        }
    m = dict(shared)
    m["hT0"] = np.ascontiguousarray(np.concatenate([np.asarray(x[b], f).T, np.asarray(ctx[b], f).T], axis=1))
    m["cvec"] = np.ascontiguousarray(np.stack([_fmaj(c[b]), _fmaj(c_ctx)], -1))
    return m, shared


_CACHE = {}


def kernel(**inputs):
    x = np.asarray(inputs["x"])
    B, S, _ = x.shape
    CT = inputs["ctx"].shape[1]
    NL = inputs["w_mod"].shape[0]
    key = (S, CT, NL)
    if key not in _CACHE:
        _CACHE[key] = build(S, CT, NL)[0]
    nc = _CACHE[key]
    in_maps = []
    shared = None
    for core in range(8):
        m, shared = prep_inputs(S, CT, NL, core % B, shared=shared, **inputs)
        in_maps.append(m)
    res = run_bass_kernel_spmd(nc, in_maps, core_ids=list(range(8)))
    out = np.stack([np.ascontiguousarray(np.asarray(res.results[b]["out_hT"]).T) for b in range(B)], 0)
    return out.astype(np.float32)
```

```python
import math
from contextlib import ExitStack
import numpy as np
import ml_dtypes
import concourse.bass as bass
import concourse.mybir as mybir
from concourse.bass_utils import run_bass_kernel_spmd

F32 = mybir.dt.float32
BF16 = mybir.dt.bfloat16
I32 = mybir.dt.int32
U32 = mybir.dt.uint32
ALU = mybir.AluOpType
AF = mybir.ActivationFunctionType
AX = mybir.AxisListType

D = 1024
KD = 8
EPS = 1e-6
Q_OFF, K_OFF, V_OFF, POOL_OFF, CIN_OFF, CB_OFF, CC_OFF, GATE_OFF = 0, 1024, 2048, 3072, 4096, 5120, 6144, 7168
IN_W = 10240
POOL_SIZES = (2, 4, 8, 16)
N_EXP = 16384


class Buf:
    _n = 0

    def __init__(self, t, name):
        self.t = t
        self.name = name
        self.last_w = None
        self.readers = []
        self.dsem = None
        Buf._n += 1
        self.id = Buf._n

    def __getitem__(self, idx):
        return self.t[idx]


class KB:
    ENG = ('pe', 'act', 'dve', 'pool', 'sp')

    def __init__(self, n_dsem=96):
        self.nc = bass.Bass("TRN2", target_bir_lowering=False)
        nc = self.nc
        self.es = ExitStack()
        self.E = {'pe': nc.tensor, 'act': nc.scalar, 'dve': nc.vector, 'pool': nc.gpsimd, 'sp': nc.sync}
        self.sems = {}
        self.cnt = {}
        for e in self.ENG:
            self.sems[('e', e)] = self.es.enter_context(nc.semaphore("p_" + e))
            self.cnt[('e', e)] = 0
        self.free_dsems = []
        for i in range(n_dsem):
            k = ('d', i)
            self.sems[k] = self.es.enter_context(nc.semaphore("d_%d" % i))
            self.cnt[k] = 0
            self.free_dsems.append(k)
        self.seen = {}
        self.n_ins = 0
        self.stack = [(self.es, [])]

    def push(self):
        self.stack.append((ExitStack(), []))

    def pop(self, tag=None):
        if tag is not None:
            self.log = getattr(self, "log", [])
            self.log.append((tag, self.n_ins))
        self.barrier()
        es, bufs = self.stack.pop()
        for b in bufs:
            if b.dsem is not None:
                self.free_dsems.append(b.dsem)
        es.close()

    def sbuf(self, name, shape, dt):
        es, bufs = self.stack[-1]
        t = es.enter_context(self.nc.sbuf_tensor(name + "_%d" % Buf._n, list(shape), dt))
        b = Buf(t, name)
        bufs.append(b)
        return b

    def psum(self, name, shape, dt=F32):
        es, bufs = self.stack[-1]
        t = es.enter_context(self.nc.psum_tensor(name + "_%d" % Buf._n, list(shape), dt))
        b = Buf(t, name)
        bufs.append(b)
        return b

    def _wait(self, eng, key, val):
        k = (eng, key)
        if self.seen.get(k, 0) >= val:
            return
        self.E[eng].wait_ge(self.sems[key], val)
        self.seen[k] = val

    def _deps(self, eng, reads, writes, is_dma=False):
        deps = {}
        me = ('e', eng)

        def add(st):
            key, val = st
            if deps.get(key, 0) < val:
                deps[key] = val
        for b in reads:
            if b.last_w is not None:
                if b.last_w[0] == me and eng == 'pe' and not is_dma:
                    continue
                add(b.last_w)
        for b in writes:
            if b.last_w is not None and (is_dma or b.last_w[0] != me):
                add(b.last_w)
            for r in b.readers:
                if r[0] == me and not is_dma:
                    continue
                add(r)
        for key, val in deps.items():
            self._wait(eng, key, val)

    def _stamp(self, st, reads, writes):
        for b in reads:
            b.readers.append(st)
            if len(b.readers) > 16:
                m = {}
                for k, v in b.readers:
                    if m.get(k, 0) < v:
                        m[k] = v
                b.readers = list(m.items())
        for b in writes:
            b.last_w = st
            b.readers = []

    def op(self, eng, fn, reads=(), writes=()):
        self._deps(eng, reads, writes)
        ins = fn(self.E[eng])
        key = ('e', eng)
        self.cnt[key] += 1
        ins.then_inc(self.sems[key], 1)
        self._stamp((key, self.cnt[key]), reads, writes)
        self.n_ins += 1
        return ins

    def dma(self, q, out, in_, reads=(), writes=(), indirect=None):
        self._deps(q, reads, writes, is_dma=True)
        b = (list(writes) + list(reads))[0]
        if b.dsem is None:
            b.dsem = self.free_dsems.pop(0)
        if indirect is None:
            ins = self.E[q].dma_start(out=out, in_=in_)
        else:
            ins = self.E[q].indirect_dma_start(out=out, out_offset=None, in_=in_, in_offset=indirect)
        self.cnt[b.dsem] += 16
        ins.then_inc(self.sems[b.dsem], 16)
        self._stamp((b.dsem, self.cnt[b.dsem]), reads, writes)
        self.n_ins += 1
        return ins

    def barrier(self):
        for f in self.ENG:
            for key, val in self.cnt.items():
                if val > 0:
                    self._wait(f, key, val)

    def finish(self):
        self.barrier()
        while len(self.stack) > 1:
            self.pop()
        return self.nc


class Ring:
    def __init__(self, bufs):
        self.bufs = bufs
        self.i = 0

    def next(self):
        b = self.bufs[self.i % len(self.bufs)]
        self.i += 1
        return b


def band_mats():
    out = np.zeros((128, 20, 128), np.float32)
    for wi, w in enumerate(POOL_SIZES):
        lo = w // 2
        hi = w - 1 - lo
        for t in range(128):
            for tp in range(-128, 256):
                if t - lo <= tp <= t + hi:
                    if tp < 0:
                        out[tp + 128, wi * 5 + 0, t] = 1.0 / w
                    elif tp < 128:
                        out[tp, wi * 5 + 1, t] = 1.0 / w
                    else:
                        out[tp - 128, wi * 5 + 2, t] = 1.0 / w
            s0, e0 = max(t - lo, 0), t + hi
            cnt = e0 - s0 + 1
            for tp in range(s0, min(e0, 127) + 1):
                out[tp, wi * 5 + 3, t] = 1.0 / cnt
            s1, e1 = t - lo, min(t + hi, 127)
            cnt = e1 - s1 + 1
            for tp in range(max(s1, 0), e1 + 1):
                out[tp, wi * 5 + 4, t] = 1.0 / cnt
        for kind in (1, 3, 4):
            out[:, wi * 5 + kind, :] -= np.eye(128, dtype=np.float32)
    return out


def scat_mats():
    out = np.zeros((128, 3, 384), np.float32)
    for t in range(128):
        if t - 1 >= 0:
            out[t - 1, 1, 0 * 128 + t] = 1.0
        else:
            out[127, 0, 0 * 128 + t] = 1.0
        out[t, 1, 1 * 128 + t] = 1.0
        if t + 1 < 128:
            out[t + 1, 1, 2 * 128 + t] = 1.0
        else:
            out[0, 2, 2 * 128 + t] = 1.0
    return out


def rope_tabs(S, CT):
    n_rows = S // 64
    rows = np.repeat(np.arange(n_rows, dtype=np.float32), 64)
    cols = np.tile(np.arange(64, dtype=np.float32), n_rows)
    inv = (10000.0 ** (-np.arange(16, dtype=np.float32) / 16)).astype(np.float32)
    ang = np.concatenate([rows[:, None] * inv, cols[:, None] * inv], axis=-1)
    cos = np.concatenate([np.cos(ang), np.ones((CT, 32), np.float32)], 0).astype(np.float32)
    sin = np.concatenate([np.sin(ang), np.zeros((CT, 32), np.float32)], 0).astype(np.float32)
    idx = np.arange(128) % 32
    return np.ascontiguousarray(cos[:, idx].T), np.ascontiguousarray(sin[:, idx].T)


def rot_mat():
    R = np.zeros((128, 128), np.float32)
    for p in range(128):
        if (p % 64) < 32:
            R[p + 32, p] = -1.0
        else:
            R[p - 32, p] = 1.0
    return R


def bd_mat():
    M = np.zeros((128, 128), np.float32)
    M[:64, :64] = 1.0 / 64
    M[64:, 64:] = 1.0 / 64
    return M


def build(S, CT, NL, dbg=False):
    kb = KB()
    nc = kb.nc
    T = S + CT
    NT = T // 128
    blocks = [(i * 512, 512) for i in range(S // 512)] + [(S, CT)]
    n_lat_blocks = S // 512
    SB = 256
    sblocks = [(i * SB, SB) for i in range(S // SB)] + [(S, CT)]
    n_lat_sblocks = S // SB

    def din(name, shape, dt=F32):
        return nc.dram_tensor(name, list(shape), dt, kind="ExternalInput").ap()

    def dscr(name, shape, dt):
        return nc.dram_tensor(name, list(shape), dt, kind="ExternalOutput" if dbg else "Internal").ap()

    hT0 = din("hT0", [D, T])
    cvec = din("cvec", [128, KD, 2])
    w_mod = din("w_mod", [NL, D, 6 * D])
    b_modT = din("b_modT", [128, NL, 48])
    g1T = din("g1T", [128, NL, KD])
    g2T = din("g2T", [128, NL, KD])
    w_in = din("w_in", [NL, D, IN_W])
    qk_g = din("qk_g", [128, NL, 2])
    lam_v = din("lam_v", [NL, 256])
    attn_g = din("attn_g", [NL, 128])
    pool_w = din("pool_w", [NL, D, 256])
    pscT = din("pscT", [128, NL, KD])
    cwT = din("cwT", [128, NL, KD, 3])
    conv_out_w = din("conv_out_w", [NL, D, D])
    w_out = din("w_out", [NL, D, D])
    peer_wq = din("peer_wq", [NL, D, 2048])
    keysT = din("keysT", [NL, 128, 2048])
    peer_uL = din("peer_uL", [NL, N_EXP, D])
    peer_v = din("peer_v", [NL, N_EXP, D])
    c_iota128 = din("c_iota128", [128, 128])
    cosT = din("cosT", [128, T])
    sinT = din("sinT", [128, T])
    c_rot = din("c_rot", [128, 128])
    c_bd = din("c_bd", [128, 128])
    c_idf = din("c_idf", [128, 128])
    c_band = din("c_band", [128, 20, 128])
    c_scat = din("c_scat", [128, 3, 384])
    c_iota = din("c_iota", [128, 16])
    out_hT = nc.dram_tensor("out_hT", [D, S], F32, kind="ExternalOutput").ap()

    hT = dscr("s_hT", [D, T], F32)
    xnT = dscr("s_xnT", [D, T], BF16)
    qT = dscr("s_qT", [D, T], BF16)
    kT = dscr("s_kT", [D, T], BF16)
    Vt = dscr("s_Vt", [T, D], BF16)
    uh = dscr("s_uh", [T, 2 * D], BF16)
    attnT = dscr("s_attnT", [D, T], BF16)
    w_in_b = dscr("s_w_in_b", [D, IN_W], BF16)
    co_b = dscr("s_co_b", [D, D], BF16)
    wo_b = dscr("s_wo_b", [D, D], BF16)
    pw_b = dscr("s_pw_b", [D, 256], BF16)
    wq_b = dscr("s_wq_b", [D, 2048], BF16)
    keys_b = dscr("s_keys_b", [128, 2048], BF16)
    uT_b = dscr("s_uT_b", [N_EXP, D], BF16)
    v_b = dscr("s_v_b", [N_EXP, D], BF16)

    def fm(ap2d, c0, n):
        return ap2d[:, c0:c0 + n].rearrange("(k p) t -> p k t", p=128)

    ones_m = kb.sbuf("ones_m", [128, 128], F32)
    rot_s = kb.sbuf("rot_s", [128, 128], F32)
    bd_s = kb.sbuf("bd_s", [128, 128], F32)
    idf = kb.sbuf("idf", [128, 128], F32)
    idb = kb.sbuf("idb", [128, 128], BF16)
    modT = kb.sbuf("modT", [128, NL, 48, 2], F32)
    A1 = kb.sbuf("A1", [128, NL, KD, 2], F32)
    A2 = kb.sbuf("A2", [128, NL, KD, 2], F32)
    g1s = kb.sbuf("g1s", [128, NL, KD], F32)
    g2s = kb.sbuf("g2s", [128, NL, KD], F32)
    qkg = kb.sbuf("qkg", [128, NL, 2], F32)
    psc = kb.sbuf("psc", [128, NL, KD], F32)
    cws = kb.sbuf("cws", [128, NL, KD, 3], F32)
    iota16 = kb.sbuf("iota16", [128, 16], F32)
    iota128 = kb.sbuf("iota128", [128, 128], F32)

    kb.op('dve', lambda e: e.memset(ones_m[:], 1.0 / D), writes=[ones_m])
    for sb, src in ((rot_s, c_rot), (bd_s, c_bd), (idf, c_idf), (g1s, g1T), (g2s, g2T), (qkg, qk_g),
                    (psc, pscT), (cws, cwT), (iota16, c_iota), (iota128, c_iota128)):
        kb.dma('sp', sb[:], src, writes=[sb])
    kb.op('dve', lambda e: e.tensor_copy(out=idb[:], in_=idf[:]), reads=[idf], writes=[idb])
    band = kb.sbuf("band", [128, 20, 128], BF16)
    scat = kb.sbuf("scat", [128, 3, 384], BF16)
    kb.push()
    bandf = kb.sbuf("bandf", [128, 20, 128], F32)
    scatf = kb.sbuf("scatf", [128, 3, 384], F32)
    kb.dma('sp', bandf[:], c_band, writes=[bandf])
    kb.dma('sp', scatf[:], c_scat, writes=[scatf])
    kb.op('dve', lambda e: e.tensor_copy(out=band[:], in_=bandf[:]), reads=[bandf], writes=[band])
    kb.op('dve', lambda e: e.tensor_copy(out=scat[:], in_=scatf[:]), reads=[scatf], writes=[scat])
    kb.pop()

    kb.push()
    stg = Ring([kb.sbuf("cp%d" % i, [128, KD, 512], F32) for i in range(2)])
    for (t0, n) in blocks:
        b = stg.next()
        kb.dma('sp', b[:, :, :n], fm(hT0, t0, n), writes=[b])
        kb.dma('sp', fm(hT, t0, n), b[:, :, :n], reads=[b])
    sc = kb.sbuf("sc", [128, KD, 2], F32)
    scs = kb.sbuf("scs", [128, KD, 2], F32)
    bm = kb.sbuf("bm", [128, NL, 48], F32)
    kb.dma('sp', sc[:], cvec, writes=[sc])
    kb.dma('sp', bm[:], b_modT, writes=[bm])
    kb.op('act', lambda e: e.activation(out=scs[:], in_=sc[:], func=AF.Silu), reads=[sc], writes=[scs])
    wmr = Ring([kb.sbuf("wm%d" % i, [128, KD, 512], F32) for i in range(2)])
    mps = kb.psum("mps", [128, 48, 2])
    for l in range(NL):
        for jt in range(12):
            wm = wmr.next()
            kb.dma('sp', wm[:], fm(w_mod[l], jt * 512, 512), writes=[wm])
            for jj in range(4):
                j = jt * 4 + jj
                for k in range(KD):
                    kb.op('pe', lambda e: e.matmul(mps[:, j, :], lhsT=wm[:, k, jj * 128:(jj + 1) * 128],
                                                   rhs=scs[:, k, :], start=(k == 0), stop=(k == KD - 1)),
                          reads=[wm, scs], writes=[mps])
        kb.op('dve', lambda e: e.tensor_tensor(out=modT[:, l], in0=mps[:],
                                               in1=bm[:, l, :].unsqueeze(2).to_broadcast([128, 48, 2]), op=ALU.add),
              reads=[mps, bm], writes=[modT])
        kb.op('dve', lambda e: e.scalar_tensor_tensor(
            out=A1[:, l], in0=modT[:, l, 8:16, :], scalar=1.0,
            in1=g1s[:, l, :].unsqueeze(2).to_broadcast([128, KD, 2]), op0=ALU.add, op1=ALU.mult),
            reads=[modT, g1s], writes=[A1])
        kb.op('dve', lambda e: e.scalar_tensor_tensor(
            out=A2[:, l], in0=modT[:, l, 32:40, :], scalar=1.0,
            in1=g2s[:, l, :].unsqueeze(2).to_broadcast([128, KD, 2]), op0=ALU.add, op1=ALU.mult),
            reads=[modT, g2s], writes=[A2])
    kb.pop()

    def convert(src2d, dst2d, R, C):
        kb.push()
        sf = Ring([kb.sbuf("cvf%d" % i, [128, 2048], F32) for i in range(3)])
        sb_ = Ring([kb.sbuf("cvb%d" % i, [128, 2048], BF16) for i in range(3)])
        engs = ['pool', 'act', 'dve']
        i = 0
        for r0 in range(0, R, 128):
            for c0 in range(0, C, 2048):
                cw = min(2048, C - c0)
                a = sf.next()
                b = sb_.next()
                kb.dma('sp', a[:, :cw], src2d[r0:r0 + 128, c0:c0 + cw], writes=[a])
                eng = engs[i % 3]
                i += 1
                if eng == 'act':
                    kb.op('act', lambda e: e.copy(out=b[:, :cw], in_=a[:, :cw]), reads=[a], writes=[b])
                else:
                    kb.op(eng, lambda e: e.tensor_copy(out=b[:, :cw], in_=a[:, :cw]), reads=[a], writes=[b])
                kb.dma('act', dst2d[r0:r0 + 128, c0:c0 + cw], b[:, :cw], reads=[b])
        kb.pop()

    def rsqrt_eps(ob, oap, ib, iap, eps):
        kb.op('dve', lambda e: e.tensor_scalar(out=oap, in0=iap, scalar1=eps, scalar2=None, op0=ALU.add), reads=[ib], writes=[ob])
        kb.op('act', lambda e: e.activation(out=oap, in_=oap, func=AF.Sqrt), reads=[ob], writes=[ob])
        kb.op('dve', lambda e: e.reciprocal(out=oap, in_=oap), reads=[ob], writes=[ob])

    def norm_block(hb, n, Acol, Bcol, outs, sqr, psn, rs, tmpr):
        for k in range(KD):
            sq = sqr.next()
            kb.op('act', lambda e: e.activation(out=sq[:, :n], in_=hb[:, k, :n], func=AF.Square), reads=[hb], writes=[sq])
            kb.op('pe', lambda e: e.matmul(psn[:, :n], lhsT=ones_m[:], rhs=sq[:, :n], start=(k == 0), stop=(k == KD - 1)),
                  reads=[ones_m, sq], writes=[psn])
        rsqrt_eps(rs, rs[:, :n], psn, psn[:, :n], EPS)
        for k in range(KD):
            tmp = tmpr.next()
            kb.op('dve', lambda e: e.scalar_tensor_tensor(out=tmp[:, :n], in0=hb[:, k, :n], scalar=Acol(k),
                                                          in1=rs[:, :n], op0=ALU.mult, op1=ALU.mult),
                  reads=[hb, rs, A1, A2], writes=[tmp])
            for ob in outs:
                kb.op('pool', lambda e: e.tensor_scalar(out=ob[:, k, :n], in0=tmp[:, :n], scalar1=Bcol(k), scalar2=None,
                                                        op0=ALU.add), reads=[tmp, modT], writes=[ob])

    for l in range(NL):
        last = (l == NL - 1)
        lam_init = 0.8 - 0.6 * math.exp(-0.3 * l)
        act_blocks = blocks[:n_lat_blocks] if last else blocks
        act_sblocks = sblocks[:n_lat_sblocks] if last else sblocks

        convert(w_in[l], w_in_b, D, IN_W)
        convert(conv_out_w[l], co_b, D, D)
        convert(w_out[l], wo_b, D, D)
        convert(pool_w[l], pw_b, D, 256)
        convert(peer_wq[l], wq_b, D, 2048)
        convert(keysT[l], keys_b, 128, 2048)
        convert(peer_uL[l], uT_b, N_EXP, D)
        convert(peer_v[l], v_b, N_EXP, D)

        kb.push()
        hb = kb.sbuf("hb", [128, KD, 512], F32)
        xn = kb.sbuf("xn", [128, KD, 512], BF16)
        sqr = Ring([kb.sbuf("sq%d" % i, [128, 512], F32) for i in range(2)])
        tmpr = Ring([kb.sbuf("tmp%d" % i, [128, 512], F32) for i in range(2)])
        rs = kb.sbuf("rs", [128, 512], F32)
        rs2 = kb.sbuf("rs2", [128, 512], F32)
        qnr = Ring([kb.sbuf("qn%d" % i, [128, 512], F32) for i in range(2)])
        t1 = kb.sbuf("t1", [128, 512], F32)
        t2 = kb.sbuf("t2", [128, 512], F32)
        cosb = kb.sbuf("cosb", [128, 512], F32)
        sinb = kb.sbuf("sinb", [128, 512], F32)
        qor = Ring([kb.sbuf("qo%d" % i, [128, 512], BF16) for i in range(2)])
        wr = Ring([kb.sbuf("wA%d" % i, [128, KD, 512], BF16) for i in range(3)])
        vbr = Ring([kb.sbuf("vb%d" % i, [128, 512], BF16) for i in range(2)])
        c1r = Ring([kb.sbuf("c1%d" % i, [128, 512], F32) for i in range(2)])
        psn = kb.psum("psn", [128, 512])
        psr = Ring([kb.psum("psA%d" % i, [128, 512]) for i in range(3)])
        ps2 = kb.psum("ps2", [128, 512])
        ps3 = kb.psum("ps3", [128, 512])
        psc2 = kb.psum("psc2", [128, 512])

        def loadw(col0):
            w = wr.next()
            kb.dma('sp', w[:], fm(w_in_b, col0, 512), writes=[w])
            return w

        for bi, (t0, n) in enumerate(blocks):
            nt = n // 128
            mc = 0 if bi < n_lat_blocks else 1
            kb.dma('sp', hb[:, :, :n], fm(hT, t0, n), writes=[hb])
            kb.dma('sp', cosb[:, :n], cosT[:, t0:t0 + n], writes=[cosb])
            kb.dma('sp', sinb[:, :n], sinT[:, t0:t0 + n], writes=[sinb])
            norm_block(hb, n, lambda k: A1[:, l, k, mc:mc + 1], lambda k: modT[:, l, k, mc:mc + 1], [xn],
                       sqr, psn, rs, tmpr)
            kb.dma('sp', fm(xnT, t0, n), xn[:, :, :n], reads=[xn])
            heads = []
            for (coloff, dst, gcol) in ((Q_OFF, qT, 0), (K_OFF, kT, 1)):
                for wt in range(2):
                    for hh in range(4):
                        heads.append((coloff, dst, gcol, wt, hh))
            wcur = {}

            def st1(hi):
                coloff, dst, gcol, wt, hh = heads[hi]
                if hh == 0:
                    wcur['w'] = loadw(coloff + wt * 512)
                w = wcur['w']
                ps = psr.next()
                for k in range(KD):
                    kb.op('pe', lambda e: e.matmul(ps[:, :n], lhsT=w[:, k, hh * 128:(hh + 1) * 128], rhs=xn[:, k, :n],
                                                   start=(k == 0), stop=(k == KD - 1)), reads=[w, xn], writes=[ps])
                sq = sqr.next()
                kb.op('act', lambda e: e.activation(out=sq[:, :n], in_=ps[:, :n], func=AF.Square), reads=[ps], writes=[sq])
                return (ps, sq)

            def st2(hi, ps, sq):
                coloff, dst, gcol, wt, hh = heads[hi]
                kb.op('pe', lambda e: e.matmul(ps2[:, :n], lhsT=bd_s[:], rhs=sq[:, :n], start=True, stop=True),
                      reads=[bd_s, sq], writes=[ps2])
                rsqrt_eps(rs2, rs2[:, :n], ps2, ps2[:, :n], EPS)
                qn_ = qnr.next()
                kb.op('dve', lambda e: e.scalar_tensor_tensor(out=qn_[:, :n], in0=ps[:, :n], scalar=qkg[:, l, gcol:gcol + 1],
                                                              in1=rs2[:, :n], op0=ALU.mult, op1=ALU.mult),
                      reads=[ps, rs2, qkg], writes=[qn_])
                return qn_

            def st3(hi, qn_):
                coloff, dst, gcol, wt, hh = heads[hi]
                h = wt * 4 + hh
                kb.op('pe', lambda e: e.matmul(ps3[:, :n], lhsT=rot_s[:], rhs=qn_[:, :n], start=True, stop=True),
                      reads=[rot_s, qn_], writes=[ps3])
                kb.op('pool', lambda e: e.tensor_tensor(out=t1[:, :n], in0=qn_[:, :n], in1=cosb[:, :n], op=ALU.mult),
                      reads=[qn_, cosb], writes=[t1])
                kb.op('dve', lambda e: e.tensor_tensor(out=t2[:, :n], in0=ps3[:, :n], in1=sinb[:, :n], op=ALU.mult),
                      reads=[ps3, sinb], writes=[t2])
                qo = qor.next()
                kb.op('pool', lambda e: e.tensor_tensor(out=qo[:, :n], in0=t1[:, :n], in1=t2[:, :n], op=ALU.add),
                      reads=[t1, t2], writes=[qo])
                kb.dma('sp', dst[h * 128:(h + 1) * 128, t0:t0 + n], qo[:, :n], reads=[qo])
            r1 = {}
            r2 = {}
            NH = len(heads)
            for step in range(NH + 2):
                if step < NH:
                    r1[step] = st1(step)
                if 0 <= step - 1 < NH:
                    r2[step - 1] = st2(step - 1, *r1.pop(step - 1))
                if 0 <= step - 2 < NH:
                    st3(step - 2, r2.pop(step - 2))
            for (coloff, dstap, dcol) in ((V_OFF, Vt, 0), (POOL_OFF, uh, 0)):
                for wt in range(2):
                    w = loadw(coloff + wt * 512)
                    for ti in range(nt):
                        ps = psr.next()
                        for k in range(KD):
                            kb.op('pe', lambda e: e.matmul(ps[:], lhsT=xn[:, k, ti * 128:(ti + 1) * 128], rhs=w[:, k, :],
                                                           start=(k == 0), stop=(k == KD - 1)), reads=[w, xn], writes=[ps])
                        vb = vbr.next()
                        kb.op('act', lambda e: e.copy(out=vb[:], in_=ps[:]), reads=[ps], writes=[vb])
                        r0 = t0 + ti * 128
                        kb.dma('sp', dstap[r0:r0 + 128, dcol + wt * 512: dcol + (wt + 1) * 512], vb[:], reads=[vb])
            for wt in range(2):
                w1 = loadw(CIN_OFF + wt * 512)
                w2 = loadw(CC_OFF + wt * 512)
                for ti in range(nt):
                    ps = psr.next()
                    for k in range(KD):
                        kb.op('pe', lambda e: e.matmul(ps[:], lhsT=xn[:, k, ti * 128:(ti + 1) * 128], rhs=w1[:, k, :],
                                                       start=(k == 0), stop=(k == KD - 1)), reads=[w1, xn], writes=[ps])
                    for k in range(KD):
                        kb.op('pe', lambda e: e.matmul(psc2[:], lhsT=xn[:, k, ti * 128:(ti + 1) * 128], rhs=w2[:, k, :],
                                                       start=(k == 0), stop=(k == KD - 1)), reads=[w2, xn], writes=[psc2])
                    c1 = c1r.next()
                    kb.op('act', lambda e: e.copy(out=c1[:], in_=ps[:]), reads=[ps], writes=[c1])
                    vb = vbr.next()
                    kb.op('dve', lambda e: e.tensor_tensor(out=vb[:], in0=psc2[:], in1=c1[:], op=ALU.mult),
                          reads=[psc2, c1], writes=[vb])
                    r0 = t0 + ti * 128
                    kb.dma('sp', uh[r0:r0 + 128, D + wt * 512: D + (wt + 1) * 512], vb[:], reads=[vb])
        kb.pop("A%d" % l)

        kb.push()
        lvb = kb.sbuf("lvb", [128, 256], F32)
        lprod = kb.sbuf("lprod", [128, 128], F32)
        lsum = kb.sbuf("lsum", [128, 2], F32)
        lexp = kb.sbuf("lexp", [128, 2], F32)
        nlam = kb.sbuf("nlam", [128, 1], F32)
        gvT = kb.sbuf("gvT", [128, 1], F32)
        ones1 = kb.sbuf("ones1", [128, 128], F32)
        kb.op('pool', lambda e: e.memset(ones1[:], 1.0), writes=[ones1])
        kb.dma('sp', lvb[:], lam_v[l:l + 1, :].partition_broadcast(128), writes=[lvb])
        kb.dma('sp', gvT[:], attn_g[l:l + 1, :].rearrange("o e -> e o"), writes=[gvT])
        lv4 = lvb[:].rearrange("p (a d) -> p a d", a=4)
        kb.op('dve', lambda e: e.tensor_tensor(out=lprod[:, 0:64], in0=lv4[:, 0, :], in1=lv4[:, 1, :], op=ALU.mult),
              reads=[lvb], writes=[lprod])
        kb.op('dve', lambda e: e.tensor_tensor(out=lprod[:, 64:128], in0=lv4[:, 2, :], in1=lv4[:, 3, :], op=ALU.mult),
              reads=[lvb, lprod], writes=[lprod])
        kb.op('dve', lambda e: e.tensor_reduce(out=lsum[:], in_=lprod[:].rearrange("p (a d) -> p a d", a=2), axis=AX.X, op=ALU.add),
              reads=[lprod], writes=[lsum])
        kb.op('act', lambda e: e.activation(out=lexp[:], in_=lsum[:], func=AF.Exp), reads=[lsum], writes=[lexp])
        kb.op('dve', lambda e: e.tensor_tensor(out=nlam[:], in0=lexp[:, 1:2], in1=lexp[:, 0:1], op=ALU.subtract),
              reads=[lexp], writes=[nlam])
        kb.op('dve', lambda e: e.tensor_scalar(out=nlam[:], in0=nlam[:], scalar1=-lam_init, scalar2=None, op0=ALU.add),
              reads=[nlam], writes=[nlam])
        kb.op('dve', lambda e: e.tensor_scalar(out=gvT[:], in0=gvT[:], scalar1=math.sqrt(128.0) * (1.0 - lam_init),
                                               scalar2=None, op0=ALU.mult), reads=[gvT], writes=[gvT])
        kTr = Ring([kb.sbuf("kTh%d" % i, [128, T], BF16) for i in range(2)])
        qTr = Ring([kb.sbuf("qTh%d" % i, [128, T], BF16) for i in range(2)])
        var = Ring([kb.sbuf("vah%d" % i, [128, NT, 128], BF16) for i in range(2)])
        ptr = Ring([kb.sbuf("pt%d" % i, [128, 1024], BF16) for i in range(4)])
        zacc = kb.sbuf("zacc", [128, 1024], F32)
        zaccP = kb.sbuf("zaccP", [128, 1024], F32)
        rzb = kb.sbuf("rzb", [128, 1024], F32)
        osb = [kb.sbuf("osb%d" % i, [128, 1024], F32) for i in range(2)]
        oc = kb.sbuf("oc", [128, 1024], F32)
        osq = kb.sbuf("osq", [128, 1024], F32)
        rsd = kb.sbuf("rsd", [128, 1024], F32)
        aor = Ring([kb.sbuf("ao%d" % i, [128, 1024], BF16) for i in range(2)])
        psS = Ring([kb.psum("pS%d" % i, [128, 2, 512]) for i in range(3)])
        psO = kb.psum("pO", [128, 2, 512])
        qblocks = [(g0, 512) for g0 in range(0, S, 512)]
        if not last:
            qblocks.append((S, CT))
        for h in range(8):
            kh = kTr.next()
            qh = qTr.next()
            va = var.next()
            kb.dma('sp', kh[:], kT[h * 128:(h + 1) * 128, :], writes=[kh])
            kb.dma('sp', qh[:], qT[h * 128:(h + 1) * 128, :], writes=[qh])
            for c0 in range(0, NT, 16):
                cn = min(16, NT - c0)
                kb.dma('sp', va[:, c0:c0 + cn, :],
                       Vt[c0 * 128:(c0 + cn) * 128, h * 128:(h + 1) * 128].rearrange("(c p) e -> p c e", p=128), writes=[va])
            for (tq, n) in qblocks:
                is_ctx = tq >= S
                kcs = list(range(S // 128, NT)) if is_ctx else list(range(NT))

                def emit_qk(kc):
                    ps = psS.next()
                    for s_ in range(2):
                        kb.op('pe', lambda e: e.matmul(ps[:, s_, :n], lhsT=kh[s_ * 64:(s_ + 1) * 64, kc * 128:(kc + 1) * 128],
                                                       rhs=qh[s_ * 64:(s_ + 1) * 64, tq:tq + n], start=True, stop=True),
                              reads=[kh, qh], writes=[ps])
                    return ps

                def emit_rest(ci, kc, ps):
                    pt = ptr.next()
                    ptv = pt[:].rearrange("p (b x) -> p b x", b=2)
                    kb.op('act', lambda e: e.activation(out=ptv[:, :, :n], in_=ps[:, :, :n], func=AF.Exp, scale=0.125),
                          reads=[ps], writes=[pt])
                    for s_ in range(2):
                        kb.op('pe', lambda e: e.matmul(psO[:, s_, :n], lhsT=va[:, kc, :], rhs=pt[:, s_ * 512:s_ * 512 + n],
                                                       start=(ci == 0), stop=(ci == len(kcs) - 1)), reads=[pt, va], writes=[psO])
                    zeng, zb = 'dve', (zacc if ci % 2 == 0 else zaccP)
                    first = (ci < 2)
                    zv = zb[:].rearrange("p (b x) -> p b x", b=2)
                    if first:
                        kb.op(zeng, lambda e: e.tensor_copy(out=zv[:, :, :n], in_=ptv[:, :, :n]), reads=[pt], writes=[zb])
                    else:
                        kb.op(zeng, lambda e: e.tensor_tensor(out=zv[:, :, :n], in0=zv[:, :, :n], in1=ptv[:, :, :n], op=ALU.add),
                              reads=[pt, zb], writes=[zb])
                pend = []
                for ci, kc in enumerate(kcs):
                    ps = emit_qk(kc)
                    pend.append((ci, kc, ps))
                    if len(pend) > 2:
                        emit_rest(*pend.pop(0))
                while pend:
                    emit_rest(*pend.pop(0))
                pzb = psS.next()
                for s_ in range(2):
                    sl = slice(s_ * 512, s_ * 512 + n)
                    use_p = len(kcs) > 1
                    kb.op('pe', lambda e: e.matmul(pzb[:, s_, :n], lhsT=ones1[:], rhs=zacc[:, sl], start=True, stop=(not use_p)),
                          reads=[ones1, zacc], writes=[pzb])
                    if use_p:
                        kb.op('pe', lambda e: e.matmul(pzb[:, s_, :n], lhsT=ones1[:], rhs=zaccP[:, sl], start=False, stop=True),
                              reads=[ones1, zaccP], writes=[pzb])
                    kb.op('dve', lambda e: e.reciprocal(out=rzb[:, sl], in_=pzb[:, s_, :n]), reads=[pzb], writes=[rzb])
                    kb.op('dve', lambda e: e.tensor_tensor(out=osb[s_][:, :n], in0=psO[:, s_, :n], in1=rzb[:, sl], op=ALU.mult),
                          reads=[psO, rzb], writes=[osb[s_]])
                ao = aor.next()
                sl = slice(0, n)
                kb.op('dve', lambda e: e.scalar_tensor_tensor(out=oc[:, sl], in0=osb[1][:, sl], scalar=nlam[:, 0:1], in1=osb[0][:, sl],
                                                              op0=ALU.mult, op1=ALU.add), reads=[osb[0], osb[1], nlam], writes=[oc])
                kb.op('act', lambda e: e.activation(out=osq[:, sl], in_=oc[:, sl], func=AF.Square), reads=[oc], writes=[osq])
                pz = psS.next()
                kb.op('pe', lambda e: e.matmul(pz[:, 0, :n], lhsT=ones1[:], rhs=osq[:, sl], start=True, stop=True),
                      reads=[ones1, osq], writes=[pz])
                rsqrt_eps(rsd, rsd[:, sl], pz, pz[:, 0, :n], 128.0 * EPS)
                kb.op('dve', lambda e: e.scalar_tensor_tensor(out=ao[:, sl], in0=oc[:, sl], scalar=gvT[:, 0:1], in1=rsd[:, sl],
                                                              op0=ALU.mult, op1=ALU.mult), reads=[oc, gvT, rsd], writes=[ao])
                kb.dma('sp', attnT[h * 128:(h + 1) * 128, tq:tq + n], ao[:, sl], reads=[ao])
        kb.pop("B1_%d" % l)

        kb.push()
        cow = kb.sbuf("cow", [128, KD, D], BF16)
        wow = kb.sbuf("wow", [128, KD, D], BF16)
        pww = kb.sbuf("pww", [128, KD, 256], BF16)
        kb.dma('sp', cow[:], fm(co_b, 0, D), writes=[cow])
        kb.dma('sp', wow[:], fm(wo_b, 0, D), writes=[wow])
        kb.dma('sp', pww[:], fm(pw_b, 0, 256), writes=[pww])
        xn = kb.sbuf("xnB", [128, KD, SB], BF16)
        uht = kb.sbuf("uht", [128, 4, 2 * D], BF16)
        att = kb.sbuf("att", [128, KD, SB], BF16)
        cbT = kb.sbuf("cbT", [128, KD, SB], BF16)
        plT = kb.sbuf("plT", [128, KD, SB], BF16)
        bcT = kb.sbuf("bcT", [128, KD, SB], BF16)
        mgT = kb.sbuf("mgT", [128, KD, SB], BF16)
        wcr = Ring([kb.sbuf("wcb%d" % i, [128, KD, 512], BF16) for i in range(2)])
        gwr = Ring([kb.sbuf("gw%d" % i, [128, KD, 3, 128], BF16) for i in range(2)])
        gTr = Ring([kb.sbuf("gT%d" % i, [128, 3, SB], F32) for i in range(2)])
        hcr = Ring([kb.sbuf("hcb%d" % i, [128, SB], F32) for i in range(2)])
        hnr = Ring([kb.sbuf("hnb%d" % i, [128, SB], F32) for i in range(2)])
        m0 = kb.sbuf("m0", [128, SB], F32)
        m1 = kb.sbuf("m1", [128, SB], F32)
        m2 = kb.sbuf("m2", [128, SB], F32)
        cacc = Ring([kb.sbuf("cacc%d" % i, [128, 128], F32) for i in range(2)])
        psr = Ring([kb.psum("psB%d" % i, [128, 512]) for i in range(4)])
        pcv = Ring([kb.psum("pcv%d" % i, [128, 512]) for i in range(2)])
        for bi, (t0, n) in enumerate(act_sblocks):
            nt = n // 128
            tile0 = t0 // 128
            is_ctx = bi >= n_lat_sblocks
            mc = 1 if is_ctx else 0
            seq_first = (tile0 == 0) or (tile0 == S // 128)
            seq_last_tile = (S // 128 - 1) if not is_ctx else (NT - 1)
            kb.dma('sp', xn[:, :, :n], fm(xnT, t0, n), writes=[xn])
            kb.dma('sp', att[:, :, :n], fm(attnT, t0, n), writes=[att])
            lo_t = tile0 if seq_first else tile0 - 1
            hi_t = min(tile0 + nt, seq_last_tile)
            kb.dma('sp', uht[:, lo_t - (tile0 - 1): hi_t - (tile0 - 1) + 1, :],
                   uh[lo_t * 128:(hi_t + 1) * 128, :].rearrange("(q p) e -> p q e", p=128), writes=[uht])
            for wt in range(2):
                w = wcr.next()
                kb.dma('sp', w[:], fm(w_in_b, CB_OFF + wt * 512, 512), writes=[w])
                for cc in range(4):
                    ps = psr.next()
                    for k in range(KD):
                        kb.op('pe', lambda e: e.matmul(ps[:, :n], lhsT=w[:, k, cc * 128:(cc + 1) * 128], rhs=xn[:, k, :n],
                                                       start=(k == 0), stop=(k == KD - 1)), reads=[w, xn], writes=[ps])
                    kb.op('act', lambda e: e.copy(out=cbT[:, wt * 4 + cc, :n], in_=ps[:, :n]), reads=[ps], writes=[cbT])
            for c in range(KD):
                wi = c // 2
                ps = psr.next()
                for ti in range(nt):
                    tg = tile0 + ti
                    first = (tg == 0) or (tg == S // 128)
                    lastt = (tg == seq_last_tile)
                    terms = []
                    if not first:
                        terms.append((ti, wi * 5 + 0))
                    terms.append((ti + 1, wi * 5 + (3 if first else (4 if lastt else 1))))
                    if not lastt:
                        terms.append((ti + 2, wi * 5 + 2))
                    for j, (slot, kind) in enumerate(terms):
                        kb.op('pe', lambda e: e.matmul(ps[:, ti * 128:(ti + 1) * 128], lhsT=uht[:, slot, c * 128:(c + 1) * 128],
                                                       rhs=band[:, kind, :], start=(j == 0), stop=(j == len(terms) - 1)),
                              reads=[uht, band], writes=[ps])
                kb.op('act', lambda e: e.copy(out=plT[:, c, :n], in_=ps[:, :n]), reads=[ps], writes=[plT])
            for c in range(KD):
                for ti in range(nt):
                    tg = tile0 + ti
                    first = (tg == 0) or (tg == S // 128)
                    lastt = (tg == seq_last_tile)
                    terms = [(ti + 1, 1)]
                    if not first:
                        terms.append((ti, 0))
                    if not lastt:
                        terms.append((ti + 2, 2))
                    pc = pcv.next()
                    for j, (slot, kind) in enumerate(terms):
                        kb.op('pe', lambda e: e.matmul(pc[:, 0:384], lhsT=uht[:, slot, D + c * 128: D + (c + 1) * 128],
                                                       rhs=scat[:, kind, :], start=(j == 0), stop=(j == len(terms) - 1)),
                              reads=[uht, scat], writes=[pc])
                    ca = cacc.next()
                    kb.op('dve', lambda e: e.tensor_scalar(out=ca[:], in0=pc[:, 0:128], scalar1=cws[:, l, c, 0:1], scalar2=None,
                                                           op0=ALU.mult), reads=[pc, cws], writes=[ca])
                    kb.op('dve', lambda e: e.scalar_tensor_tensor(out=ca[:], in0=pc[:, 128:256], scalar=cws[:, l, c, 1:2], in1=ca[:],
                                                                  op0=ALU.mult, op1=ALU.add), reads=[pc, cws, ca], writes=[ca])
                    kb.op('dve', lambda e: e.scalar_tensor_tensor(out=ca[:], in0=pc[:, 256:384], scalar=cws[:, l, c, 2:3], in1=ca[:],
                                                                  op0=ALU.mult, op1=ALU.add), reads=[pc, cws, ca], writes=[ca])
                    kb.op('pool', lambda e: e.tensor_tensor(out=bcT[:, c, ti * 128:(ti + 1) * 128], in0=ca[:],
                                                            in1=cbT[:, c, ti * 128:(ti + 1) * 128], op=ALU.mult),
                          reads=[ca, cbT], writes=[bcT])
            for dc in range(KD):
                gw = gwr.next()
                for j in range(3):
                    kb.dma('sp', gw[:, :, j, :], fm(w_in_b, GATE_OFF + j * D + dc * 128, 128), writes=[gw])
                gT = gTr.next()
                for j in range(3):
                    ps = psr.next()
                    for k in range(KD):
                        kb.op('pe', lambda e: e.matmul(ps[:, :n], lhsT=gw[:, k, j, :], rhs=xn[:, k, :n],
                                                       start=(k == 0), stop=(k == KD - 1)), reads=[gw, xn], writes=[ps])
                    kb.op('act', lambda e: e.activation(out=gT[:, j, :n], in_=ps[:, :n], func=AF.Sigmoid), reads=[ps], writes=[gT])
                kb.op('dve', lambda e: e.tensor_tensor(out=m0[:, :n], in0=att[:, dc, :n], in1=gT[:, 0, :n], op=ALU.mult),
                      reads=[att, gT], writes=[m0])
                ps = psr.next()
                g = dc // 2
                dh = dc % 2
                for kk in range(2):
                    kb.op('pe', lambda e: e.matmul(ps[:, :n], lhsT=pww[:, g * 2 + kk, dh * 128:(dh + 1) * 128], rhs=plT[:, g * 2 + kk, :n],
                                                   start=(kk == 0), stop=(kk == 1)), reads=[pww, plT], writes=[ps])
                kb.op('dve', lambda e: e.scalar_tensor_tensor(out=m1[:, :n], in0=ps[:, :n], scalar=psc[:, l, dc:dc + 1], in1=gT[:, 1, :n],
                                                              op0=ALU.mult, op1=ALU.mult), reads=[ps, psc, gT], writes=[m1])
                ps = psr.next()
                for k in range(KD):
                    kb.op('pe', lambda e: e.matmul(ps[:, :n], lhsT=cow[:, k, dc * 128:(dc + 1) * 128], rhs=bcT[:, k, :n],
                                                   start=(k == 0), stop=(k == KD - 1)), reads=[cow, bcT], writes=[ps])
                kb.op('dve', lambda e: e.tensor_tensor(out=m2[:, :n], in0=ps[:, :n], in1=gT[:, 2, :n], op=ALU.mult),
                      reads=[ps, gT], writes=[m2])
                kb.op('pool', lambda e: e.tensor_tensor(out=m0[:, :n], in0=m0[:, :n], in1=m1[:, :n], op=ALU.add),
                      reads=[m0, m1], writes=[m0])
                kb.op('pool', lambda e: e.tensor_tensor(out=mgT[:, dc, :n], in0=m0[:, :n], in1=m2[:, :n], op=ALU.add),
                      reads=[m0, m2], writes=[mgT])
            for dc in range(KD):
                ps = psr.next()
                for k in range(KD):
                    kb.op('pe', lambda e: e.matmul(ps[:, :n], lhsT=wow[:, k, dc * 128:(dc + 1) * 128], rhs=mgT[:, k, :n],
                                                   start=(k == 0), stop=(k == KD - 1)), reads=[wow, mgT], writes=[ps])
                hc_ = hcr.next()
                kb.dma('sp', hc_[:, :n], hT[dc * 128:(dc + 1) * 128, t0:t0 + n], writes=[hc_])
                hn = hnr.next()
                kb.op('dve', lambda e: e.scalar_tensor_tensor(out=hn[:, :n], in0=ps[:, :n], scalar=modT[:, l, 16 + dc, mc:mc + 1],
                                                              in1=hc_[:, :n], op0=ALU.mult, op1=ALU.add),
                      reads=[ps, modT, hc_], writes=[hn])
                kb.dma('sp', hT[dc * 128:(dc + 1) * 128, t0:t0 + n], hn[:, :n], reads=[hn])
        kb.pop("B2_%d" % l)

        kb.push()
        kys = kb.sbuf("kys", [128, 16, 128], BF16)
        kb.dma('sp', kys[:], keys_b.rearrange("p (g k) -> p g k", g=16), writes=[kys])
        hbs = [kb.sbuf("hbC%d" % i, [128, KD, SB], F32) for i in range(2)]
        xbs = [kb.sbuf("xbC%d" % i, [128, KD, SB], BF16) for i in range(2)]
        itTs = [kb.sbuf("itT%d" % i, [128, 3, SB], F32) for i in range(2)]
        sqr = Ring([kb.sbuf("sqC%d" % i, [128, SB], F32) for i in range(2)])
        tmpr = Ring([kb.sbuf("tmpC%d" % i, [128, SB], F32) for i in range(2)])
        rs = kb.sbuf("rsC", [128, SB], F32)
        wqr = Ring([kb.sbuf("wq%d" % i, [128, KD, 512], BF16) for i in range(2)])
        qp = kb.sbuf("qp", [128, 16, SB], BF16)
        bufA = kb.sbuf("bufA", [128, 2048], F32)
        bufB = kb.sbuf("bufB", [128, 2048], F32)
        stop_ = kb.sbuf("stop", [128, 16, 16], F32)
        itop = kb.sbuf("itop", [128, 16, 16], U32)
        itf = kb.sbuf("itf", [128, 16, 16], F32)
        tops = kb.sbuf("tops", [128, 8, 16], F32)
        pos = kb.sbuf("pos", [128, 8, 16], U32)
        pa = kb.sbuf("pa", [128, 8, 16], U32)
        pbb = kb.sbuf("pbb", [128, 8, 16], U32)
        paf = kb.sbuf("paf", [128, 8, 16], F32)
        pbf = kb.sbuf("pbf", [128, 8, 16], F32)
        sel3 = kb.sbuf("sel3", [128, 3, 128], F32)
        ex = kb.sbuf("ex", [128, 8, 16], F32)
        esum = kb.sbuf("esum", [128, 8], F32)
        Ar = Ring([kb.sbuf("Aoh%d" % i, [128, 16, 128], BF16) for i in range(2)])
        Br = Ring([kb.sbuf("Boh%d" % i, [128, 16, 128], BF16) for i in range(2)])
        GT = kb.sbuf("GT", [128, 128, SB], BF16)
        ucr = Ring([kb.sbuf("uc%d" % i, [128, 2, D], BF16) for i in range(2)])
        vcr = Ring([kb.sbuf("vc%d" % i, [128, 2, D], BF16) for i in range(2)])
        x2r = Ring([kb.sbuf("x2_%d" % i, [128, SB], F32) for i in range(2)])
        ttr = Ring([kb.sbuf("tt_%d" % i, [128, SB], F32) for i in range(2)])
        sgr = Ring([kb.sbuf("sg_%d" % i, [128, SB], F32) for i in range(2)])
        xgr = Ring([kb.sbuf("xg_%d" % i, [128, SB], F32) for i in range(2)])
        Wr = Ring([kb.sbuf("W_%d" % i, [128, SB], BF16) for i in range(2)])
        hnr = Ring([kb.sbuf("hnC%d" % i, [128, SB], F32) for i in range(2)])
        psn = kb.psum("psnC", [128, 512])
        psr_ = kb.psum("psrC", [128, 512])
        psq = Ring([kb.psum("psq%d" % i, [128, 512]) for i in range(2)])
        big4 = kb.psum("big4", [128, 4, 512])
        sS = bufA[:].rearrange("p (g k) -> p g k", g=16)
        sS2 = bufB[:].rearrange("p (g k) -> p g k", g=16)
        cand = bufA[:].rearrange("p (h c) -> p h c", h=8)
        cand2 = bufB[:].rearrange("p (h c) -> p h c", h=8)
        oh = bufB[:].rearrange("p (h k a) -> p h k a", h=8, k=16)
        GC = 1.5957691216057308

        def routing_gen(bi, t0, n):
            hb = hbs[bi % 2]
            xb = xbs[bi % 2]
            itT = itTs[bi % 2]
            nt = n // 128
            mc = 1 if bi >= n_lat_sblocks else 0
            kb.dma('sp', hb[:, :, :n], fm(hT, t0, n), writes=[hb])
            norm_block(hb, n, lambda k: A2[:, l, k, mc:mc + 1], lambda k: modT[:, l, 24 + k, mc:mc + 1], [xb],
                       sqr, psn, rs, tmpr)
            yield
            for wt in range(4):
                wq = wqr.next()
                kb.dma('sp', wq[:], fm(wq_b, wt * 512, 512), writes=[wq])
                for gg in range(4):
                    g = wt * 4 + gg
                    for k in range(KD):
                        kb.op('pe', lambda e: e.matmul(psr_[:, :n], lhsT=wq[:, k, gg * 128:(gg + 1) * 128], rhs=xb[:, k, :n],
                                                       start=(k == 0), stop=(k == KD - 1)), reads=[wq, xb], writes=[psr_])
                    kb.op('act', lambda e: e.copy(out=qp[:, g, :n], in_=psr_[:, :n]), reads=[psr_], writes=[qp])
                    yield
            for ti in range(nt):
                tsl = slice(ti * 128, (ti + 1) * 128)
                for a4 in range(4):
                    for gg in range(4):
                        g = a4 * 4 + gg
                        kb.op('pe', lambda e: e.matmul(psn[:, gg * 128:(gg + 1) * 128], lhsT=qp[:, g, tsl], rhs=kys[:, g, :],
                                                       start=True, stop=True), reads=[qp, kys], writes=[psn])
                    kb.op('act', lambda e: e.copy(out=bufA[:, a4 * 512:(a4 + 1) * 512], in_=psn[:]), reads=[psn], writes=[bufA])
                yield
                for g in range(16):
                    kb.op('dve', lambda e: e.max(out=stop_[:, g, 0:8], in_=sS[:, g, :]), reads=[bufA], writes=[stop_])
                    kb.op('dve', lambda e: e.max_index(out=itop[:, g, 0:8], in_max=stop_[:, g, 0:8], in_values=sS[:, g, :]),
                          reads=[bufA, stop_], writes=[itop])
                    kb.op('dve', lambda e: e.match_replace(out=sS2[:, g, :], in_to_replace=stop_[:, g, 0:8], in_values=sS[:, g, :],
                                                           imm_value=-1e30), reads=[bufA, stop_], writes=[bufB])
                    kb.op('dve', lambda e: e.max(out=stop_[:, g, 8:16], in_=sS2[:, g, :]), reads=[bufB], writes=[stop_])
                    kb.op('dve', lambda e: e.max_index(out=itop[:, g, 8:16], in_max=stop_[:, g, 8:16], in_values=sS2[:, g, :]),
                          reads=[bufB, stop_], writes=[itop])
                    yield
                kb.op('dve', lambda e: e.tensor_copy(out=itf[:], in_=itop[:]), reads=[itop], writes=[itf])
                s4 = stop_[:].rearrange("p (h i) a -> p h i a", i=2)
                kb.op('dve', lambda e: e.tensor_tensor(
                    out=cand.rearrange("p h (a b) -> p h a b", a=16),
                    in0=s4[:, :, 0, :].unsqueeze(3).to_broadcast([128, 8, 16, 16]),
                    in1=s4[:, :, 1, :].unsqueeze(2).to_broadcast([128, 8, 16, 16]), op=ALU.add), reads=[stop_], writes=[bufA])
                yield
                for h in range(8):
                    kb.op('dve', lambda e: e.max(out=tops[:, h, 0:8], in_=cand[:, h, :]), reads=[bufA], writes=[tops])
                    kb.op('dve', lambda e: e.max_index(out=pos[:, h, 0:8], in_max=tops[:, h, 0:8], in_values=cand[:, h, :]),
                          reads=[bufA, tops], writes=[pos])
                    kb.op('dve', lambda e: e.match_replace(out=cand2[:, h, :], in_to_replace=tops[:, h, 0:8], in_values=cand[:, h, :],
                                                           imm_value=-1e30), reads=[bufA, tops], writes=[bufB])
                    kb.op('dve', lambda e: e.max(out=tops[:, h, 8:16], in_=cand2[:, h, :]), reads=[bufB], writes=[tops])
                    kb.op('dve', lambda e: e.max_index(out=pos[:, h, 8:16], in_max=tops[:, h, 8:16], in_values=cand2[:, h, :]),
                          reads=[bufB, tops], writes=[pos])
                    yield
                kb.op('dve', lambda e: e.tensor_single_scalar(out=pa[:], in_=pos[:], scalar=4, op=ALU.logical_shift_right),
                      reads=[pos], writes=[pa])
                kb.op('dve', lambda e: e.tensor_single_scalar(out=pbb[:], in_=pos[:], scalar=15, op=ALU.bitwise_and),
                      reads=[pos], writes=[pbb])
                kb.op('dve', lambda e: e.tensor_copy(out=paf[:], in_=pa[:]), reads=[pa], writes=[paf])
                kb.op('dve', lambda e: e.tensor_copy(out=pbf[:], in_=pbb[:]), reads=[pbb], writes=[pbf])
                yield
                i4 = itf[:].rearrange("p (h i) a -> p h i a", i=2)
                for (pf, ii) in ((paf, 0), (pbf, 1)):
                    kb.op('dve', lambda e: e.tensor_tensor(
                        out=oh, in0=pf[:].unsqueeze(3).to_broadcast([128, 8, 16, 16]),
                        in1=iota16[:].unsqueeze(1).unsqueeze(1).to_broadcast([128, 8, 16, 16]), op=ALU.is_equal),
                        reads=[pf, iota16], writes=[bufB])
                    kb.op('dve', lambda e: e.tensor_tensor(
                        out=oh, in0=oh, in1=i4[:, :, ii, :].unsqueeze(2).to_broadcast([128, 8, 16, 16]), op=ALU.mult),
                        reads=[bufB, itf], writes=[bufB])
                    kb.op('dve', lambda e: e.tensor_reduce(out=sel3[:, ii, :].rearrange("p (h k) -> p h k", h=8), in_=oh, axis=AX.X, op=ALU.add),
                          reads=[bufB], writes=[sel3])
                    yield
                kb.op('dve', lambda e: e.tensor_tensor(out=ex[:], in0=tops[:], in1=tops[:, :, 0:1].to_broadcast([128, 8, 16]),
                                                       op=ALU.subtract), reads=[tops], writes=[ex])
                kb.op('act', lambda e: e.activation(out=ex[:], in_=ex[:], func=AF.Exp), reads=[ex], writes=[ex])
                kb.op('dve', lambda e: e.tensor_reduce(out=esum[:], in_=ex[:], axis=AX.X, op=ALU.add), reads=[ex], writes=[esum])
                kb.op('dve', lambda e: e.reciprocal(out=esum[:], in_=esum[:]), reads=[esum], writes=[esum])
                kb.op('dve', lambda e: e.tensor_tensor(out=sel3[:, 2, :].rearrange("p (h k) -> p h k", h=8), in0=ex[:],
                                                       in1=esum[:].unsqueeze(2).to_broadcast([128, 8, 16]), op=ALU.mult),
                      reads=[ex, esum], writes=[sel3])
                for x3 in range(3):
                    kb.op('pe', lambda e: e.transpose(psn[:, x3 * 128:(x3 + 1) * 128], sel3[:, x3, :], idf[:]), reads=[sel3, idf], writes=[psn])
                kb.op('act', lambda e: e.copy(out=itT[:, :, tsl], in_=psn[:, 0:384].rearrange("p (x t) -> p x t", x=3)), reads=[psn], writes=[itT])
                yield

        def g_build(bi, n):
            itT = itTs[bi % 2]
            for tg in range(0, n, 16):
                A_ = Ar.next()
                B_ = Br.next()
                io_b = iota128[:].unsqueeze(1).to_broadcast([128, 16, 128])
                kb.op('dve', lambda e: e.tensor_tensor(out=A_[:], in0=io_b, in1=itT[:, 0, tg:tg + 16].unsqueeze(2).to_broadcast([128, 16, 128]),
                                                       op=ALU.is_equal), reads=[iota128, itT], writes=[A_])
                kb.op('dve', lambda e: e.tensor_tensor(out=A_[:], in0=A_[:], in1=itT[:, 2, tg:tg + 16].unsqueeze(2).to_broadcast([128, 16, 128]),
                                                       op=ALU.mult), reads=[A_, itT], writes=[A_])
                kb.op('dve', lambda e: e.tensor_tensor(out=B_[:], in0=io_b, in1=itT[:, 1, tg:tg + 16].unsqueeze(2).to_broadcast([128, 16, 128]),
                                                       op=ALU.is_equal), reads=[iota128, itT], writes=[B_])
                for t4 in range(4):
                    pg = psq.next()
                    for tt in range(4):
                        t_ = t4 * 4 + tt
                        kb.op('pe', lambda e: e.matmul(pg[:].rearrange("p (i t) -> p i t", t=4)[:, :, tt], lhsT=B_[:, t_, :], rhs=A_[:, t_, :],
                                                       start=True, stop=True), reads=[A_, B_], writes=[pg])
                    tok0 = tg + t4 * 4
                    kb.op('act', lambda e: e.copy(out=GT[:, :, tok0:tok0 + 4], in_=pg[:].rearrange("p (i t) -> p i t", t=4)),
                          reads=[pg], writes=[GT])

        def dense(bi, n, gen):
            xb = xbs[bi % 2]

            def emit_hid(i, uc, c):
                ph = psq.next()
                for k in range(KD):
                    kb.op('pe', lambda e: e.matmul(ph[:, :n], lhsT=uc[:, c, k * 128:(k + 1) * 128], rhs=xb[:, k, :n],
                                                   start=(k == 0), stop=(k == KD - 1)), reads=[uc, xb], writes=[ph])
                return ph

            def emit_out(i, vc, c, ph):
                x2 = x2r.next()
                tt_ = ttr.next()
                sg = sgr.next()
                xg = xgr.next()
                W_ = Wr.next()
                kb.op('act', lambda e: e.activation(out=x2[:, :n], in_=ph[:, :n], func=AF.Square), reads=[ph], writes=[x2])
                kb.op('dve', lambda e: e.tensor_scalar(out=tt_[:, :n], in0=x2[:, :n], scalar1=0.044715, scalar2=1.0,
                                                       op0=ALU.mult, op1=ALU.add), reads=[x2], writes=[tt_])
                kb.op('dve', lambda e: e.tensor_tensor(out=tt_[:, :n], in0=ph[:, :n], in1=tt_[:, :n], op=ALU.mult),
                      reads=[ph, tt_], writes=[tt_])
                kb.op('act', lambda e: e.activation(out=sg[:, :n], in_=tt_[:, :n], func=AF.Sigmoid, scale=GC), reads=[tt_], writes=[sg])
                kb.op('dve', lambda e: e.tensor_tensor(out=xg[:, :n], in0=ph[:, :n], in1=GT[:, i, :n], op=ALU.mult),
                      reads=[ph, GT], writes=[xg])
                kb.op('pool', lambda e: e.tensor_tensor(out=W_[:, :n], in0=xg[:, :n], in1=sg[:, :n], op=ALU.mult),
                      reads=[xg, sg], writes=[W_])
                for dc in range(KD):
                    kb.op('pe', lambda e: e.matmul(big4[:, dc // 2, (dc % 2) * 256:(dc % 2) * 256 + n], lhsT=vc[:, c, dc * 128:(dc + 1) * 128],
                                                   rhs=W_[:, :n], start=(i == 0), stop=(i == 127)), reads=[vc, W_], writes=[big4])
            prevd = None
            for ig in range(0, 128, 2):
                uc = ucr.next()
                vc = vcr.next()
                kb.dma('sp', uc[:], uT_b[ig * 128:(ig + 2) * 128, :].rearrange("(c p) x -> p c x", p=128), writes=[uc])
                kb.dma('sp', vc[:], v_b[ig * 128:(ig + 2) * 128, :].rearrange("(c p) x -> p c x", p=128), writes=[vc])
                for c in range(2):
                    i = ig + c
                    ph = emit_hid(i, uc, c)
                    if prevd is not None:
                        emit_out(*prevd)
                    prevd = (i, vc, c, ph)
                    if gen is not None and i >= 2:
                        next(gen, None)
            emit_out(*prevd)
            if gen is not None:
                for _ in gen:
                    pass

        nsb = len(act_sblocks)
        for _ in routing_gen(0, *act_sblocks[0]):
            pass
        for bi, (t0, n) in enumerate(act_sblocks):
            mc = 1 if bi >= n_lat_sblocks else 0
            g_build(bi, n)
            gen = routing_gen(bi + 1, *act_sblocks[bi + 1]) if bi + 1 < nsb else None
            dense(bi, n, gen)
            hb = hbs[bi % 2]
            for dc in range(KD):
                hn = hnr.next()
                kb.op('dve', lambda e: e.scalar_tensor_tensor(out=hn[:, :n], in0=big4[:, dc // 2, (dc % 2) * 256:(dc % 2) * 256 + n],
                                                              scalar=modT[:, l, 40 + dc, mc:mc + 1], in1=hb[:, dc, :n],
                                                              op0=ALU.mult, op1=ALU.add), reads=[big4, modT, hb], writes=[hn])
                if last:
                    kb.dma('sp', out_hT[dc * 128:(dc + 1) * 128, t0:t0 + n], hn[:, :n], reads=[hn])
                else:
                    kb.dma('sp', hT[dc * 128:(dc + 1) * 128, t0:t0 + n], hn[:, :n], reads=[hn])
        kb.pop("C%d" % l)

    return kb.finish(), kb


def _fmaj(v):
    v = np.asarray(v, np.float32)
    lead = v.shape[:-1]
    r = v.reshape(lead + (KD, 128))
    r = np.moveaxis(r, -1, 0)
    return np.ascontiguousarray(r)


def prep_inputs(S, CT, NL, b, x, c, ctx, c_ctx, norm1_g, norm2_g, w_mod, b_mod, w_in, q_norm_g, k_norm_g,
                lam_vecs, attn_norm_g, pool_w, pool_scale, conv_w, conv_out_w, w_out, peer_wq, peer_keys, peer_u, peer_v,
                shared=None):
    f = np.float32
    if shared is None:
        cos, sin = rope_tabs(S, CT)
        shared = {
            "w_mod": np.ascontiguousarray(w_mod, f),
            "b_modT": np.ascontiguousarray(np.asarray(b_mod, f).reshape(NL, 48, 128).transpose(2, 0, 1)),
            "g1T": _fmaj(norm1_g), "g2T": _fmaj(norm2_g),
            "w_in": np.ascontiguousarray(w_in, f),
            "qk_g": np.ascontiguousarray(np.stack([np.tile(np.asarray(q_norm_g, f), (1, 2)), np.tile(np.asarray(k_norm_g, f), (1, 2))], -1).transpose(1, 0, 2)),
            "lam_v": np.ascontiguousarray(np.asarray(lam_vecs, f).reshape(NL, 256)),
            "attn_g": np.ascontiguousarray(attn_norm_g, f),
            "pool_w": np.ascontiguousarray(np.asarray(pool_w, f).reshape(NL, D, 256)),
            "pscT": _fmaj(pool_scale),
            "cwT": np.ascontiguousarray(np.moveaxis(_fmaj(conv_w), 2, 3)),
            "conv_out_w": np.ascontiguousarray(conv_out_w, f),
            "w_out": np.ascontiguousarray(w_out, f),
            "peer_wq": np.ascontiguousarray(peer_wq, f),
            "keysT": np.ascontiguousarray(np.asarray(peer_keys, f).reshape(NL, 16, 128, 128).transpose(0, 3, 1, 2).reshape(NL, 128, 2048)),
            "peer_uL": np.ascontiguousarray(np.asarray(peer_u, f).reshape(NL, 128, 128, KD, 128).transpose(0, 1, 4, 3, 2).reshape(NL, N_EXP, D)),
            "peer_v": np.ascontiguousarray(peer_v, f),
            "c_iota128": np.ascontiguousarray(np.tile(np.arange(128, dtype=f), (128, 1))),
            "cosT": cos, "sinT": sin,
            "c_rot": rot_mat(), "c_bd": bd_mat(), "c_idf": np.eye(128, dtype=f),
            "c_band": band_mats(), "c_scat": scat_mats(),
            "c_iota": np.ascontiguousarray(np.tile(np.arange(16, dtype=f), (128, 1))),
        }
    m = dict(shared)
    m["hT0"] = np.ascontiguousarray(np.concatenate([np.asarray(x[b], f).T, np.asarray(ctx[b], f).T], axis=1))
    m["cvec"] = np.ascontiguousarray(np.stack([_fmaj(c[b]), _fmaj(c_ctx)], -1))
    return m, shared


_CACHE = {}


def kernel(**inputs):
    x = np.asarray(inputs["x"])
    B, S, _ = x.shape
    CT = inputs["ctx"].shape[1]
    NL = inputs["w_mod"].shape[0]
    key = (S, CT, NL)
    if key not in _CACHE:
        _CACHE[key] = build(S, CT, NL)[0]
    nc = _CACHE[key]
    in_maps = []
    shared = None
    for core in range(8):
        m, shared = prep_inputs(S, CT, NL, core % B, shared=shared, **inputs)
        in_maps.append(m)
    res = run_bass_kernel_spmd(nc, in_maps, core_ids=list(range(8)))
    out = np.stack([np.ascontiguousarray(np.asarray(res.results[b]["out_hT"]).T) for b in range(B)], 0)
    return out.astype(np.float32)
```
